# Optimizing a Trainium2 kernel written in Bass

```python
import jax, jax.numpy as jnp
from jax import lax
import numpy as np

D_MODEL = 2048
BATCH = 4
SEQ = 2048
DEPTH = 2
DEC_BATCH = 128
DEC_SEQ = 8
PAST_LEN = 16384
PAGE_SIZE = 128

N_MIXERS = 2
N_CONV_LAYERS = (DEPTH + 1) // 2
N_GMLP_LAYERS = DEPTH // 2
D_CONV = D_MODEL
CONV_W = 31
D_GMLP = D_MODEL
CHUNK = 128
N_SG = 8
SG_DIM = D_GMLP // N_SG
D_FF = 7 * D_MODEL // 2
N_EXP = 8
TOP_K = 2
EPS_RMS = 1e-6
EPS_LN = 1e-5

kernel_name = "conformer_conv_gmlp_hybrid_step"


def rmsnorm(x, g):
    xf = x.astype(jnp.float32)
    y = xf * lax.rsqrt(jnp.mean(xf * xf, axis=-1, keepdims=True) + EPS_RMS)
    return (y * g.astype(jnp.float32)).astype(x.dtype)


def layernorm(x, g, b):
    xf = x.astype(jnp.float32)
    mu = jnp.mean(xf, axis=-1, keepdims=True)
    xc = xf - mu
    y = xc * lax.rsqrt(jnp.mean(xc * xc, axis=-1, keepdims=True) + EPS_LN)
    return (y * g.astype(jnp.float32) + b.astype(jnp.float32)).astype(x.dtype)


def conv_mixer(h, buf, w_in, b_in, w_dw, b_dw, ln_g, ln_b, w_out, b_out):
    z = h @ w_in + b_in
    a, gate = jnp.split(z, 2, axis=-1)
    glu = a * jax.nn.sigmoid(gate)
    xp = jnp.concatenate([buf.astype(glu.dtype), glu], axis=1)
    y = lax.conv_general_dilated(
        xp, w_dw[:, None, :].astype(xp.dtype), window_strides=(1,), padding="VALID",
        dimension_numbers=("NWC", "WIO", "NWC"), feature_group_count=D_CONV) + b_dw
    y = jax.nn.silu(layernorm(y, ln_g, ln_b))
    return y @ w_out + b_out, xp[:, -(CONV_W - 1):]


def gmlp_mixer(h, w_in, b_in, ln_g, ln_b, w_s, b_s, w_out, b_out):
    z = jax.nn.gelu(h @ w_in + b_in, approximate=False)
    u, v = jnp.split(z, 2, axis=-1)
    v = layernorm(v, ln_g, ln_b)
    B, T, _ = v.shape
    n_chunks = -(-T // CHUNK)
    vp = jnp.pad(v, ((0, 0), (0, n_chunks * CHUNK - T), (0, 0)))
    vc = vp.reshape(B, n_chunks, CHUNK, N_SG, SG_DIM)
    mask = jnp.tril(jnp.ones((CHUNK, CHUNK), dtype=bool))
    ws = jnp.where(mask[None], w_s, 0).astype(v.dtype)
    mixed = jnp.einsum("gts,bcsgd->bctgd", ws, vc) + b_s.T.astype(v.dtype)[None, None, :, :, None]
    mixed = mixed.reshape(B, n_chunks * CHUNK, D_GMLP)[:, :T]
    return (u * mixed) @ w_out + b_out, v


def swiglu(h, wg, wu, wd):
    return (jax.nn.silu(h @ wg) * (h @ wu)) @ wd


def moe_swiglu(h, router, wg, wu, wd):
    logits = h.astype(jnp.float32) @ router.astype(jnp.float32)
    top_v, top_i = lax.top_k(logits, TOP_K)
    gates = jax.nn.softmax(top_v, axis=-1)
    comb = jnp.sum(jax.nn.one_hot(top_i, N_EXP, dtype=jnp.float32) * gates[..., None], axis=-2)
    comb = comb.astype(h.dtype)
    out = jnp.zeros(h.shape[:-1] + (D_MODEL,), h.dtype)
    for e in range(N_EXP):
        out = out + comb[..., e:e + 1] * swiglu(h, wg[e], wu[e], wd[e])
    return out


def trunk(x, conv_bufs, keep_chunk_rows, norm_mix, norm_ffn, norm_final,
          conv_w_in, conv_b_in, conv_w_dw, conv_b_dw, conv_ln_g, conv_ln_b, conv_w_out, conv_b_out,
          gmlp_w_in, gmlp_b_in, gmlp_ln_g, gmlp_ln_b, gmlp_w_s, gmlp_b_s, gmlp_w_out, gmlp_b_out,
          ffn_w_gate, ffn_w_up, ffn_w_down, moe_router, moe_w_gate, moe_w_up, moe_w_down):
    new_bufs, v_rows = [], []
    for i in range(DEPTH):
        j = i // N_MIXERS
        h = rmsnorm(x, norm_mix[i])
        if i % N_MIXERS == 0:
            out, nb = conv_mixer(h, conv_bufs[j], conv_w_in[j], conv_b_in[j], conv_w_dw[j], conv_b_dw[j],
                                 conv_ln_g[j], conv_ln_b[j], conv_w_out[j], conv_b_out[j])
            new_bufs.append(nb)
        else:
            out, v = gmlp_mixer(h, gmlp_w_in[j], gmlp_b_in[j], gmlp_ln_g[j], gmlp_ln_b[j],
                                gmlp_w_s[j], gmlp_b_s[j], gmlp_w_out[j], gmlp_b_out[j])
            if keep_chunk_rows:
                v_rows.append(v)
        x = x + out
        h = rmsnorm(x, norm_ffn[i])
        if i % 2 == 0:
            x = x + swiglu(h, ffn_w_gate[j], ffn_w_up[j], ffn_w_down[j])
        else:
            x = x + moe_swiglu(h, moe_router[j], moe_w_gate[j], moe_w_up[j], moe_w_down[j])
    return rmsnorm(x, norm_final), new_bufs, v_rows


def setup_inputs(seed: int = 0) -> dict:
    key = jax.random.key(seed)
    ks = iter(jax.random.split(key, 40))

    def nrm(shape, scale):
        return jax.random.normal(next(ks), shape, jnp.float32) * scale

    NC, NG, ND, NM = N_CONV_LAYERS, N_GMLP_LAYERS, N_CONV_LAYERS, N_GMLP_LAYERS
    return {
        "x_prompt": nrm((BATCH, SEQ, D_MODEL), 1.0),
        "x_sample": nrm((DEC_BATCH, DEC_SEQ, D_MODEL), 1.0),
        "state_conv": nrm((NC, DEC_BATCH, CONV_W - 1, D_CONV), 0.5),
        "norm_mix": 1.0 + nrm((DEPTH, D_MODEL), 0.02),
        "norm_ffn": 1.0 + nrm((DEPTH, D_MODEL), 0.02),
        "norm_final": 1.0 + nrm((D_MODEL,), 0.02),
        "conv_w_in": nrm((NC, D_MODEL, 2 * D_CONV), D_MODEL ** -0.5),
        "conv_b_in": nrm((NC, 2 * D_CONV), 0.02),
        "conv_w_dw": nrm((NC, CONV_W, D_CONV), CONV_W ** -0.5),
        "conv_b_dw": nrm((NC, D_CONV), 0.02),
        "conv_ln_g": 1.0 + nrm((NC, D_CONV), 0.02),
        "conv_ln_b": nrm((NC, D_CONV), 0.02),
        "conv_w_out": nrm((NC, D_CONV, D_MODEL), D_CONV ** -0.5),
        "conv_b_out": nrm((NC, D_MODEL), 0.02),
        "gmlp_w_in": nrm((NG, D_MODEL, 2 * D_GMLP), D_MODEL ** -0.5),
        "gmlp_b_in": nrm((NG, 2 * D_GMLP), 0.02),
        "gmlp_ln_g": 1.0 + nrm((NG, D_GMLP), 0.02),
        "gmlp_ln_b": nrm((NG, D_GMLP), 0.02),
        "gmlp_w_s": nrm((NG, N_SG, CHUNK, CHUNK), 0.5 * CHUNK ** -0.5),
        "gmlp_b_s": 1.0 + nrm((NG, N_SG, CHUNK), 0.02),
        "gmlp_w_out": nrm((NG, D_GMLP, D_MODEL), D_GMLP ** -0.5),
        "gmlp_b_out": nrm((NG, D_MODEL), 0.02),
        "ffn_w_gate": nrm((ND, D_MODEL, D_FF), D_MODEL ** -0.5),
        "ffn_w_up": nrm((ND, D_MODEL, D_FF), D_MODEL ** -0.5),
        "ffn_w_down": nrm((ND, D_FF, D_MODEL), D_FF ** -0.5),
        "moe_router": nrm((NM, D_MODEL, N_EXP), D_MODEL ** -0.5),
        "moe_w_gate": nrm((NM, N_EXP, D_MODEL, D_FF), D_MODEL ** -0.5),
        "moe_w_up": nrm((NM, N_EXP, D_MODEL, D_FF), D_MODEL ** -0.5),
        "moe_w_down": nrm((NM, N_EXP, D_FF, D_MODEL), D_FF ** -0.5),
    }


def reference(x_prompt, x_sample, state_conv, norm_mix, norm_ffn, norm_final,
              conv_w_in, conv_b_in, conv_w_dw, conv_b_dw, conv_ln_g, conv_ln_b, conv_w_out, conv_b_out,
              gmlp_w_in, gmlp_b_in, gmlp_ln_g, gmlp_ln_b, gmlp_w_s, gmlp_b_s, gmlp_w_out, gmlp_b_out,
              ffn_w_gate, ffn_w_up, ffn_w_down, moe_router, moe_w_gate, moe_w_up, moe_w_down):
    weights = (norm_mix, norm_ffn, norm_final,
               conv_w_in, conv_b_in, conv_w_dw, conv_b_dw, conv_ln_g, conv_ln_b, conv_w_out, conv_b_out,
               gmlp_w_in, gmlp_b_in, gmlp_ln_g, gmlp_ln_b, gmlp_w_s, gmlp_b_s, gmlp_w_out, gmlp_b_out,
               ffn_w_gate, ffn_w_up, ffn_w_down, moe_router, moe_w_gate, moe_w_up, moe_w_down)
    zero_bufs = jnp.zeros((N_CONV_LAYERS, x_prompt.shape[0], CONV_W - 1, D_CONV), x_prompt.dtype)
    y_prompt, bufs_p, _ = trunk(x_prompt, zero_bufs, False, *weights)
    y_sample, bufs_s, v_s = trunk(x_sample, state_conv, True, *weights)
    new_conv_prompt = jnp.stack(bufs_p, axis=0)
    new_conv_sample = jnp.stack(bufs_s, axis=0)
    new_chunk_v_sample = jnp.stack(v_s, axis=0)
    return (y_prompt, y_sample, new_conv_prompt, new_conv_sample, new_chunk_v_sample)
```

```python
import os
import numpy as np
import concourse.bass as bass
import concourse.mybir as mybir
from concourse.bass_utils import run_bass_kernel_spmd

F32 = mybir.dt.float32
BF16 = mybir.dt.bfloat16
ALU = mybir.AluOpType
AF = mybir.ActivationFunctionType

NCORES = 8
D = 2048
KC = 16
NT = 1152
BLK = 384
NB = 3
FF = 7168
FC = 56
NE = 8
HALO = 30
NTAP = 31
NSEQ = 16
DSEQ = 8
EPS_RMS = 1e-6
EPS_LN = 1e-5

V_NM0, V_NM1, V_NF0, V_NF1, V_NFIN, V_CBA, V_CBG, V_CBDW, V_CLNG, V_CLNB, V_CBOUT, V_GBU, V_GBOUT = [
    16 * i for i in range(13)]
V_CWDW = 16 * 13
NV = 16 * 13 + NTAP * 16
R_GBV, R_LNG, R_LNB, R_BSP, R_BSS = 0, 2048, 4096, 6144, 7168
NR = 8192

ARENA_BYTES = 212000


def _prod(s):
    r = 1
    for x in s:
        r *= x
    return r


class V:
    __slots__ = ("ap", "mem", "lo", "hi")

    def __init__(self, ap, mem, lo, hi):
        self.ap, self.mem, self.lo, self.hi = ap, mem, lo, hi


class Reg:
    def __init__(self, base_ap, mem, off, shape, dtype):
        self.mem = mem
        self.off = off
        self.shape = tuple(shape)
        self.esz = 4 if dtype == F32 else 2
        n = _prod(shape)
        self.nbytes = n * self.esz
        assert off % 4 == 0 and self.nbytes % 4 == 0
        ap = base_ap[:, off // 4:(off + self.nbytes) // 4]
        if dtype != F32:
            ap = ap.bitcast(dtype)
        if len(shape) == 2:
            ap = ap.rearrange("p (a b) -> p a b", a=shape[0])
        elif len(shape) == 3:
            ap = ap.rearrange("p (a b c) -> p a b c", a=shape[0], b=shape[1])
        self.ap = ap
        self.strides = [_prod(shape[i + 1:]) for i in range(len(shape))]

    def __getitem__(self, idx):
        if not isinstance(idx, tuple):
            idx = (idx,)
        ap = self.ap[idx]
        lo = 0
        hi = 0
        for d, (sz, st) in enumerate(zip(self.shape, self.strides)):
            ix = idx[d + 1] if d + 1 < len(idx) else slice(None)
            if isinstance(ix, int):
                a, b = ix, ix + 1
            else:
                a = ix.start or 0
                b = sz if ix.stop is None else ix.stop
            lo += a * st
            hi += (b - 1) * st
        hi += 1
        return V(ap, self.mem, self.off + lo * self.esz, self.off + hi * self.esz)

    def all(self):
        return self[(slice(None),)]


class Prog:
    ENGS = ("pe", "act", "dve", "pool", "sp")

    def __init__(self, nc, sems):
        self.nc = nc
        self.sems = sems
        self.q = {e: [] for e in self.ENGS}
        self.cnt = {"pe": 0, "act": 0, "dve": 0}
        self.dcnt = {}
        self.known = {e: {} for e in self.ENGS}
        self.pe_idx = 0
        self.pe_miles_idx = []
        self.pe_miles_cnt = []
        self.recs = {"sb": [], "ps": []}
        self.out_events = {}
        self.sp_rr = 0
        self.n_wait = 0

    def _resolve(self, key, val):
        if key == "PE#":
            import bisect
            i = bisect.bisect_left(self.pe_miles_idx, val)
            if i >= len(self.pe_miles_idx):
                raise RuntimeError("PE read/write without a later milestone (idx %d)" % val)
            return "pe", self.pe_miles_cnt[i]
        return key, val

    def wait(self, eng, key, val):
        if key == "PE#" and eng == "pe":
            return
        key, val = self._resolve(key, val)
        if key == "pe" and eng == "pe":
            return
        if self.known[eng].get(key, 0) >= val:
            return
        self.known[eng][key] = val
        h = self.sems[key]
        self.q[eng].append(lambda e, h=h, v=val: e.wait_ge(h, v))
        self.n_wait += 1

    def _deps(self, eng, outs, ins):
        need = {}

        def add(evd):
            for k, v in evd.items():
                if need.get(k, -1) < v:
                    need[k] = v
        for v in ins:
            for r in self.recs[v.mem]:
                if r[0] < v.hi and v.lo < r[1]:
                    add(r[2])
        for v in outs:
            for r in self.recs[v.mem]:
                if r[0] < v.hi and v.lo < r[1]:
                    add(r[2])
                    add(r[3])
        for k, val in need.items():
            self.wait(eng, k, val)

    def _record(self, outs, ins, key, val):
        for v in ins:
            hit = False
            for r in self.recs[v.mem]:
                if r[0] < v.hi and v.lo < r[1]:
                    if r[3].get(key, -1) < val:
                        r[3][key] = val
                    if r[0] <= v.lo and v.hi <= r[1]:
                        hit = True
            if not hit:
                self.recs[v.mem].append([v.lo, v.hi, {}, {key: val}])
        for v in outs:
            lst = self.recs[v.mem]
            lst[:] = [r for r in lst if not (v.lo <= r[0] and r[1] <= v.hi)]
            lst.append([v.lo, v.hi, {key: val}, {}])

    def op(self, eng, fn, outs, ins):
        self._deps(eng, outs, ins)
        self.cnt[eng] += 1
        val = self.cnt[eng]
        h = self.sems[eng]
        self.q[eng].append(lambda e, fn=fn, h=h: fn(e).then_inc(h, 1))
        self.known[eng][eng] = max(self.known[eng].get(eng, 0), 0)
        self._record(outs, ins, eng, val)

    def mm(self, out, lhsT, rhs, start, stop, inc=False):
        self._deps("pe", [out], [lhsT, rhs])
        idx = self.pe_idx
        self.pe_idx += 1
        o, l, r = out.ap, lhsT.ap, rhs.ap
        if inc:
            self.cnt["pe"] += 1
            self.pe_miles_idx.append(idx)
            self.pe_miles_cnt.append(self.cnt["pe"])
            h = self.sems["pe"]
            self.q["pe"].append(lambda e, o=o, l=l, r=r, s=start, t=stop, h=h:
                                e.matmul(o, l, r, start=s, stop=t).then_inc(h, 1))
        else:
            self.q["pe"].append(lambda e, o=o, l=l, r=r, s=start, t=stop:
                                e.matmul(o, l, r, start=s, stop=t))
        self._record([out], [lhsT, rhs], "PE#", idx)

    def dma(self, queue, out, in_, sem=None, is_output=False):
        outs = [out] if isinstance(out, V) else []
        ins = [in_] if isinstance(in_, V) else []
        if sem is None:
            sem = "S%d" % (self.sp_rr % 8)
            self.sp_rr += 1
        prev = self.dcnt.get(sem, 0)
        if prev:
            self.wait(queue, sem, prev)
        self._deps(queue, outs, ins)
        val = prev + 16
        self.dcnt[sem] = val
        h = self.sems[sem]
        oa = out.ap if isinstance(out, V) else out
        ia = in_.ap if isinstance(in_, V) else in_
        self.q[queue].append(lambda e, oa=oa, ia=ia, h=h: e.dma_start(out=oa, in_=ia).then_inc(h, 16))
        self._record(outs, ins, sem, val)
        if is_output:
            self.out_events[sem] = val

    def act(self, out, in_, func, bias=None, scale=None):
        ins = [in_]
        kw = {}
        if bias is not None:
            if isinstance(bias, V):
                ins.append(bias)
                kw["bias"] = bias.ap
            else:
                kw["bias"] = bias
        if scale is not None:
            if isinstance(scale, V):
                ins.append(scale)
                kw["scale"] = scale.ap
            else:
                kw["scale"] = scale
        o, i = out.ap, in_.ap
        self.op("act", lambda e: e.activation(out=o, in_=i, func=func, **kw), [out], ins)

    def tt(self, out, in0, in1, op):
        o, a, b = out.ap, in0.ap, in1.ap
        self.op("dve", lambda e: e.tensor_tensor(out=o, in0=a, in1=b, op=op), [out], [in0, in1])

    def ts(self, out, in0, s1, op0, s2=None, op1=None):
        ins = [in0]
        a1 = s1
        if isinstance(s1, V):
            ins.append(s1)
            a1 = s1.ap
        a2 = s2
        if isinstance(s2, V):
            ins.append(s2)
            a2 = s2.ap
        o, a = out.ap, in0.ap
        if op1 is None:
            self.op("dve", lambda e: e.tensor_single_scalar(out=o, in_=a, scalar=a1, op=op0), [out], ins)
        else:
            self.op("dve", lambda e: e.tensor_scalar(out=o, in0=a, scalar1=a1, scalar2=a2, op0=op0, op1=op1),
                    [out], ins)

    def stt(self, out, in0, scalar, in1, op0, op1):
        ins = [in0, in1]
        sc = scalar
        if isinstance(scalar, V):
            ins.append(scalar)
            sc = scalar.ap
        o, a, b = out.ap, in0.ap, in1.ap
        self.op("dve", lambda e: e.scalar_tensor_tensor(out=o, in0=a, scalar=sc, in1=b, op0=op0, op1=op1),
                [out], ins)

    def copy(self, out, in_):
        o, a = out.ap, in_.ap
        self.op("dve", lambda e: e.tensor_copy(out=o, in_=a), [out], [in_])

    def memset(self, out, val):
        o = out.ap
        self.op("dve", lambda e: e.memset(o, val), [out], [])

    def finish(self):
        for sem, val in self.out_events.items():
            self.wait("sp", sem, val)


def build_program(stages, n_exp):
    nc = bass.Bass("TRN2", target_bir_lowering=False)

    def din(name, shape):
        return nc.dram_tensor(name, list(shape), F32, kind="ExternalInput").ap()

    def dout(name, shape):
        return nc.dram_tensor(name, list(shape), F32, kind="ExternalOutput").ap()

    d_xT = din("xT", [D, NT])
    d_xh = din("xh", [D, HALO])
    d_hmask = din("hmask", [128, 1])
    d_sconv = din("sconv", [D, NSEQ * HALO])
    d_vecs = din("vecs", [128, NV])
    d_rows = din("rows", [1, NR])
    d_wsp = din("wsp", [128, 8 * 128])
    d_wss = din("wss", [128, 8 * 128])
    d_masks = din("masks", [128, 2 * 128])
    d_ident = din("ident", [128, 128])
    d_router = din("router", [D, NE])
    d_cwin = din("conv_w_in", [D, 2 * D])
    d_cwout = din("conv_w_out", [D, D])
    d_gwin = din("gmlp_w_in", [D, 2 * D])
    d_gwout = din("gmlp_w_out", [D, D])
    d_fwg = din("ffn_wg", [D, FF])
    d_fwu = din("ffn_wu", [D, FF])
    d_fwd = din("ffn_wd", [FF, D])
    d_mwg = din("moe_wg", [NE * D, FF])
    d_mwu = din("moe_wu", [NE * D, FF])
    d_mwd = din("moe_wd", [NE * FF, D])

    o_yT = dout("yT", [D, NT])
    o_ncp = dout("ncp", [D, HALO])
    o_ncs = dout("ncs", [D, NSEQ * HALO])
    o_vout = dout("vout", [128, D])

    sem_names = ["pe", "act", "dve"] + ["R%d" % i for i in range(6)] + ["S%d" % i for i in range(8)]

    import contextlib
    with contextlib.ExitStack() as es:
        arena = es.enter_context(nc.sbuf_tensor("arena", [128, ARENA_BYTES // 4], F32))
        psum = es.enter_context(nc.psum_tensor("psum", [128, 8 * 512], F32))
        sems = {n: es.enter_context(nc.semaphore("sem_" + n)) for n in sem_names}
        P = Prog(nc, sems)

        cur = [0]

        def alloc(shape, dtype, at=None):
            esz = 4 if dtype == F32 else 2
            nb = (_prod(shape) * esz + 3) // 4 * 4
            if at is None:
                off = cur[0]
                cur[0] += nb
            else:
                off = at
            assert off + nb <= ARENA_BYTES, (off, nb)
            return Reg(arena, "sb", off, shape, dtype)

        PS = [Reg(psum, "ps", b * 2048, [512], F32) for b in range(8)]

        XT = alloc([KC, NT], F32)
        ring_off = cur[0]
        cur[0] += 6 * 8192
        VECS = alloc([NV], F32)
        RSTD = alloc([NT], F32)
        SCR = [alloc([416], F32) for _ in range(2)]
        SCR2 = [alloc([416], F32) for _ in range(2)]
        ONES = alloc([128], F32)
        IDENT = alloc([128], F32)
        MISC = alloc([64], F32)
        HALOS = alloc([KC, HALO], F32)
        stage_off = cur[0]
        STAGE_BYTES = ARENA_BYTES - stage_off

        def ring_gu(slot):
            return Reg(arena, "sb", ring_off + slot * 8192, [KC, 256], BF16)

        def ring_dn(slot):
            return Reg(arena, "sb", ring_off + slot * 8192, [2, D], BF16)

        HMASK = MISC[:, 0:1]
        EPSR = MISC[:, 1:2]
        EPSL = MISC[:, 2:3]

        def vcol(base, kc):
            return VECS[:, base + kc:base + kc + 1]

        for q4 in range(4):
            P.dma("sp", XT[:, 4 * q4:4 * q4 + 4, :],
                  d_xT[512 * q4:512 * (q4 + 1), :].rearrange("(k p) t -> p k t", p=128))
        P.dma("sp", VECS.all(), d_vecs)
        P.dma("sp", IDENT.all(), d_ident)
        P.dma("sp", MISC[:, 0:1], d_hmask)
        P.memset(ONES.all(), 1.0)
        P.memset(MISC[:, 1:2], EPS_RMS)
        P.memset(MISC[:, 2:3], EPS_LN)

        wcount = [0]

        def load_gu(w_ap, row0, col0, nslots, slot_base=0):
            slot = slot_base + wcount[0] % nslots
            wcount[0] += 1
            r = ring_gu(slot)
            src = w_ap[row0:row0 + D, col0:col0 + 256].rearrange("(k p) c -> p k c", p=128)
            P.dma("pool", r.all(), src, sem="R%d" % slot)
            return r

        def load_dn(w_ap, row0, nslots):
            slot = wcount[0] % nslots
            wcount[0] += 1
            r = ring_dn(slot)
            src = w_ap[row0:row0 + 256, :].rearrange("(j p) c -> p j c", p=128)
            P.dma("pool", r.all(), src, sem="R%d" % slot)
            return r

        def rms(xv, n, gbase, hv, rstd, out_f32=False):
            ps = PS[6][:, 0:n]
            for kc in range(KC):
                s = SCR[kc % 2][:, 0:n]
                P.act(s, xv(kc), AF.Square)
                P.mm(ps, ONES.all(), s, start=(kc == 0), stop=(kc == KC - 1), inc=True)
            P.act(rstd, ps, AF.Sqrt, bias=EPSR, scale=1.0 / D)
            o, a = rstd.ap, rstd.ap
            P.op("dve", lambda e: e.reciprocal(out=o, in_=a), [rstd], [rstd])
            for kc in range(KC):
                P.stt(hv(kc), xv(kc), vcol(gbase, kc), rstd, ALU.mult, ALU.mult)

        def out_proj(w_ap, sin, n, t0, bbase, nslots):
            unit = None
            for co in range(KC):
                if co % 2 == 0:
                    unit = load_gu(w_ap, 0, (co // 2) * 256, nslots)
                j = co % 2
                po = PS[4 + co % 2][:, 0:n]
                for kc in range(KC):
                    P.mm(po, unit[:, kc, j * 128:(j + 1) * 128], sin(kc), start=(kc == 0),
                         stop=(kc == KC - 1), inc=(kc == KC - 1))
                xs = XT[:, co, t0:t0 + n]
                P.stt(xs, po, vcol(bbase, co), xs, ALU.add, ALU.add)

        def conv_stage():
            cur[0] = stage_off
            HP = alloc([KC, 416], BF16)
            Y = alloc([KC, BLK], F32)
            ST = alloc([KC, BLK], BF16)
            GLU = [alloc([416], F32) for _ in range(2)]
            SCB = [alloc([NSEQ, HALO + DSEQ], F32) for _ in range(2)]
            XH = alloc([KC, 32], F32)
            MEAN = alloc([BLK], F32)
            RS = alloc([BLK], F32)
            MSQ = alloc([BLK], F32)
            RH = alloc([32], F32)
            P.dma("sp", XH[:, :, 0:HALO], d_xh.rearrange("(k p) t -> p k t", p=128))
            for b in range(NB):
                t0 = b * BLK
                c0 = 0 if b == 0 else HALO
                nprompt = BLK if b < 2 else 256
                rms(lambda kc: XT[:, kc, t0:t0 + BLK], BLK, V_NM0,
                    lambda kc: HP[:, kc, HALO:HALO + BLK], RSTD[:, t0:t0 + BLK])
                if b == 0:
                    rms(lambda kc: XH[:, kc, 0:HALO], HALO, V_NM0,
                        lambda kc: HP[:, kc, 0:HALO], RH[:, 0:HALO])
                ua = ug = None
                for c in range(KC):
                    if c % 2 == 0:
                        ua = load_gu(d_cwin, 0, (c // 2) * 256, 4)
                        ug = load_gu(d_cwin, 0, D + (c // 2) * 256, 4)
                    j = c % 2
                    n = HALO + BLK - c0
                    pa = PS[c % 2][:, c0:c0 + n]
                    pg = PS[2 + c % 2][:, c0:c0 + n]
                    for kc in range(KC):
                        P.mm(pa, ua[:, kc, j * 128:(j + 1) * 128], HP[:, kc, c0:c0 + n],
                             start=(kc == 0), stop=(kc == KC - 1), inc=(kc == KC - 1))
                    for kc in range(KC):
                        P.mm(pg, ug[:, kc, j * 128:(j + 1) * 128], HP[:, kc, c0:c0 + n],
                             start=(kc == 0), stop=(kc == KC - 1), inc=(kc == KC - 1))
                    sg = SCR[c % 2][:, c0:c0 + n]
                    P.act(sg, pg, AF.Sigmoid, bias=vcol(V_CBG, c))
                    glu = GLU[c % 2]
                    P.stt(glu[:, c0:c0 + n], pa, vcol(V_CBA, c), sg, ALU.add, ALU.mult)
                    if b == 0:
                        P.ts(glu[:, 0:HALO], glu[:, 0:HALO], HMASK, ALU.mult)
                    else:
                        P.copy(glu[:, 0:HALO], HALOS[:, c, :])
                    acc = Y[:, c, 0:nprompt]
                    P.ts(acc, glu[:, 0:nprompt], vcol(V_CWDW + 0, c), ALU.mult, vcol(V_CBDW, c), ALU.add)
                    for j2 in range(1, NTAP):
                        P.stt(acc, glu[:, j2:j2 + nprompt], vcol(V_CWDW + 16 * j2, c), acc, ALU.mult, ALU.add)
                    P.copy(HALOS[:, c, :], glu[:, nprompt:nprompt + HALO])
                    if b == 2:
                        scb = SCB[c % 2]
                        P.dma("sp", scb[:, :, 0:HALO],
                              d_sconv[c * 128:(c + 1) * 128, :].rearrange("p (s r) -> p s r", s=NSEQ))
                        gs = V(glu.ap[:, HALO + 256:HALO + 384].rearrange("p (s r) -> p s r", s=NSEQ), "sb",
                               glu.off + (HALO + 256) * 4, glu.off + (HALO + 384) * 4)
                        P.copy(scb[:, :, HALO:HALO + DSEQ], gs)
                        accs = V(Y.ap[:, c, 256:384].rearrange("p (s r) -> p s r", s=NSEQ), "sb",
                                 Y.off + (c * BLK + 256) * 4, Y.off + (c * BLK + 384) * 4)
                        P.ts(accs, scb[:, :, 0:DSEQ], vcol(V_CWDW + 0, c), ALU.mult, vcol(V_CBDW, c), ALU.add)
                        for j2 in range(1, NTAP):
                            P.stt(accs, scb[:, :, j2:j2 + DSEQ], vcol(V_CWDW + 16 * j2, c), accs,
                                  ALU.mult, ALU.add)
                        P.dma("sp", o_ncs[c * 128:(c + 1) * 128, :].rearrange("p (s r) -> p s r", s=NSEQ),
                              scb[:, :, DSEQ:DSEQ + HALO], is_output=True)
                if b == 2:
                    P.dma("sp", o_ncp.rearrange("(k p) r -> p k r", p=128), HALOS.all(), is_output=True)
                s1 = PS[6][:, 0:BLK]
                s2 = PS[7][:, 0:BLK]
                for c in range(KC):
                    P.mm(s1, ONES.all(), Y[:, c, :], start=(c == 0), stop=(c == KC - 1), inc=True)
                    sq = SCR[c % 2][:, 0:BLK]
                    P.act(sq, Y[:, c, :], AF.Square)
                    P.mm(s2, ONES.all(), sq, start=(c == 0), stop=(c == KC - 1), inc=True)
                P.ts(MEAN.all(), s1, 1.0 / D, ALU.mult)
                P.tt(MSQ.all(), MEAN.all(), MEAN.all(), ALU.mult)
                P.stt(MSQ.all(), s2, 1.0 / D, MSQ.all(), ALU.mult, ALU.subtract)
                P.act(RS.all(), MSQ.all(), AF.Sqrt, bias=EPSL, scale=1.0)
                o, a = RS.all().ap, RS.all().ap
                P.op("dve", lambda e: e.reciprocal(out=o, in_=a), [RS.all()], [RS.all()])
                for c in range(KC):
                    P.tt(Y[:, c, :], Y[:, c, :], MEAN.all(), ALU.subtract)
                    P.tt(Y[:, c, :], Y[:, c, :], RS.all(), ALU.mult)
                    P.act(ST[:, c, :], Y[:, c, :], AF.Silu, bias=vcol(V_CLNB, c), scale=vcol(V_CLNG, c))
                out_proj(d_cwout, lambda kc: ST[:, kc, :], BLK, t0, V_CBOUT, 4)

        def ffn_pass(HT, AT, wg, wu, wd, grow0, drow0, CB=None):
            NG = FC // 2
            units = {}

            def gu_phase(g):
                ug = load_gu(wg, grow0, g * 256, 6)
                uu = load_gu(wu, grow0, g * 256, 6)
                units[g] = load_dn(wd, drow0 + g * 256, 6)
                at = AT[g % 2]
                k = 0
                for j in range(2):
                    for blk in range(NB):
                        pg = PS[k % 2][:, 0:BLK]
                        pu = PS[2 + k % 2][:, 0:BLK]
                        for kc in range(KC):
                            P.mm(pg, ug[:, kc, j * 128:(j + 1) * 128], HT[:, kc, blk * BLK:(blk + 1) * BLK],
                                 start=(kc == 0), stop=(kc == KC - 1), inc=(kc == KC - 1))
                        for kc in range(KC):
                            P.mm(pu, uu[:, kc, j * 128:(j + 1) * 128], HT[:, kc, blk * BLK:(blk + 1) * BLK],
                                 start=(kc == 0), stop=(kc == KC - 1), inc=(kc == KC - 1))
                        sg = SCR[k % 2][:, 0:BLK]
                        P.act(sg, pg, AF.Silu)
                        a_out = at[:, j, blk * BLK:(blk + 1) * BLK]
                        if CB is None:
                            P.tt(a_out, pu, sg, ALU.mult)
                        else:
                            t2 = SCR2[k % 2][:, 0:BLK]
                            P.tt(t2, pu, sg, ALU.mult)
                            P.tt(a_out, t2, CB[:, blk * BLK:(blk + 1) * BLK], ALU.mult)
                        k += 1

            def dn_phase(g):
                ud = units.pop(g)
                at = AT[g % 2]
                k = 0
                for co in range(KC):
                    for blk in range(NB):
                        pd = PS[4 + k % 2][:, 0:BLK]
                        for j in range(2):
                            P.mm(pd, ud[:, j, co * 128:(co + 1) * 128], at[:, j, blk * BLK:(blk + 1) * BLK],
                                 start=(j == 0), stop=(j == 1), inc=(j == 1))
                        xs = XT[:, co, blk * BLK:(blk + 1) * BLK]
                        P.tt(xs, pd, xs, ALU.add)
                        k += 1

            gu_phase(0)
            for g in range(1, NG):
                gu_phase(g)
                dn_phase(g - 1)
            dn_phase(NG - 1)

        def ffn_stage():
            cur[0] = stage_off
            HT = alloc([KC, NT], BF16)
            AT = [alloc([2, NT], BF16) for _ in range(2)]
            for blk in range(NB):
                t0 = blk * BLK
                rms(lambda kc: XT[:, kc, t0:t0 + BLK], BLK, V_NF0,
                    lambda kc: HT[:, kc, t0:t0 + BLK], RSTD[:, t0:t0 + BLK])
            ffn_pass(HT, AT, d_fwg, d_fwu, d_fwd, 0, 0)

        def gmlp_stage():
            cur[0] = stage_off
            HP = alloc([KC, BLK], BF16)
            VBF = [Reg(arena, "sb", HP.off + i * 4096, [D], BF16) for i in range(2)]
            cur[0] = max(cur[0], HP.off + 12288)
            VRAW = alloc([3, D], F32)
            U = alloc([KC, BLK], BF16)
            WSM = alloc([2, 8, 128], BF16)
            BSB = alloc([2, 8, 128], F32)
            BROW = alloc([D], F32)
            STATS = alloc([4, 6], F32)
            MV = alloc([4], F32)
            LNG = Reg(arena, "sb", ring_off + 4 * 8192, [D], F32)
            LNB = Reg(arena, "sb", ring_off + 5 * 8192, [D], F32)
            WST = Reg(arena, "sb", VRAW.off, [2, 8, 128], F32)
            MSK = Reg(arena, "sb", VRAW.off + 8192, [2, 128], F32)
            P.dma("sp", WST[:, 0, :, :], d_wsp.rearrange("p (g t) -> p g t", g=8))
            P.dma("sp", WST[:, 1, :, :], d_wss.rearrange("p (g t) -> p g t", g=8))
            P.dma("sp", MSK.all(), d_masks.rearrange("p (v t) -> p v t", v=2))
            for v in range(2):
                for g in range(8):
                    P.tt(WSM[:, v, g, :], WST[:, v, g, :], MSK[:, v, :], ALU.mult)
            P.dma("sp", BSB[:, 0, :, :], d_rows[0, R_BSP:R_BSP + 1024].partition_broadcast(128)
                  .rearrange("p (g t) -> p g t", g=8))
            P.dma("sp", BSB[:, 1, :, :], d_rows[0, R_BSS:R_BSS + 1024].partition_broadcast(128)
                  .rearrange("p (g t) -> p g t", g=8))
            P.dma("sp", LNG.all(), d_rows[0, R_LNG:R_LNG + D].partition_broadcast(128))
            P.dma("sp", LNB.all(), d_rows[0, R_LNB:R_LNB + D].partition_broadcast(128))
            P.dma("sp", BROW[0:1, :], d_rows[0:1, R_GBV:R_GBV + D])
            for b in range(NB):
                t0 = b * BLK
                rms(lambda kc: XT[:, kc, t0:t0 + BLK], BLK, V_NM1,
                    lambda kc: HP[:, kc, :], RSTD[:, t0:t0 + BLK])
                unit = None
                for c in range(KC):
                    if c % 2 == 0:
                        unit = load_gu(d_gwin, 0, (c // 2) * 256, 4)
                    j = c % 2
                    pu = PS[c % 2][:, 0:BLK]
                    for kc in range(KC):
                        P.mm(pu, unit[:, kc, j * 128:(j + 1) * 128], HP[:, kc, :], start=(kc == 0),
                             stop=(kc == KC - 1), inc=(kc == KC - 1))
                    P.act(U[:, c, :], pu, AF.Gelu, bias=vcol(V_GBU, c))
                k = 0
                for c2 in range(8):
                    unit = load_gu(d_gwin, 0, D + c2 * 256, 4)
                    for t in range(3):
                        pv = PS[2 + k % 2][:, 0:256]
                        for kc in range(KC):
                            P.mm(pv, HP[:, kc, t * 128:(t + 1) * 128], unit[:, kc, :], start=(kc == 0),
                                 stop=False)
                        P.mm(pv, ONES[0:1, :], BROW[0:1, c2 * 256:(c2 + 1) * 256], start=False, stop=True,
                             inc=True)
                        P.act(VRAW[:, t, c2 * 256:(c2 + 1) * 256], pv, AF.Gelu)
                        k += 1
                for t in range(3):
                    vr = VRAW[:, t, :]
                    for q in range(4):
                        so, si = STATS[:, q, :], VRAW[:, t, q * 512:(q + 1) * 512]
                        P.op("dve", lambda e, so=so, si=si: e.bn_stats(out=so.ap, in_=si.ap), [so], [si])
                    mv = MV[:, 0:2]
                    sa = STATS.all()
                    sflat = V(STATS.ap.rearrange("p a b -> p (a b)"), "sb", STATS.off, STATS.off + STATS.nbytes)
                    P.op("dve", lambda e, mv=mv, sflat=sflat: e.bn_aggr(out=mv.ap, in_=sflat.ap), [mv], [sflat])
                    rs = MV[:, 2:3]
                    P.act(rs, MV[:, 1:2], AF.Sqrt, bias=EPSL, scale=1.0)
                    P.op("dve", lambda e, rs=rs: e.reciprocal(out=rs.ap, in_=rs.ap), [rs], [rs])
                    P.ts(vr, vr, MV[:, 0:1], ALU.subtract, rs, ALU.mult)
                    P.tt(vr, vr, LNG.all(), ALU.mult)
                    P.tt(vr, vr, LNB.all(), ALU.add)
                    sample = (b == 2 and t == 2)
                    if sample:
                        P.dma("sp", o_vout, vr, is_output=True)
                    vb = VBF[t % 2]
                    P.copy(vb.all(), vr)
                    var = 1 if sample else 0
                    for c in range(KC):
                        g = c // 2
                        pm = PS[4 + c % 2][:, 0:128]
                        P.mm(pm, vb[:, c * 128:(c + 1) * 128], WSM[:, var, g, :], start=True, stop=True, inc=True)
                        tmp = SCR[c % 2][:, 0:128]
                        P.tt(tmp, pm, BSB[:, var, g, :], ALU.add)
                        us = U[:, c, t * 128:(t + 1) * 128]
                        P.tt(us, tmp, us, ALU.mult)
                out_proj(d_gwout, lambda kc: U[:, kc, :], BLK, t0, V_GBOUT, 4)

        def moe_stage():
            cur[0] = stage_off
            HT = alloc([KC, NT], BF16)
            AT = [alloc([2, NT], BF16) for _ in range(2)]
            CB = alloc([NT], F32)
            GR = alloc([KC, NE], F32)
            COMB = alloc([9, NE], F32)
            LG = alloc([NE], F32)
            L2 = alloc([NE], F32)
            EQ1 = alloc([NE], F32)
            EQ2 = alloc([NE], F32)
            SM = alloc([8], F32)
            for blk in range(NB):
                t0 = blk * BLK
                rms(lambda kc: XT[:, kc, t0:t0 + BLK], BLK, V_NF1,
                    lambda kc: HT[:, kc, t0:t0 + BLK], RSTD[:, t0:t0 + BLK])
            P.dma("sp", GR.all(), d_router.rearrange("(k p) e -> p k e", p=128))
            for kc in range(KC):
                P.ts(GR[:, kc, :], GR[:, kc, :], vcol(V_NF1, kc), ALU.mult)
            for t in range(9):
                pl = PS[7][:, 0:NE]
                for kc in range(KC):
                    P.mm(pl, XT[:, kc, t * 128:(t + 1) * 128], GR[:, kc, :], start=(kc == 0),
                         stop=(kc == KC - 1), inc=(kc == KC - 1))
                pr = PS[6][:, 0:1]
                P.mm(pr, RSTD[0:1, t * 128:(t + 1) * 128], ONES[0:1, 0:1], start=True, stop=True, inc=True)
                rc = SM[:, 0:1]
                P.copy(rc, pr)
                P.ts(LG.all(), pl, rc, ALU.mult)
                m1 = SM[:, 1:2]
                m2 = SM[:, 2:3]
                o1, i1 = m1.ap, LG.all().ap
                P.op("dve", lambda e, o1=o1, i1=i1: e.reduce_max(out=o1, in_=i1, axis=mybir.AxisListType.X),
                     [m1], [LG.all()])
                P.ts(EQ1.all(), LG.all(), m1, ALU.is_equal)
                P.stt(L2.all(), EQ1.all(), -1e30, LG.all(), ALU.mult, ALU.add)
                o2, i2 = m2.ap, L2.all().ap
                P.op("dve", lambda e, o2=o2, i2=i2: e.reduce_max(out=o2, in_=i2, axis=mybir.AxisListType.X),
                     [m2], [L2.all()])
                P.ts(EQ2.all(), L2.all(), m2, ALU.is_equal)
                dl = SM[:, 3:4]
                P.tt(dl, m2, m1, ALU.subtract)
                ex = SM[:, 4:5]
                P.act(ex, dl, AF.Exp)
                g1 = SM[:, 5:6]
                P.ts(g1, ex, 1.0, ALU.add)
                P.op("dve", lambda e, g1=g1: e.reciprocal(out=g1.ap, in_=g1.ap), [g1], [g1])
                g2 = SM[:, 6:7]
                P.tt(g2, ex, g1, ALU.mult)
                P.ts(EQ1.all(), EQ1.all(), g1, ALU.mult)
                P.stt(COMB[:, t, :], EQ2.all(), g2, EQ1.all(), ALU.mult, ALU.add)
            for e_i in range(n_exp):
                for t in range(9):
                    lb = SCR2[t % 2][:, 0:128]
                    P.ts(lb, ONES.all(), COMB[:, t, e_i:e_i + 1], ALU.mult)
                    pc = PS[7][:, (t % 3) * 128:(t % 3 + 1) * 128]
                    P.mm(pc, lb, IDENT.all(), start=True, stop=True, inc=True)
                    if t % 3 == 2:
                        blk = t // 3
                        P.copy(CB[:, blk * BLK:(blk + 1) * BLK], PS[7][:, 0:BLK])
                ffn_pass(HT, AT, d_mwg, d_mwu, d_mwd, e_i * D, e_i * FF, CB=CB)

        def final_stage():
            cur[0] = stage_off
            YO = [alloc([KC, BLK], F32) for _ in range(2)]
            for blk in range(NB):
                t0 = blk * BLK
                yo = YO[blk % 2]
                rms(lambda kc: XT[:, kc, t0:t0 + BLK], BLK, V_NFIN,
                    lambda kc: yo[:, kc, :], RSTD[:, t0:t0 + BLK])
                for q4 in range(4):
                    P.dma("sp", o_yT[512 * q4:512 * (q4 + 1), t0:t0 + BLK].rearrange("(k p) t -> p k t", p=128),
                          yo[:, 4 * q4:4 * q4 + 4, :], is_output=True)

        if "conv" in stages:
            conv_stage()
        if "ffn" in stages:
            ffn_stage()
        if "gmlp" in stages:
            gmlp_stage()
        if "moe" in stages:
            moe_stage()
        final_stage()
        P.finish()

        with nc.Block() as block:
            @block.tensor
            def _(e):
                for fn in P.q["pe"]:
                    fn(e)

            @block.scalar
            def _(e):
                for fn in P.q["act"]:
                    fn(e)

            @block.vector
            def _(e):
                for fn in P.q["dve"]:
                    fn(e)

            @block.gpsimd
            def _(e):
                for fn in P.q["pool"]:
                    fn(e)

            @block.sync
            def _(e):
                for fn in P.q["sp"]:
                    fn(e)
        stats = {k: len(v) for k, v in P.q.items()}
        stats["waits"] = P.n_wait
    return nc, stats


def _pack_cols(vec):
    return np.ascontiguousarray(np.asarray(vec, np.float32).reshape(KC, 128).T)


def kernel(x_prompt, x_sample, state_conv, norm_mix, norm_ffn, norm_final,
           conv_w_in, conv_b_in, conv_w_dw, conv_b_dw, conv_ln_g, conv_ln_b, conv_w_out, conv_b_out,
           gmlp_w_in, gmlp_b_in, gmlp_ln_g, gmlp_ln_b, gmlp_w_s, gmlp_b_s, gmlp_w_out, gmlp_b_out,
           ffn_w_gate, ffn_w_up, ffn_w_down, moe_router, moe_w_gate, moe_w_up, moe_w_down):
    stages = os.environ.get("MK_STAGES", "conv,ffn,gmlp,moe").split(",")
    n_exp = int(os.environ.get("MK_NEXP", str(NE)))
    f = np.float32
    x_prompt = np.asarray(x_prompt, f)
    x_sample = np.asarray(x_sample, f)
    state_conv = np.asarray(state_conv, f)

    cols = [norm_mix[0], norm_mix[1], norm_ffn[0], norm_ffn[1], norm_final,
            conv_b_in[0, :D], conv_b_in[0, D:], conv_b_dw[0], conv_ln_g[0], conv_ln_b[0], conv_b_out[0],
            gmlp_b_in[0, :D], gmlp_b_out[0]]
    cols += [conv_w_dw[0, j] for j in range(NTAP)]
    vecs = np.ascontiguousarray(np.concatenate([_pack_cols(c) for c in cols], axis=1))
    assert vecs.shape == (128, NV)
    rows = np.zeros((1, NR), f)
    rows[0, R_GBV:R_GBV + D] = np.asarray(gmlp_b_in, f)[0, D:]
    rows[0, R_LNG:R_LNG + D] = np.asarray(gmlp_ln_g, f)[0]
    rows[0, R_LNB:R_LNB + D] = np.asarray(gmlp_ln_b, f)[0]
    bs = np.asarray(gmlp_b_s, f)[0]
    rows[0, R_BSP:R_BSP + 1024] = bs.reshape(-1)
    rows[0, R_BSS:R_BSS + 1024] = np.tile(bs[:, :DSEQ], (1, NSEQ)).reshape(-1)
    ws = np.asarray(gmlp_w_s, f)[0]
    wsp = np.ascontiguousarray(ws.transpose(2, 0, 1)).reshape(128, 1024)
    ws8 = ws[:, :DSEQ, :DSEQ].transpose(2, 0, 1)
    wss = np.ascontiguousarray(np.tile(ws8, (NSEQ, 1, NSEQ))).reshape(128, 1024)
    ii = np.arange(128)
    mtril = (ii[:, None] <= ii[None, :]).astype(f)
    mbd = mtril * ((ii[:, None] // DSEQ) == (ii[None, :] // DSEQ)).astype(f)
    masks = np.ascontiguousarray(np.concatenate([mtril, mbd], axis=1))
    ident = np.eye(128, dtype=f)
    shared = {
        "vecs": vecs, "rows": rows, "wsp": wsp, "wss": wss, "masks": masks, "ident": ident,
        "router": np.ascontiguousarray(np.asarray(moe_router, f)[0]),
        "conv_w_in": np.asarray(conv_w_in, f)[0], "conv_w_out": np.asarray(conv_w_out, f)[0],
        "gmlp_w_in": np.asarray(gmlp_w_in, f)[0], "gmlp_w_out": np.asarray(gmlp_w_out, f)[0],
        "ffn_wg": np.asarray(ffn_w_gate, f)[0], "ffn_wu": np.asarray(ffn_w_up, f)[0],
        "ffn_wd": np.asarray(ffn_w_down, f)[0],
        "moe_wg": np.asarray(moe_w_gate, f).reshape(NE * D, FF),
        "moe_wu": np.asarray(moe_w_up, f).reshape(NE * D, FF),
        "moe_wd": np.asarray(moe_w_down, f).reshape(NE * FF, D),
    }
    in_maps = []
    for c in range(NCORES):
        b, half = c // 2, c % 2
        xp = x_prompt[b, half * 1024:(half + 1) * 1024]
        xs = x_sample[c * NSEQ:(c + 1) * NSEQ].reshape(NSEQ * DSEQ, D)
        xT = np.ascontiguousarray(np.concatenate([xp, xs], axis=0).T)
        if half == 1:
            xh = np.ascontiguousarray(x_prompt[b, 1024 - HALO:1024].T)
        else:
            xh = np.zeros((D, HALO), f)
        hmask = np.full((128, 1), float(half), f)
        sc = state_conv[0, c * NSEQ:(c + 1) * NSEQ]
        sconv = np.ascontiguousarray(sc.transpose(2, 0, 1)).reshape(D, NSEQ * HALO)
        m = {"xT": xT, "xh": xh, "hmask": hmask, "sconv": sconv}
        m.update(shared)
        in_maps.append(m)

    nc, _ = build_program(stages, n_exp)
    res = run_bass_kernel_spmd(nc, in_maps, core_ids=list(range(NCORES)))
    outs = res.results

    y_prompt = np.empty((4, 2048, D), f)
    y_sample = np.empty((128, DSEQ, D), f)
    ncp = np.empty((1, 4, HALO, D), f)
    ncs = np.empty((1, 128, HALO, D), f)
    vout = np.empty((1, 128, DSEQ, D), f)
    for c in range(NCORES):
        b, half = c // 2, c % 2
        yT = np.asarray(outs[c]["yT"])
        y_prompt[b, half * 1024:(half + 1) * 1024] = yT[:, :1024].T
        y_sample[c * NSEQ:(c + 1) * NSEQ] = yT[:, 1024:].T.reshape(NSEQ, DSEQ, D)
        if half == 1:
            ncp[0, b] = np.asarray(outs[c]["ncp"]).T
        ncs[0, c * NSEQ:(c + 1) * NSEQ] = np.asarray(outs[c]["ncs"]).reshape(D, NSEQ, HALO).transpose(1, 2, 0)
        vout[0, c * NSEQ:(c + 1) * NSEQ] = np.asarray(outs[c]["vout"]).reshape(NSEQ, DSEQ, D)
    return (y_prompt, y_sample, ncp, ncs, vout)
```

```python
import os
import numpy as np
import concourse.bass as bass
import concourse.mybir as mybir
from concourse.bass_utils import run_bass_kernel_spmd

F32 = mybir.dt.float32
BF16 = mybir.dt.bfloat16
ALU = mybir.AluOpType
AF = mybir.ActivationFunctionType

NCORES = 8
D = 2048
KC = 16
NT = 1152
BLK = 384
NB = 3
FF = 7168
FC = 56
NE = 8
HALO = 30
NTAP = 31
NSEQ = 16
DSEQ = 8
EPS_RMS = 1e-6
EPS_LN = 1e-5

V_NM0, V_NM1, V_NF0, V_NF1, V_NFIN, V_CBA, V_CBG, V_CBDW, V_CLNG, V_CLNB, V_CBOUT, V_GBU, V_GBOUT = [
    16 * i for i in range(13)]
V_CWDW = 16 * 13
NV = 16 * 13 + NTAP * 16
R_GBV, R_LNG, R_LNB, R_BSP, R_BSS = 0, 2048, 4096, 6144, 7168
NR = 8192

ARENA_BYTES = 212800
I32 = mybir.dt.int32


def _prod(s):
    r = 1
    for x in s:
        r *= x
    return r


class V:
    __slots__ = ("ap", "mem", "lo", "hi")

    def __init__(self, ap, mem, lo, hi):
        self.ap, self.mem, self.lo, self.hi = ap, mem, lo, hi


class Reg:
    def __init__(self, base_ap, mem, off, shape, dtype):
        self.mem = mem
        self.off = off
        self.shape = tuple(shape)
        self.esz = 2 if dtype == BF16 else 4
        n = _prod(shape)
        self.nbytes = n * self.esz
        assert off % 4 == 0 and self.nbytes % 4 == 0
        ap = base_ap[:, off // 4:(off + self.nbytes) // 4]
        if dtype != F32:
            ap = ap.bitcast(dtype)
        if len(shape) == 2:
            ap = ap.rearrange("p (a b) -> p a b", a=shape[0])
        elif len(shape) == 3:
            ap = ap.rearrange("p (a b c) -> p a b c", a=shape[0], b=shape[1])
        self.ap = ap
        self.strides = [_prod(shape[i + 1:]) for i in range(len(shape))]

    def __getitem__(self, idx):
        if not isinstance(idx, tuple):
            idx = (idx,)
        ap = self.ap[idx]
        lo = 0
        hi = 0
        for d, (sz, st) in enumerate(zip(self.shape, self.strides)):
            ix = idx[d + 1] if d + 1 < len(idx) else slice(None)
            if isinstance(ix, int):
                a, b = ix, ix + 1
            else:
                a = ix.start or 0
                b = sz if ix.stop is None else ix.stop
            lo += a * st
            hi += (b - 1) * st
        hi += 1
        return V(ap, self.mem, self.off + lo * self.esz, self.off + hi * self.esz)

    def all(self):
        return self[(slice(None),)]


class Prog:
    ENGS = ("pe", "act", "dve", "pool", "sp")

    def __init__(self, nc, sems):
        self.nc = nc
        self.sems = sems
        self.q = {e: [] for e in self.ENGS}
        self.cnt = {"pe": 0, "act": 0, "dve": 0}
        self.dcnt = {}
        self.known = {e: {} for e in self.ENGS}
        self.pe_idx = 0
        self.pe_miles_idx = []
        self.pe_miles_cnt = []
        self.recs = {"sb": [], "ps": []}
        self.out_events = {}
        self.sp_rr = 0
        self.n_wait = 0

    def _resolve(self, key, val):
        if key == "PE#":
            import bisect
            i = bisect.bisect_left(self.pe_miles_idx, val)
            if i >= len(self.pe_miles_idx):
                raise RuntimeError("PE read/write without a later milestone (idx %d)" % val)
            return "pe", self.pe_miles_cnt[i]
        return key, val

    def wait(self, eng, key, val):
        if key == "PE#" and eng == "pe":
            return
        key, val = self._resolve(key, val)
        if key == "pe" and eng == "pe":
            return
        if self.known[eng].get(key, 0) >= val:
            return
        self.known[eng][key] = val
        h = self.sems[key]
        self.q[eng].append(lambda e, h=h, v=val: e.wait_ge(h, v))
        self.n_wait += 1

    def _deps(self, eng, outs, ins):
        need = {}

        def add(evd):
            for k, v in evd.items():
                if need.get(k, -1) < v:
                    need[k] = v
        for v in ins:
            for r in self.recs[v.mem]:
                if r[0] < v.hi and v.lo < r[1]:
                    add(r[2])
        for v in outs:
            for r in self.recs[v.mem]:
                if r[0] < v.hi and v.lo < r[1]:
                    add(r[2])
                    add(r[3])
        for k, val in need.items():
            self.wait(eng, k, val)

    def _record(self, outs, ins, key, val):
        for v in ins:
            hit = False
            for r in self.recs[v.mem]:
                if r[0] < v.hi and v.lo < r[1]:
                    if r[3].get(key, -1) < val:
                        r[3][key] = val
                    if r[0] <= v.lo and v.hi <= r[1]:
                        hit = True
            if not hit:
                self.recs[v.mem].append([v.lo, v.hi, {}, {key: val}])
        for v in outs:
            lst = self.recs[v.mem]
            lst[:] = [r for r in lst if not (v.lo <= r[0] and r[1] <= v.hi)]
            lst.append([v.lo, v.hi, {key: val}, {}])

    def op(self, eng, fn, outs, ins):
        self._deps(eng, outs, ins)
        self.cnt[eng] += 1
        val = self.cnt[eng]
        h = self.sems[eng]
        self.q[eng].append(lambda e, fn=fn, h=h: fn(e).then_inc(h, 1))
        self.known[eng][eng] = max(self.known[eng].get(eng, 0), 0)
        self._record(outs, ins, eng, val)

    def mm(self, out, lhsT, rhs, start, stop, inc=False):
        self._deps("pe", [out], [lhsT, rhs])
        idx = self.pe_idx
        self.pe_idx += 1
        o, l, r = out.ap, lhsT.ap, rhs.ap
        if inc:
            self.cnt["pe"] += 1
            self.pe_miles_idx.append(idx)
            self.pe_miles_cnt.append(self.cnt["pe"])
            h = self.sems["pe"]
            self.q["pe"].append(lambda e, o=o, l=l, r=r, s=start, t=stop, h=h:
                                e.matmul(o, l, r, start=s, stop=t).then_inc(h, 1))
        else:
            self.q["pe"].append(lambda e, o=o, l=l, r=r, s=start, t=stop:
                                e.matmul(o, l, r, start=s, stop=t))
        self._record([out], [lhsT, rhs], "PE#", idx)

    def dma(self, queue, out, in_, sem=None, is_output=False):
        outs = [out] if isinstance(out, V) else []
        ins = [in_] if isinstance(in_, V) else []
        if sem is None:
            sem = "S%d" % (self.sp_rr % 8)
            self.sp_rr += 1
        prev = self.dcnt.get(sem, 0)
        if prev:
            self.wait(queue, sem, prev)
        self._deps(queue, outs, ins)
        val = prev + 16
        self.dcnt[sem] = val
        h = self.sems[sem]
        oa = out.ap if isinstance(out, V) else out
        ia = in_.ap if isinstance(in_, V) else in_
        self.q[queue].append(lambda e, oa=oa, ia=ia, h=h: e.dma_start(out=oa, in_=ia).then_inc(h, 16))
        self._record(outs, ins, sem, val)
        if is_output:
            self.out_events[sem] = val

    def act(self, out, in_, func, bias=None, scale=None):
        ins = [in_]
        kw = {}
        if bias is not None:
            if isinstance(bias, V):
                ins.append(bias)
                kw["bias"] = bias.ap
            else:
                kw["bias"] = bias
        if scale is not None:
            if isinstance(scale, V):
                ins.append(scale)
                kw["scale"] = scale.ap
            else:
                kw["scale"] = scale
        o, i = out.ap, in_.ap
        self.op("act", lambda e: e.activation(out=o, in_=i, func=func, **kw), [out], ins)

    def tt(self, out, in0, in1, op):
        o, a, b = out.ap, in0.ap, in1.ap
        self.op("dve", lambda e: e.tensor_tensor(out=o, in0=a, in1=b, op=op), [out], [in0, in1])

    def ts(self, out, in0, s1, op0, s2=None, op1=None):
        ins = [in0]
        a1 = s1
        if isinstance(s1, V):
            ins.append(s1)
            a1 = s1.ap
        a2 = s2
        if isinstance(s2, V):
            ins.append(s2)
            a2 = s2.ap
        o, a = out.ap, in0.ap
        if op1 is None:
            self.op("dve", lambda e: e.tensor_single_scalar(out=o, in_=a, scalar=a1, op=op0), [out], ins)
        else:
            self.op("dve", lambda e: e.tensor_scalar(out=o, in0=a, scalar1=a1, scalar2=a2, op0=op0, op1=op1),
                    [out], ins)

    def stt(self, out, in0, scalar, in1, op0, op1):
        ins = [in0, in1]
        sc = scalar
        if isinstance(scalar, V):
            ins.append(scalar)
            sc = scalar.ap
        o, a, b = out.ap, in0.ap, in1.ap
        self.op("dve", lambda e: e.scalar_tensor_tensor(out=o, in0=a, scalar=sc, in1=b, op0=op0, op1=op1),
                [out], ins)

    def copy(self, out, in_):
        o, a = out.ap, in_.ap
        self.op("dve", lambda e: e.tensor_copy(out=o, in_=a), [out], [in_])

    def memset(self, out, val):
        o = out.ap
        self.op("dve", lambda e: e.memset(o, val), [out], [])

    def finish(self):
        for sem, val in self.out_events.items():
            self.wait("sp", sem, val)

    def region_begin(self, engines, flag_view):
        self._region = dict(engines=engines, known={e: dict(self.known[e]) for e in engines},
                            cnt0=dict(self.cnt), dcnt0=dict(self.dcnt))
        for e in engines:
            self._deps(e, [], [flag_view])
            self.q[e].append(("if", flag_view.ap))

    def region_end(self):
        r = self._region
        self._region = None
        for e in r["engines"]:
            comp = []
            if e in self.cnt:
                m0, m1 = r["cnt0"][e], self.cnt[e]
                if m1 > m0:
                    comp.append((self.sems[e], m0, m1 - m0))
            if e == "pool":
                for sname, v1 in self.dcnt.items():
                    v0 = r["dcnt0"].get(sname, 0)
                    if sname.startswith("R") and v1 > v0:
                        comp.append((self.sems[sname], v0, v1 - v0))
            self.q[e].append(("else", comp))
            self.q[e].append(("endif",))
            self.known[e] = r["known"][e]


def replay(e, items):
    stack = []
    rguard = e.register("flag")
    reg = rguard.__enter__()
    for it in items:
        if isinstance(it, tuple):
            if it[0] == "if":
                e.reg_load(reg, it[1])
                g = e.If_ne(reg, 0)
                g.__enter__()
                stack.append(g)
            elif it[0] == "else":
                stack.pop().__exit__(None, None, None)
                g = e.Else()
                g.__enter__()
                stack.append(g)
                for semh, v0, n in it[1]:
                    if v0 > 0:
                        e.wait_ge(semh, v0)
                    e.sem_inc(semh, n)
            else:
                stack.pop().__exit__(None, None, None)
        else:
            it(e)
    rguard.__exit__(None, None, None)


def build_program(stages, n_exp):
    nc = bass.Bass("TRN2", target_bir_lowering=False)

    def din(name, shape):
        return nc.dram_tensor(name, list(shape), F32, kind="ExternalInput").ap()

    def dout(name, shape):
        return nc.dram_tensor(name, list(shape), F32, kind="ExternalOutput").ap()

    d_xT = din("xT", [D, NT])
    d_xh = din("xh", [D, HALO])
    d_hmask = din("hmask", [128, 1])
    d_sconv = din("sconv", [D, NSEQ * HALO])
    d_vecs = din("vecs", [128, NV])
    d_rows = din("rows", [1, NR])
    d_wsp = din("wsp", [128, 8 * 128])
    d_wss = din("wss", [128, 8 * 128])
    d_masks = din("masks", [128, 2 * 128])
    d_ident = din("ident", [128, 128])
    d_router = din("router", [D, NE])
    d_iotas = din("iotas", [128, 512 + 4])
    d_ut = din("ut", [128, 128])
    d_cwin = din("conv_w_in", [D, 2 * D])
    d_cwout = din("conv_w_out", [D, D])
    d_gwin = din("gmlp_w_in", [D, 2 * D])
    d_gwout = din("gmlp_w_out", [D, D])
    d_fwg = din("ffn_wg", [D, FF])
    d_fwu = din("ffn_wu", [D, FF])
    d_fwd = din("ffn_wd", [FF, D])
    d_mwg = din("moe_wg", [NE * D, FF])
    d_mwu = din("moe_wu", [NE * D, FF])
    d_mwd = din("moe_wd", [NE * FF, D])

    o_yT = dout("yT", [D, NT])
    o_ncp = dout("ncp", [D, HALO])
    o_ncs = dout("ncs", [D, NSEQ * HALO])
    o_vout = dout("vout", [128, D])

    sem_names = ["pe", "act", "dve"] + ["R%d" % i for i in range(6)] + ["S%d" % i for i in range(8)]

    import contextlib
    with contextlib.ExitStack() as es:
        arena = es.enter_context(nc.sbuf_tensor("arena", [128, ARENA_BYTES // 4], F32))
        psum = es.enter_context(nc.psum_tensor("psum", [128, 8 * 512], F32))
        sems = {n: es.enter_context(nc.semaphore("sem_" + n)) for n in sem_names}
        P = Prog(nc, sems)

        cur = [0]

        def alloc(shape, dtype, at=None):
            esz = 4 if dtype == F32 else 2
            nb = (_prod(shape) * esz + 3) // 4 * 4
            if at is None:
                off = cur[0]
                cur[0] += nb
            else:
                off = at
            assert off + nb <= ARENA_BYTES, (off, nb)
            return Reg(arena, "sb", off, shape, dtype)

        PS = [Reg(psum, "ps", b * 2048, [512], F32) for b in range(8)]

        XT = alloc([KC, NT], F32)
        ring_off = cur[0]
        cur[0] += 6 * 8192
        VECS = alloc([NV], F32)
        RSTD = alloc([NT], F32)
        SCR = [alloc([416], F32) for _ in range(2)]
        SCR2 = [alloc([416], F32) for _ in range(2)]
        ONES = alloc([128], F32)
        IDENT = alloc([128], F32)
        MISC = alloc([64], F32)
        HALOS = alloc([KC, HALO], F32)
        stage_off = cur[0]
        STAGE_BYTES = ARENA_BYTES - stage_off

        def ring_gu(slot):
            return Reg(arena, "sb", ring_off + slot * 8192, [KC, 256], BF16)

        def ring_dn(slot):
            return Reg(arena, "sb", ring_off + slot * 8192, [2, D], BF16)

        HMASK = MISC[:, 0:1]
        EPSR = MISC[:, 1:2]
        EPSL = MISC[:, 2:3]

        def vcol(base, kc):
            return VECS[:, base + kc:base + kc + 1]

        for q4 in range(4):
            P.dma("sp", XT[:, 4 * q4:4 * q4 + 4, :],
                  d_xT[512 * q4:512 * (q4 + 1), :].rearrange("(k p) t -> p k t", p=128))
        P.dma("sp", VECS.all(), d_vecs)
        P.dma("sp", IDENT.all(), d_ident)
        P.dma("sp", MISC[:, 0:1], d_hmask)
        P.memset(ONES.all(), 1.0)
        P.memset(MISC[:, 1:2], EPS_RMS)
        P.memset(MISC[:, 2:3], EPS_LN)

        wcount = [0]

        def load_gu(w_ap, row0, col0, nslots, slot_base=0):
            slot = slot_base + wcount[0] % nslots
            wcount[0] += 1
            r = ring_gu(slot)
            src = w_ap[row0:row0 + D, col0:col0 + 256].rearrange("(k p) c -> p k c", p=128)
            P.dma("pool", r.all(), src, sem="R%d" % slot)
            return r

        def load_dn(w_ap, row0, nslots):
            slot = wcount[0] % nslots
            wcount[0] += 1
            r = ring_dn(slot)
            src = w_ap[row0:row0 + 256, :].rearrange("(j p) c -> p j c", p=128)
            P.dma("pool", r.all(), src, sem="R%d" % slot)
            return r

        def rms(xv, n, gbase, hv, rstd, out_f32=False):
            ps = PS[6][:, 0:n]
            for kc in range(KC):
                s = SCR[kc % 2][:, 0:n]
                P.act(s, xv(kc), AF.Square)
                P.mm(ps, ONES.all(), s, start=(kc == 0), stop=(kc == KC - 1), inc=True)
            P.act(rstd, ps, AF.Sqrt, bias=EPSR, scale=1.0 / D)
            o, a = rstd.ap, rstd.ap
            P.op("dve", lambda e: e.reciprocal(out=o, in_=a), [rstd], [rstd])
            for kc in range(KC):
                P.stt(hv(kc), xv(kc), vcol(gbase, kc), rstd, ALU.mult, ALU.mult)

        def out_proj(w_ap, sin, n, t0, bbase, nslots):
            unit = None
            for co in range(KC):
                if co % 2 == 0:
                    unit = load_gu(w_ap, 0, (co // 2) * 256, nslots)
                j = co % 2
                po = PS[4 + co % 2][:, 0:n]
                for kc in range(KC):
                    P.mm(po, unit[:, kc, j * 128:(j + 1) * 128], sin(kc), start=(kc == 0),
                         stop=(kc == KC - 1), inc=(kc == KC - 1))
                xs = XT[:, co, t0:t0 + n]
                P.stt(xs, po, vcol(bbase, co), xs, ALU.add, ALU.add)

        def conv_stage():
            cur[0] = stage_off
            HP = alloc([KC, 416], BF16)
            Y = alloc([KC, BLK], F32)
            ST = alloc([KC, BLK], BF16)
            GLU = [alloc([416], F32) for _ in range(2)]
            SCB = [alloc([NSEQ, HALO + DSEQ], F32) for _ in range(2)]
            XH = alloc([KC, 32], F32)
            MEAN = alloc([BLK], F32)
            RS = alloc([BLK], F32)
            MSQ = alloc([BLK], F32)
            RH = alloc([32], F32)
            P.dma("sp", XH[:, :, 0:HALO], d_xh.rearrange("(k p) t -> p k t", p=128))
            for b in range(NB):
                t0 = b * BLK
                c0 = 0 if b == 0 else HALO
                nprompt = BLK if b < 2 else 256
                rms(lambda kc: XT[:, kc, t0:t0 + BLK], BLK, V_NM0,
                    lambda kc: HP[:, kc, HALO:HALO + BLK], RSTD[:, t0:t0 + BLK])
                if b == 0:
                    rms(lambda kc: XH[:, kc, 0:HALO], HALO, V_NM0,
                        lambda kc: HP[:, kc, 0:HALO], RH[:, 0:HALO])
                ua = ug = None
                for c in range(KC):
                    if c % 2 == 0:
                        ua = load_gu(d_cwin, 0, (c // 2) * 256, 4)
                        ug = load_gu(d_cwin, 0, D + (c // 2) * 256, 4)
                    j = c % 2
                    n = HALO + BLK - c0
                    pa = PS[c % 2][:, c0:c0 + n]
                    pg = PS[2 + c % 2][:, c0:c0 + n]
                    for kc in range(KC):
                        P.mm(pa, ua[:, kc, j * 128:(j + 1) * 128], HP[:, kc, c0:c0 + n],
                             start=(kc == 0), stop=(kc == KC - 1), inc=(kc == KC - 1))
                    for kc in range(KC):
                        P.mm(pg, ug[:, kc, j * 128:(j + 1) * 128], HP[:, kc, c0:c0 + n],
                             start=(kc == 0), stop=(kc == KC - 1), inc=(kc == KC - 1))
                    sg = SCR[c % 2][:, c0:c0 + n]
                    P.act(sg, pg, AF.Sigmoid, bias=vcol(V_CBG, c))
                    glu = GLU[c % 2]
                    P.stt(glu[:, c0:c0 + n], pa, vcol(V_CBA, c), sg, ALU.add, ALU.mult)
                    if b == 0:
                        P.ts(glu[:, 0:HALO], glu[:, 0:HALO], HMASK, ALU.mult)
                    else:
                        P.copy(glu[:, 0:HALO], HALOS[:, c, :])
                    acc = Y[:, c, 0:nprompt]
                    P.ts(acc, glu[:, 0:nprompt], vcol(V_CWDW + 0, c), ALU.mult, vcol(V_CBDW, c), ALU.add)
                    for j2 in range(1, NTAP):
                        P.stt(acc, glu[:, j2:j2 + nprompt], vcol(V_CWDW + 16 * j2, c), acc, ALU.mult, ALU.add)
                    P.copy(HALOS[:, c, :], glu[:, nprompt:nprompt + HALO])
                    if b == 2:
                        scb = SCB[c % 2]
                        P.dma("sp", scb[:, :, 0:HALO],
                              d_sconv[c * 128:(c + 1) * 128, :].rearrange("p (s r) -> p s r", s=NSEQ))
                        gs = V(glu.ap[:, HALO + 256:HALO + 384].rearrange("p (s r) -> p s r", s=NSEQ), "sb",
                               glu.off + (HALO + 256) * 4, glu.off + (HALO + 384) * 4)
                        P.copy(scb[:, :, HALO:HALO + DSEQ], gs)
                        accs = V(Y.ap[:, c, 256:384].rearrange("p (s r) -> p s r", s=NSEQ), "sb",
                                 Y.off + (c * BLK + 256) * 4, Y.off + (c * BLK + 384) * 4)
                        P.ts(accs, scb[:, :, 0:DSEQ], vcol(V_CWDW + 0, c), ALU.mult, vcol(V_CBDW, c), ALU.add)
                        for j2 in range(1, NTAP):
                            P.stt(accs, scb[:, :, j2:j2 + DSEQ], vcol(V_CWDW + 16 * j2, c), accs,
                                  ALU.mult, ALU.add)
                        P.dma("sp", o_ncs[c * 128:(c + 1) * 128, :].rearrange("p (s r) -> p s r", s=NSEQ),
                              scb[:, :, DSEQ:DSEQ + HALO], is_output=True)
                if b == 2:
                    P.dma("sp", o_ncp.rearrange("(k p) r -> p k r", p=128), HALOS.all(), is_output=True)
                s1 = PS[6][:, 0:BLK]
                s2 = PS[7][:, 0:BLK]
                for c in range(KC):
                    P.mm(s1, ONES.all(), Y[:, c, :], start=(c == 0), stop=(c == KC - 1), inc=True)
                    sq = SCR[c % 2][:, 0:BLK]
                    P.act(sq, Y[:, c, :], AF.Square)
                    P.mm(s2, ONES.all(), sq, start=(c == 0), stop=(c == KC - 1), inc=True)
                P.ts(MEAN.all(), s1, 1.0 / D, ALU.mult)
                P.tt(MSQ.all(), MEAN.all(), MEAN.all(), ALU.mult)
                P.stt(MSQ.all(), s2, 1.0 / D, MSQ.all(), ALU.mult, ALU.subtract)
                P.act(RS.all(), MSQ.all(), AF.Sqrt, bias=EPSL, scale=1.0)
                o, a = RS.all().ap, RS.all().ap
                P.op("dve", lambda e: e.reciprocal(out=o, in_=a), [RS.all()], [RS.all()])
                for c in range(KC):
                    P.tt(Y[:, c, :], Y[:, c, :], MEAN.all(), ALU.subtract)
                    P.tt(Y[:, c, :], Y[:, c, :], RS.all(), ALU.mult)
                    P.act(ST[:, c, :], Y[:, c, :], AF.Silu, bias=vcol(V_CLNB, c), scale=vcol(V_CLNG, c))
                out_proj(d_cwout, lambda kc: ST[:, kc, :], BLK, t0, V_CBOUT, 4)

        def ffn_pass(HT, AT, wg, wu, wd, grow0, drow0, CB=None):
            NG = FC // 2
            units = {}

            def gu_phase(g):
                ug = load_gu(wg, grow0, g * 256, 6)
                uu = load_gu(wu, grow0, g * 256, 6)
                units[g] = load_dn(wd, drow0 + g * 256, 6)
                at = AT[g % 2]
                k = 0
                for j in range(2):
                    for blk in range(NB):
                        pg = PS[k % 2][:, 0:BLK]
                        pu = PS[2 + k % 2][:, 0:BLK]
                        for kc in range(KC):
                            P.mm(pg, ug[:, kc, j * 128:(j + 1) * 128], HT[:, kc, blk * BLK:(blk + 1) * BLK],
                                 start=(kc == 0), stop=(kc == KC - 1), inc=(kc == KC - 1))
                        for kc in range(KC):
                            P.mm(pu, uu[:, kc, j * 128:(j + 1) * 128], HT[:, kc, blk * BLK:(blk + 1) * BLK],
                                 start=(kc == 0), stop=(kc == KC - 1), inc=(kc == KC - 1))
                        sg = SCR[k % 2][:, 0:BLK]
                        P.act(sg, pg, AF.Silu)
                        a_out = at[:, j, blk * BLK:(blk + 1) * BLK]
                        if CB is None:
                            P.tt(a_out, pu, sg, ALU.mult)
                        else:
                            t2 = SCR2[k % 2][:, 0:BLK]
                            P.tt(t2, pu, sg, ALU.mult)
                            P.tt(a_out, t2, CB[:, blk * BLK:(blk + 1) * BLK], ALU.mult)
                        k += 1

            def dn_phase(g):
                ud = units.pop(g)
                at = AT[g % 2]
                k = 0
                for co in range(KC):
                    for blk in range(NB):
                        pd = PS[4 + k % 2][:, 0:BLK]
                        for j in range(2):
                            P.mm(pd, ud[:, j, co * 128:(co + 1) * 128], at[:, j, blk * BLK:(blk + 1) * BLK],
                                 start=(j == 0), stop=(j == 1), inc=(j == 1))
                        xs = XT[:, co, blk * BLK:(blk + 1) * BLK]
                        P.tt(xs, pd, xs, ALU.add)
                        k += 1

            gu_phase(0)
            for g in range(1, NG):
                gu_phase(g)
                dn_phase(g - 1)
            dn_phase(NG - 1)

        def ffn_stage():
            cur[0] = stage_off
            HT = alloc([KC, NT], BF16)
            AT = [alloc([2, NT], BF16) for _ in range(2)]
            for blk in range(NB):
                t0 = blk * BLK
                rms(lambda kc: XT[:, kc, t0:t0 + BLK], BLK, V_NF0,
                    lambda kc: HT[:, kc, t0:t0 + BLK], RSTD[:, t0:t0 + BLK])
            ffn_pass(HT, AT, d_fwg, d_fwu, d_fwd, 0, 0)

        def gmlp_stage():
            cur[0] = stage_off
            HP = alloc([KC, BLK], BF16)
            VBF = [Reg(arena, "sb", HP.off + i * 4096, [D], BF16) for i in range(2)]
            cur[0] = max(cur[0], HP.off + 12288)
            VRAW = alloc([3, D], F32)
            U = alloc([KC, BLK], BF16)
            WSM = alloc([2, 8, 128], BF16)
            BSB = alloc([2, 8, 128], F32)
            BROW = alloc([D], F32)
            STATS = alloc([4, 6], F32)
            MV = alloc([4], F32)
            LNG = Reg(arena, "sb", ring_off + 4 * 8192, [D], F32)
            LNB = Reg(arena, "sb", ring_off + 5 * 8192, [D], F32)
            WST = Reg(arena, "sb", VRAW.off, [2, 8, 128], F32)
            MSK = Reg(arena, "sb", VRAW.off + 8192, [2, 128], F32)
            P.dma("sp", WST[:, 0, :, :], d_wsp.rearrange("p (g t) -> p g t", g=8))
            P.dma("sp", WST[:, 1, :, :], d_wss.rearrange("p (g t) -> p g t", g=8))
            P.dma("sp", MSK.all(), d_masks.rearrange("p (v t) -> p v t", v=2))
            for v in range(2):
                for g in range(8):
                    P.tt(WSM[:, v, g, :], WST[:, v, g, :], MSK[:, v, :], ALU.mult)
            P.dma("sp", BSB[:, 0, :, :], d_rows[0, R_BSP:R_BSP + 1024].partition_broadcast(128)
                  .rearrange("p (g t) -> p g t", g=8))
            P.dma("sp", BSB[:, 1, :, :], d_rows[0, R_BSS:R_BSS + 1024].partition_broadcast(128)
                  .rearrange("p (g t) -> p g t", g=8))
            P.dma("sp", LNG.all(), d_rows[0, R_LNG:R_LNG + D].partition_broadcast(128))
            P.dma("sp", LNB.all(), d_rows[0, R_LNB:R_LNB + D].partition_broadcast(128))
            P.dma("sp", BROW[0:1, :], d_rows[0:1, R_GBV:R_GBV + D])
            for b in range(NB):
                t0 = b * BLK
                rms(lambda kc: XT[:, kc, t0:t0 + BLK], BLK, V_NM1,
                    lambda kc: HP[:, kc, :], RSTD[:, t0:t0 + BLK])
                unit = None
                for c in range(KC):
                    if c % 2 == 0:
                        unit = load_gu(d_gwin, 0, (c // 2) * 256, 4)
                    j = c % 2
                    pu = PS[c % 2][:, 0:BLK]
                    for kc in range(KC):
                        P.mm(pu, unit[:, kc, j * 128:(j + 1) * 128], HP[:, kc, :], start=(kc == 0),
                             stop=(kc == KC - 1), inc=(kc == KC - 1))
                    P.act(U[:, c, :], pu, AF.Gelu, bias=vcol(V_GBU, c))
                k = 0
                for c2 in range(8):
                    unit = load_gu(d_gwin, 0, D + c2 * 256, 4)
                    for t in range(3):
                        pv = PS[2 + k % 2][:, 0:256]
                        for kc in range(KC):
                            P.mm(pv, HP[:, kc, t * 128:(t + 1) * 128], unit[:, kc, :], start=(kc == 0),
                                 stop=False)
                        P.mm(pv, ONES[0:1, :], BROW[0:1, c2 * 256:(c2 + 1) * 256], start=False, stop=True,
                             inc=True)
                        P.act(VRAW[:, t, c2 * 256:(c2 + 1) * 256], pv, AF.Gelu)
                        k += 1
                for t in range(3):
                    vr = VRAW[:, t, :]
                    for q in range(4):
                        so, si = STATS[:, q, :], VRAW[:, t, q * 512:(q + 1) * 512]
                        P.op("dve", lambda e, so=so, si=si: e.bn_stats(out=so.ap, in_=si.ap), [so], [si])
                    mv = MV[:, 0:2]
                    sa = STATS.all()
                    sflat = V(STATS.ap.rearrange("p a b -> p (a b)"), "sb", STATS.off, STATS.off + STATS.nbytes)
                    P.op("dve", lambda e, mv=mv, sflat=sflat: e.bn_aggr(out=mv.ap, in_=sflat.ap), [mv], [sflat])
                    rs = MV[:, 2:3]
                    P.act(rs, MV[:, 1:2], AF.Sqrt, bias=EPSL, scale=1.0)
                    P.op("dve", lambda e, rs=rs: e.reciprocal(out=rs.ap, in_=rs.ap), [rs], [rs])
                    P.ts(vr, vr, MV[:, 0:1], ALU.subtract, rs, ALU.mult)
                    P.tt(vr, vr, LNG.all(), ALU.mult)
                    P.tt(vr, vr, LNB.all(), ALU.add)
                    sample = (b == 2 and t == 2)
                    if sample:
                        P.dma("sp", o_vout, vr, is_output=True)
                    vb = VBF[t % 2]
                    P.copy(vb.all(), vr)
                    var = 1 if sample else 0
                    for c in range(KC):
                        g = c // 2
                        pm = PS[4 + c % 2][:, 0:128]
                        P.mm(pm, vb[:, c * 128:(c + 1) * 128], WSM[:, var, g, :], start=True, stop=True, inc=True)
                        tmp = SCR[c % 2][:, 0:128]
                        P.tt(tmp, pm, BSB[:, var, g, :], ALU.add)
                        us = U[:, c, t * 128:(t + 1) * 128]
                        P.tt(us, tmp, us, ALU.mult)
                out_proj(d_gwout, lambda kc: U[:, kc, :], BLK, t0, V_GBOUT, 4)

        def moe_stage():
            cur[0] = stage_off
            HT = alloc([KC, NT], BF16)
            AT = [alloc([2, NT], BF16) for _ in range(2)]
            CB = alloc([NT], F32)
            GR = alloc([KC, NE], F32)
            COMB = alloc([9, NE], F32)
            LG = alloc([NE], F32)
            L2 = alloc([NE], F32)
            EQ1 = alloc([NE], F32)
            EQ2 = alloc([NE], F32)
            SM = alloc([8], F32)
            for blk in range(NB):
                t0 = blk * BLK
                rms(lambda kc: XT[:, kc, t0:t0 + BLK], BLK, V_NF1,
                    lambda kc: HT[:, kc, t0:t0 + BLK], RSTD[:, t0:t0 + BLK])
            P.dma("sp", GR.all(), d_router.rearrange("(k p) e -> p k e", p=128))
            for kc in range(KC):
                P.ts(GR[:, kc, :], GR[:, kc, :], vcol(V_NF1, kc), ALU.mult)
            for t in range(9):
                pl = PS[7][:, 0:NE]
                for kc in range(KC):
                    P.mm(pl, XT[:, kc, t * 128:(t + 1) * 128], GR[:, kc, :], start=(kc == 0),
                         stop=(kc == KC - 1), inc=(kc == KC - 1))
                pr = PS[6][:, 0:1]
                P.mm(pr, RSTD[0:1, t * 128:(t + 1) * 128], ONES[0:1, 0:1], start=True, stop=True, inc=True)
                rc = SM[:, 0:1]
                P.copy(rc, pr)
                P.ts(LG.all(), pl, rc, ALU.mult)
                m1 = SM[:, 1:2]
                m2 = SM[:, 2:3]
                o1, i1 = m1.ap, LG.all().ap
                P.op("dve", lambda e, o1=o1, i1=i1: e.reduce_max(out=o1, in_=i1, axis=mybir.AxisListType.X),
                     [m1], [LG.all()])
                P.ts(EQ1.all(), LG.all(), m1, ALU.is_equal)
                P.stt(L2.all(), EQ1.all(), -1e30, LG.all(), ALU.mult, ALU.add)
                o2, i2 = m2.ap, L2.all().ap
                P.op("dve", lambda e, o2=o2, i2=i2: e.reduce_max(out=o2, in_=i2, axis=mybir.AxisListType.X),
                     [m2], [L2.all()])
                P.ts(EQ2.all(), L2.all(), m2, ALU.is_equal)
                dl = SM[:, 3:4]
                P.tt(dl, m2, m1, ALU.subtract)
                ex = SM[:, 4:5]
                P.act(ex, dl, AF.Exp)
                g1 = SM[:, 5:6]
                P.ts(g1, ex, 1.0, ALU.add)
                P.op("dve", lambda e, g1=g1: e.reciprocal(out=g1.ap, in_=g1.ap), [g1], [g1])
                g2 = SM[:, 6:7]
                P.tt(g2, ex, g1, ALU.mult)
                P.ts(EQ1.all(), EQ1.all(), g1, ALU.mult)
                P.stt(COMB[:, t, :], EQ2.all(), g2, EQ1.all(), ALU.mult, ALU.add)
            for e_i in range(n_exp):
                for t in range(9):
                    lb = SCR2[t % 2][:, 0:128]
                    P.ts(lb, ONES.all(), COMB[:, t, e_i:e_i + 1], ALU.mult)
                    pc = PS[7][:, (t % 3) * 128:(t % 3 + 1) * 128]
                    P.mm(pc, lb, IDENT.all(), start=True, stop=True, inc=True)
                    if t % 3 == 2:
                        blk = t // 3
                        P.copy(CB[:, blk * BLK:(blk + 1) * BLK], PS[7][:, 0:BLK])
                ffn_pass(HT, AT, d_mwg, d_mwu, d_mwd, e_i * D, e_i * FF, CB=CB)

        def moe_routed_stage(S0):
            NSC = S0 // 128
            NPASS = -(-NT // S0)
            NG = FC // 2
            cur[0] = HALOS.off
            HTOK = alloc([9, D], BF16)
            hg_off = cur[0]
            cur[0] += max(KC * S0 * 2, KC * BLK * 2)
            HG = Reg(arena, "sb", hg_off, [KC, S0], BF16)
            HTB = Reg(arena, "sb", hg_off, [KC, BLK], BF16)
            OBF = Reg(arena, "sb", hg_off, [NSC, D], BF16)
            OACC = alloc([NSC, D], F32)
            assert cur[0] <= ARENA_BYTES, cur[0]
            c5 = [ring_off + 5 * 8192]

            def a5(shape, dtype):
                esz = 2 if dtype == BF16 else 4
                nb = (_prod(shape) * esz + 3) // 4 * 4
                r = Reg(arena, "sb", c5[0], shape, dtype)
                c5[0] += nb
                assert c5[0] <= ring_off + 6 * 8192, c5[0]
                return r
            AT = [a5([2, S0], BF16) for _ in range(2)]
            COMB = a5([9, NE], F32)
            RR = a5([9, NE], F32)
            POSM = a5([9, NE], F32)
            HIF = a5([9, NE], F32)
            HIB = a5([9, NE], BF16)
            GHL = a5([9, NE, 2], BF16)
            CNT = a5([NE], F32)
            FLG = a5([32], F32)
            FLGI = a5([32], I32)
            PM = a5([12], F32)
            GS = a5([4], F32)
            GT = a5([4, 2], F32)
            IOTA_ROW = a5([S0], F32)
            IOTA_COL = a5([4], F32)
            UT = a5([128], F32)
            IDENTB = a5([128], BF16)
            LG = a5([NE], F32)
            L2 = a5([NE], F32)
            EQ1 = a5([NE], F32)
            EQ2 = a5([NE], F32)
            SM = a5([8], F32)
            GR = a5([KC, NE], F32)
            SEL = [Reg(arena, "sb", RSTD.off + i * 2304, [3, S0], BF16) for i in range(2)]
            SELT = [Reg(arena, "sb", RSTD.off + i * 2304, [NSC, BLK], BF16) for i in range(2)]

            P.dma("sp", IOTA_ROW.all(), d_iotas[:, 0:S0])
            P.dma("sp", IOTA_COL.all(), d_iotas[:, 512:516])
            P.dma("sp", UT.all(), d_ut)
            P.dma("sp", GR.all(), d_router.rearrange("(k p) e -> p k e", p=128))
            P.copy(IDENTB.all(), IDENT.all())
            for blk in range(NB):
                t0 = blk * BLK
                rms(lambda kc: XT[:, kc, t0:t0 + BLK], BLK, V_NF1,
                    lambda kc: HTB[:, kc, :], RSTD[:, t0:t0 + BLK])
                k = 0
                for tl in range(3):
                    t = 3 * blk + tl
                    for q4 in range(4):
                        ps = PS[k % 2]
                        for i in range(4):
                            kc = 4 * q4 + i
                            P.mm(ps[:, i * 128:(i + 1) * 128], HTB[:, kc, tl * 128:(tl + 1) * 128], IDENTB.all(),
                                 start=True, stop=True, inc=(i == 3))
                        P.copy(HTOK[:, t, q4 * 512:(q4 + 1) * 512], ps[:, 0:512])
                        k += 1
            for kc in range(KC):
                P.ts(GR[:, kc, :], GR[:, kc, :], vcol(V_NF1, kc), ALU.mult)
            for t in range(9):
                pl = PS[7][:, 0:NE]
                for kc in range(KC):
                    P.mm(pl, XT[:, kc, t * 128:(t + 1) * 128], GR[:, kc, :], start=(kc == 0),
                         stop=(kc == KC - 1), inc=(kc == KC - 1))
                pr = PS[6][:, 0:1]
                P.mm(pr, RSTD[0:1, t * 128:(t + 1) * 128], ONES[0:1, 0:1], start=True, stop=True, inc=True)
                rc = SM[:, 0:1]
                P.copy(rc, pr)
                P.ts(LG.all(), pl, rc, ALU.mult)
                m1 = SM[:, 1:2]
                m2 = SM[:, 2:3]
                o1, i1 = m1.ap, LG.all().ap
                P.op("dve", lambda e, o1=o1, i1=i1: e.reduce_max(out=o1, in_=i1, axis=mybir.AxisListType.X),
                     [m1], [LG.all()])
                P.ts(EQ1.all(), LG.all(), m1, ALU.is_equal)
                P.stt(L2.all(), EQ1.all(), -1e30, LG.all(), ALU.mult, ALU.add)
                o2, i2 = m2.ap, L2.all().ap
                P.op("dve", lambda e, o2=o2, i2=i2: e.reduce_max(out=o2, in_=i2, axis=mybir.AxisListType.X),
                     [m2], [L2.all()])
                P.ts(EQ2.all(), L2.all(), m2, ALU.is_equal)
                dl = SM[:, 3:4]
                P.tt(dl, m2, m1, ALU.subtract)
                ex = SM[:, 4:5]
                P.act(ex, dl, AF.Exp)
                g1 = SM[:, 5:6]
                P.ts(g1, ex, 1.0, ALU.add)
                P.op("dve", lambda e, g1=g1: e.reciprocal(out=g1.ap, in_=g1.ap), [g1], [g1])
                g2 = SM[:, 6:7]
                P.tt(g2, ex, g1, ALU.mult)
                P.ts(EQ1.all(), EQ1.all(), g1, ALU.mult)
                P.stt(COMB[:, t, :], EQ2.all(), g2, EQ1.all(), ALU.mult, ALU.add)
            P.ts(RR.all(), COMB.all(), 0.0, ALU.is_gt)
            for t in range(9):
                ps = PS[7][:, 0:NE]
                for t2 in range(t):
                    P.mm(ps, ONES.all(), RR[:, t2, :], start=(t2 == 0), stop=False)
                P.mm(ps, UT.all(), RR[:, t, :], start=(t == 0), stop=True, inc=True)
                P.stt(POSM[:, t, :], ps, 1.0, RR[:, t, :], ALU.add, ALU.mult)
                P.ts(POSM[:, t, :], POSM[:, t, :], -1.0, ALU.add)
            pc = PS[6][:, 0:NE]
            for t in range(9):
                P.mm(pc, ONES.all(), RR[:, t, :], start=(t == 0), stop=(t == 8), inc=(t == 8))
            P.copy(CNT.all(), pc)
            P.memset(FLG.all(), 0.0)
            for p in range(1, NPASS):
                P.ts(FLG[:, (p - 1) * NE:p * NE], CNT.all(), float(p * S0), ALU.is_gt)
            P.copy(FLGI.all(), FLG.all())
            P.copy(HIB.all(), COMB.all())
            P.copy(HIF.all(), HIB.all())
            P.copy(GHL[:, :, :, 0], HIB.all())
            P.tt(GHL[:, :, :, 1], COMB.all(), HIF.all(), ALU.subtract)

            mcount = [0]

            def expert_pass(e_i, p):
                P.ts(PM[:, 0:9], POSM[:, :, e_i], float(-p * S0), ALU.add)
                pgs = PS[6]
                for tb in range(3):
                    sel = SEL[tb % 2]
                    for tl in range(3):
                        t = 3 * tb + tl
                        P.ts(sel[:, tl, :], IOTA_ROW.all(), PM[:, t:t + 1], ALU.is_equal)
                    for kc in range(KC):
                        ps = PS[4 + kc % 2][:, 0:S0]
                        for tl in range(3):
                            t = 3 * tb + tl
                            P.mm(ps, HTOK[:, t, kc * 128:(kc + 1) * 128], sel[:, tl, :], start=(tl == 0),
                                 stop=(tl == 2), inc=(tl == 2))
                        if tb == 0:
                            P.copy(HG[:, kc, :], ps)
                        else:
                            P.tt(HG[:, kc, :], ps, HG[:, kc, :], ALU.add)
                    for sc in range(NSC):
                        for tl in range(3):
                            t = 3 * tb + tl
                            P.mm(pgs[:, 2 * sc:2 * sc + 2], sel[:, tl, sc * 128:(sc + 1) * 128], GHL[:, t, e_i, :],
                                 start=(tl == 0), stop=(tl == 2), inc=(tl == 2))
                    gt = V(GT.ap.rearrange("p a b -> p (a b)")[:, 0:2 * NSC], "sb", GT.off, GT.off + GT.nbytes)
                    if tb == 0:
                        P.copy(gt, pgs[:, 0:2 * NSC])
                    else:
                        P.tt(gt, pgs[:, 0:2 * NSC], gt, ALU.add)
                P.tt(GS[:, 0:NSC], GT[:, 0:NSC, 0], GT[:, 0:NSC, 1], ALU.add)
                seq = []
                for g in range(NG):
                    seq += [("g", g), ("u", g)]
                    if g >= 1:
                        seq.append(("d", g - 1))
                seq.append(("d", NG - 1))
                where = {it: i for i, it in enumerate(seq)}
                loaded = {}
                nxt = [0]

                def ensure(item):
                    while nxt[0] <= where[item]:
                        kind, g = seq[nxt[0]]
                        slot = mcount[0] % 5
                        mcount[0] += 1
                        if kind == "d":
                            r = ring_dn(slot)
                            src = d_mwd[e_i * FF + g * 256:e_i * FF + (g + 1) * 256, :].rearrange(
                                "(j p) c -> p j c", p=128)
                        else:
                            r = ring_gu(slot)
                            w_ap = d_mwg if kind == "g" else d_mwu
                            src = w_ap[e_i * D:(e_i + 1) * D, g * 256:(g + 1) * 256].rearrange(
                                "(k p) c -> p k c", p=128)
                        P.dma("pool", r.all(), src, sem="R%d" % slot)
                        loaded[(kind, g)] = r
                        nxt[0] += 1
                    return loaded[item]

                def gu_phase(g):
                    ug = ensure(("g", g))
                    uu = ensure(("u", g))
                    at = AT[g % 2]
                    for j in range(2):
                        pg = PS[j % 2][:, 0:S0]
                        pu = PS[2 + j % 2][:, 0:S0]
                        for kc in range(KC):
                            P.mm(pg, ug[:, kc, j * 128:(j + 1) * 128], HG[:, kc, :],
                                 start=(kc == 0), stop=(kc == KC - 1), inc=(kc == KC - 1))
                        for kc in range(KC):
                            P.mm(pu, uu[:, kc, j * 128:(j + 1) * 128], HG[:, kc, :],
                                 start=(kc == 0), stop=(kc == KC - 1), inc=(kc == KC - 1))
                        sg = SCR[j % 2][:, 0:S0]
                        P.act(sg, pg, AF.Silu)
                        P.tt(at[:, j, :], pu, sg, ALU.mult)

                def dn_phase(g):
                    ud = ensure(("d", g))
                    at = AT[g % 2]
                    k = 0
                    for sc in range(NSC):
                        for dq in range(4):
                            pd = PS[4 + k % 2][:, 0:512]
                            for j in range(2):
                                P.mm(pd, at[:, j, sc * 128:(sc + 1) * 128], ud[:, j, dq * 512:(dq + 1) * 512],
                                     start=(j == 0), stop=(j == 1), inc=(j == 1))
                            oa = OACC[:, sc, dq * 512:(dq + 1) * 512]
                            if g == 0:
                                P.copy(oa, pd)
                            else:
                                P.tt(oa, pd, oa, ALU.add)
                            k += 1

                gu_phase(0)
                for g in range(1, NG):
                    gu_phase(g)
                    dn_phase(g - 1)
                dn_phase(NG - 1)
                for sc in range(NSC):
                    P.ts(OBF[:, sc, :], OACC[:, sc, :], GS[:, sc:sc + 1], ALU.mult)
                k = 0
                for blk in range(NB):
                    for tl in range(3):
                        t = 3 * blk + tl
                        lb = SCR2[tl % 2][:, 0:128]
                        P.ts(lb, ONES.all(), PM[:, t:t + 1], ALU.mult)
                        P.mm(PS[7][:, tl * 128:(tl + 1) * 128], lb, IDENT.all(), start=True, stop=True, inc=True)
                    prow = SCR[blk % 2][:, 0:BLK]
                    P.copy(prow, PS[7][:, 0:BLK])
                    selt = SELT[blk % 2]
                    for sc in range(NSC):
                        P.ts(selt[:, sc, :], prow, IOTA_COL[:, sc:sc + 1], ALU.is_equal)
                    for co in range(KC):
                        ps = PS[k % 2][:, 0:BLK]
                        for sc in range(NSC):
                            P.mm(ps, OBF[:, sc, co * 128:(co + 1) * 128], selt[:, sc, :], start=(sc == 0),
                                 stop=(sc == NSC - 1), inc=(sc == NSC - 1))
                        xs = XT[:, co, blk * BLK:(blk + 1) * BLK]
                        P.tt(xs, ps, xs, ALU.add)
                        k += 1

            npass_emit = int(os.environ.get("MK_NPASS", str(NPASS)))
            for e_i in range(n_exp):
                for p in range(min(NPASS, npass_emit)):
                    if p > 0:
                        fi = (p - 1) * NE + e_i
                        P.region_begin(["pe", "act", "dve", "pool"], FLGI[0:1, fi:fi + 1])
                    expert_pass(e_i, p)
                    if p > 0:
                        P.region_end()

        def final_stage():
            cur[0] = stage_off
            YO = [alloc([KC, BLK], F32) for _ in range(2)]
            for blk in range(NB):
                t0 = blk * BLK
                yo = YO[blk % 2]
                rms(lambda kc: XT[:, kc, t0:t0 + BLK], BLK, V_NFIN,
                    lambda kc: yo[:, kc, :], RSTD[:, t0:t0 + BLK])
                for q4 in range(4):
                    P.dma("sp", o_yT[512 * q4:512 * (q4 + 1), t0:t0 + BLK].rearrange("(k p) t -> p k t", p=128),
                          yo[:, 4 * q4:4 * q4 + 4, :], is_output=True)

        if "conv" in stages:
            conv_stage()
        if "ffn" in stages:
            ffn_stage()
        if "gmlp" in stages:
            gmlp_stage()
        if "moe" in stages:
            if os.environ.get("MK_MOE", "routed") == "dense":
                moe_stage()
            else:
                moe_routed_stage(int(os.environ.get("MK_S0", "384")))
        final_stage()
        P.finish()

        with nc.Block() as block:
            @block.tensor
            def _(e):
                replay(e, P.q["pe"])

            @block.scalar
            def _(e):
                replay(e, P.q["act"])

            @block.vector
            def _(e):
                replay(e, P.q["dve"])

            @block.gpsimd
            def _(e):
                replay(e, P.q["pool"])

            @block.sync
            def _(e):
                replay(e, P.q["sp"])
        stats = {k: len(v) for k, v in P.q.items()}
        stats["waits"] = P.n_wait
    return nc, stats


def _pack_cols(vec):
    return np.ascontiguousarray(np.asarray(vec, np.float32).reshape(KC, 128).T)


def kernel(x_prompt, x_sample, state_conv, norm_mix, norm_ffn, norm_final,
           conv_w_in, conv_b_in, conv_w_dw, conv_b_dw, conv_ln_g, conv_ln_b, conv_w_out, conv_b_out,
           gmlp_w_in, gmlp_b_in, gmlp_ln_g, gmlp_ln_b, gmlp_w_s, gmlp_b_s, gmlp_w_out, gmlp_b_out,
           ffn_w_gate, ffn_w_up, ffn_w_down, moe_router, moe_w_gate, moe_w_up, moe_w_down):
    stages = os.environ.get("MK_STAGES", "conv,ffn,gmlp,moe").split(",")
    n_exp = int(os.environ.get("MK_NEXP", str(NE)))
    f = np.float32
    x_prompt = np.asarray(x_prompt, f)
    x_sample = np.asarray(x_sample, f)
    state_conv = np.asarray(state_conv, f)

    cols = [norm_mix[0], norm_mix[1], norm_ffn[0], norm_ffn[1], norm_final,
            conv_b_in[0, :D], conv_b_in[0, D:], conv_b_dw[0], conv_ln_g[0], conv_ln_b[0], conv_b_out[0],
            gmlp_b_in[0, :D], gmlp_b_out[0]]
    cols += [conv_w_dw[0, j] for j in range(NTAP)]
    vecs = np.ascontiguousarray(np.concatenate([_pack_cols(c) for c in cols], axis=1))
    assert vecs.shape == (128, NV)
    rows = np.zeros((1, NR), f)
    rows[0, R_GBV:R_GBV + D] = np.asarray(gmlp_b_in, f)[0, D:]
    rows[0, R_LNG:R_LNG + D] = np.asarray(gmlp_ln_g, f)[0]
    rows[0, R_LNB:R_LNB + D] = np.asarray(gmlp_ln_b, f)[0]
    bs = np.asarray(gmlp_b_s, f)[0]
    rows[0, R_BSP:R_BSP + 1024] = bs.reshape(-1)
    rows[0, R_BSS:R_BSS + 1024] = np.tile(bs[:, :DSEQ], (1, NSEQ)).reshape(-1)
    ws = np.asarray(gmlp_w_s, f)[0]
    wsp = np.ascontiguousarray(ws.transpose(2, 0, 1)).reshape(128, 1024)
    ws8 = ws[:, :DSEQ, :DSEQ].transpose(2, 0, 1)
    wss = np.ascontiguousarray(np.tile(ws8, (NSEQ, 1, NSEQ))).reshape(128, 1024)
    ii = np.arange(128)
    mtril = (ii[:, None] <= ii[None, :]).astype(f)
    mbd = mtril * ((ii[:, None] // DSEQ) == (ii[None, :] // DSEQ)).astype(f)
    masks = np.ascontiguousarray(np.concatenate([mtril, mbd], axis=1))
    ident = np.eye(128, dtype=f)
    iotas = np.zeros((128, 516), f)
    iotas[:, :512] = np.arange(512, dtype=f)[None, :]
    iotas[:, 512:516] = ii[:, None].astype(f) + 128.0 * np.arange(4, dtype=f)[None, :]
    ut = (ii[:, None] < ii[None, :]).astype(f)
    shared = {
        "iotas": iotas, "ut": ut,
        "vecs": vecs, "rows": rows, "wsp": wsp, "wss": wss, "masks": masks, "ident": ident,
        "router": np.ascontiguousarray(np.asarray(moe_router, f)[0]),
        "conv_w_in": np.asarray(conv_w_in, f)[0], "conv_w_out": np.asarray(conv_w_out, f)[0],
        "gmlp_w_in": np.asarray(gmlp_w_in, f)[0], "gmlp_w_out": np.asarray(gmlp_w_out, f)[0],
        "ffn_wg": np.asarray(ffn_w_gate, f)[0], "ffn_wu": np.asarray(ffn_w_up, f)[0],
        "ffn_wd": np.asarray(ffn_w_down, f)[0],
        "moe_wg": np.asarray(moe_w_gate, f).reshape(NE * D, FF),
        "moe_wu": np.asarray(moe_w_up, f).reshape(NE * D, FF),
        "moe_wd": np.asarray(moe_w_down, f).reshape(NE * FF, D),
    }
    in_maps = []
    for c in range(NCORES):
        b, half = c // 2, c % 2
        xp = x_prompt[b, half * 1024:(half + 1) * 1024]
        xs = x_sample[c * NSEQ:(c + 1) * NSEQ].reshape(NSEQ * DSEQ, D)
        xT = np.ascontiguousarray(np.concatenate([xp, xs], axis=0).T)
        if half == 1:
            xh = np.ascontiguousarray(x_prompt[b, 1024 - HALO:1024].T)
        else:
            xh = np.zeros((D, HALO), f)
        hmask = np.full((128, 1), float(half), f)
        sc = state_conv[0, c * NSEQ:(c + 1) * NSEQ]
        sconv = np.ascontiguousarray(sc.transpose(2, 0, 1)).reshape(D, NSEQ * HALO)
        m = {"xT": xT, "xh": xh, "hmask": hmask, "sconv": sconv}
        m.update(shared)
        in_maps.append(m)

    nc, _ = build_program(stages, n_exp)
    res = run_bass_kernel_spmd(nc, in_maps, core_ids=list(range(NCORES)))
    outs = res.results

    y_prompt = np.empty((4, 2048, D), f)
    y_sample = np.empty((128, DSEQ, D), f)
    ncp = np.empty((1, 4, HALO, D), f)
    ncs = np.empty((1, 128, HALO, D), f)
    vout = np.empty((1, 128, DSEQ, D), f)
    for c in range(NCORES):
        b, half = c // 2, c % 2
        yT = np.asarray(outs[c]["yT"])
        y_prompt[b, half * 1024:(half + 1) * 1024] = yT[:, :1024].T
        y_sample[c * NSEQ:(c + 1) * NSEQ] = yT[:, 1024:].T.reshape(NSEQ, DSEQ, D)
        if half == 1:
            ncp[0, b] = np.asarray(outs[c]["ncp"]).T
        ncs[0, c * NSEQ:(c + 1) * NSEQ] = np.asarray(outs[c]["ncs"]).reshape(D, NSEQ, HALO).transpose(1, 2, 0)
        vout[0, c * NSEQ:(c + 1) * NSEQ] = np.asarray(outs[c]["vout"]).reshape(NSEQ, DSEQ, D)
    return (y_prompt, y_sample, ncp, ncs, vout)
```

```python
import os
import numpy as np
import concourse.bass as bass
import concourse.mybir as mybir
from concourse.bass_utils import run_bass_kernel_spmd

F32 = mybir.dt.float32
BF16 = mybir.dt.bfloat16
ALU = mybir.AluOpType
AF = mybir.ActivationFunctionType

NCORES = 8
D = 2048
KC = 16
NT = 1152
BLK = 384
NB = 3
FF = 7168
FC = 56
NE = 8
HALO = 30
NTAP = 31
NSEQ = 16
DSEQ = 8
EPS_RMS = 1e-6
EPS_LN = 1e-5

V_NM0, V_NM1, V_NF0, V_NF1, V_NFIN, V_CBA, V_CBG, V_CBDW, V_CLNG, V_CLNB, V_CBOUT, V_GBU, V_GBOUT = [
    16 * i for i in range(13)]
V_CWDW = 16 * 13
NV = 16 * 13 + NTAP * 16
R_GBV, R_LNG, R_LNB, R_BSP, R_BSS = 0, 2048, 4096, 6144, 7168
NR = 8192

ARENA_BYTES = 212800
I32 = mybir.dt.int32


def _prod(s):
    r = 1
    for x in s:
        r *= x
    return r


class V:
    __slots__ = ("ap", "mem", "lo", "hi")

    def __init__(self, ap, mem, lo, hi):
        self.ap, self.mem, self.lo, self.hi = ap, mem, lo, hi


class Reg:
    def __init__(self, base_ap, mem, off, shape, dtype):
        self.mem = mem
        self.off = off
        self.shape = tuple(shape)
        self.esz = 2 if dtype == BF16 else 4
        n = _prod(shape)
        self.nbytes = n * self.esz
        assert off % 4 == 0 and self.nbytes % 4 == 0
        ap = base_ap[:, off // 4:(off + self.nbytes) // 4]
        if dtype != F32:
            ap = ap.bitcast(dtype)
        if len(shape) == 2:
            ap = ap.rearrange("p (a b) -> p a b", a=shape[0])
        elif len(shape) == 3:
            ap = ap.rearrange("p (a b c) -> p a b c", a=shape[0], b=shape[1])
        self.ap = ap
        self.strides = [_prod(shape[i + 1:]) for i in range(len(shape))]

    def __getitem__(self, idx):
        if not isinstance(idx, tuple):
            idx = (idx,)
        ap = self.ap[idx]
        lo = 0
        hi = 0
        for d, (sz, st) in enumerate(zip(self.shape, self.strides)):
            ix = idx[d + 1] if d + 1 < len(idx) else slice(None)
            if isinstance(ix, int):
                a, b = ix, ix + 1
            else:
                a = ix.start or 0
                b = sz if ix.stop is None else ix.stop
            lo += a * st
            hi += (b - 1) * st
        hi += 1
        return V(ap, self.mem, self.off + lo * self.esz, self.off + hi * self.esz)

    def all(self):
        return self[(slice(None),)]


class Prog:
    ENGS = ("pe", "act", "dve", "pool", "sp")

    def __init__(self, nc, sems):
        self.nc = nc
        self.sems = sems
        self.q = {e: [] for e in self.ENGS}
        self.cnt = {"pe": 0, "act": 0, "dve": 0}
        self.dcnt = {}
        self.known = {e: {} for e in self.ENGS}
        self.pe_idx = 0
        self.pe_miles_idx = []
        self.pe_miles_cnt = []
        self.recs = {"sb": [], "ps": []}
        self.out_events = {}
        self.sp_rr = 0
        self.n_wait = 0

    def _resolve(self, key, val):
        if key == "PE#":
            import bisect
            i = bisect.bisect_left(self.pe_miles_idx, val)
            if i >= len(self.pe_miles_idx):
                raise RuntimeError("PE read/write without a later milestone (idx %d)" % val)
            return "pe", self.pe_miles_cnt[i]
        return key, val

    def wait(self, eng, key, val):
        if key == "PE#" and eng == "pe":
            return
        key, val = self._resolve(key, val)
        if key == "pe" and eng == "pe":
            return
        if self.known[eng].get(key, 0) >= val:
            return
        self.known[eng][key] = val
        h = self.sems[key]
        self.q[eng].append(lambda e, h=h, v=val: e.wait_ge(h, v))
        self.n_wait += 1

    def _deps(self, eng, outs, ins):
        need = {}

        def add(evd):
            for k, v in evd.items():
                if need.get(k, -1) < v:
                    need[k] = v
        for v in ins:
            for r in self.recs[v.mem]:
                if r[0] < v.hi and v.lo < r[1]:
                    add(r[2])
        for v in outs:
            for r in self.recs[v.mem]:
                if r[0] < v.hi and v.lo < r[1]:
                    add(r[2])
                    add(r[3])
        for k, val in need.items():
            self.wait(eng, k, val)

    def _record(self, outs, ins, key, val):
        for v in ins:
            hit = False
            for r in self.recs[v.mem]:
                if r[0] < v.hi and v.lo < r[1]:
                    if r[3].get(key, -1) < val:
                        r[3][key] = val
                    if r[0] <= v.lo and v.hi <= r[1]:
                        hit = True
            if not hit:
                self.recs[v.mem].append([v.lo, v.hi, {}, {key: val}])
        for v in outs:
            lst = self.recs[v.mem]
            lst[:] = [r for r in lst if not (v.lo <= r[0] and r[1] <= v.hi)]
            lst.append([v.lo, v.hi, {key: val}, {}])

    def op(self, eng, fn, outs, ins):
        self._deps(eng, outs, ins)
        self.cnt[eng] += 1
        val = self.cnt[eng]
        h = self.sems[eng]
        self.q[eng].append(lambda e, fn=fn, h=h: fn(e).then_inc(h, 1))
        self.known[eng][eng] = max(self.known[eng].get(eng, 0), 0)
        self._record(outs, ins, eng, val)

    def mm(self, out, lhsT, rhs, start, stop, inc=False):
        self._deps("pe", [out], [lhsT, rhs])
        idx = self.pe_idx
        self.pe_idx += 1
        o, l, r = out.ap, lhsT.ap, rhs.ap
        if inc:
            self.cnt["pe"] += 1
            self.pe_miles_idx.append(idx)
            self.pe_miles_cnt.append(self.cnt["pe"])
            h = self.sems["pe"]
            self.q["pe"].append(lambda e, o=o, l=l, r=r, s=start, t=stop, h=h:
                                e.matmul(o, l, r, start=s, stop=t).then_inc(h, 1))
        else:
            self.q["pe"].append(lambda e, o=o, l=l, r=r, s=start, t=stop:
                                e.matmul(o, l, r, start=s, stop=t))
        self._record([out], [lhsT, rhs], "PE#", idx)

    def dma(self, queue, out, in_, sem=None, is_output=False):
        outs = [out] if isinstance(out, V) else []
        ins = [in_] if isinstance(in_, V) else []
        if sem is None:
            sem = "S%d" % (self.sp_rr % 8)
            self.sp_rr += 1
        prev = self.dcnt.get(sem, 0)
        if prev:
            self.wait(queue, sem, prev)
        self._deps(queue, outs, ins)
        val = prev + 16
        self.dcnt[sem] = val
        h = self.sems[sem]
        oa = out.ap if isinstance(out, V) else out
        ia = in_.ap if isinstance(in_, V) else in_
        self.q[queue].append(lambda e, oa=oa, ia=ia, h=h: e.dma_start(out=oa, in_=ia).then_inc(h, 16))
        self._record(outs, ins, sem, val)
        if is_output:
            self.out_events[sem] = val

    def act(self, out, in_, func, bias=None, scale=None):
        ins = [in_]
        kw = {}
        if bias is not None:
            if isinstance(bias, V):
                ins.append(bias)
                kw["bias"] = bias.ap
            else:
                kw["bias"] = bias
        if scale is not None:
            if isinstance(scale, V):
                ins.append(scale)
                kw["scale"] = scale.ap
            else:
                kw["scale"] = scale
        o, i = out.ap, in_.ap
        self.op("act", lambda e: e.activation(out=o, in_=i, func=func, **kw), [out], ins)

    def tt(self, out, in0, in1, op):
        o, a, b = out.ap, in0.ap, in1.ap
        self.op("dve", lambda e: e.tensor_tensor(out=o, in0=a, in1=b, op=op), [out], [in0, in1])

    def ts(self, out, in0, s1, op0, s2=None, op1=None):
        ins = [in0]
        a1 = s1
        if isinstance(s1, V):
            ins.append(s1)
            a1 = s1.ap
        a2 = s2
        if isinstance(s2, V):
            ins.append(s2)
            a2 = s2.ap
        o, a = out.ap, in0.ap
        if op1 is None:
            self.op("dve", lambda e: e.tensor_single_scalar(out=o, in_=a, scalar=a1, op=op0), [out], ins)
        else:
            self.op("dve", lambda e: e.tensor_scalar(out=o, in0=a, scalar1=a1, scalar2=a2, op0=op0, op1=op1),
                    [out], ins)

    def stt(self, out, in0, scalar, in1, op0, op1):
        ins = [in0, in1]
        sc = scalar
        if isinstance(scalar, V):
            ins.append(scalar)
            sc = scalar.ap
        o, a, b = out.ap, in0.ap, in1.ap
        self.op("dve", lambda e: e.scalar_tensor_tensor(out=o, in0=a, scalar=sc, in1=b, op0=op0, op1=op1),
                [out], ins)

    def copy(self, out, in_):
        o, a = out.ap, in_.ap
        self.op("dve", lambda e: e.tensor_copy(out=o, in_=a), [out], [in_])

    def memset(self, out, val):
        o = out.ap
        self.op("dve", lambda e: e.memset(o, val), [out], [])

    def finish(self):
        for sem, val in self.out_events.items():
            self.wait("sp", sem, val)

    def region_begin(self, engines, flag_view):
        self._region = dict(engines=engines, known={e: dict(self.known[e]) for e in engines},
                            cnt0=dict(self.cnt), dcnt0=dict(self.dcnt))
        for e in engines:
            self._deps(e, [], [flag_view])
            self.q[e].append(("if", flag_view.ap))

    def region_end(self):
        r = self._region
        self._region = None
        for e in r["engines"]:
            comp = []
            if e in self.cnt:
                m0, m1 = r["cnt0"][e], self.cnt[e]
                if m1 > m0:
                    comp.append((self.sems[e], m0, m1 - m0))
            if e == "pool":
                for sname, v1 in self.dcnt.items():
                    v0 = r["dcnt0"].get(sname, 0)
                    if sname.startswith("R") and v1 > v0:
                        comp.append((self.sems[sname], v0, v1 - v0))
            self.q[e].append(("else", comp))
            self.q[e].append(("endif",))
            self.known[e] = r["known"][e]


def replay(e, items):
    stack = []
    rguard = e.register("flag")
    reg = rguard.__enter__()
    for it in items:
        if isinstance(it, tuple):
            if it[0] == "if":
                e.reg_load(reg, it[1])
                g = e.If_ne(reg, 0)
                g.__enter__()
                stack.append(g)
            elif it[0] == "else":
                stack.pop().__exit__(None, None, None)
                g = e.Else()
                g.__enter__()
                stack.append(g)
                for semh, v0, n in it[1]:
                    if v0 > 0:
                        e.wait_ge(semh, v0)
                    e.sem_inc(semh, n)
            else:
                stack.pop().__exit__(None, None, None)
        else:
            it(e)
    rguard.__exit__(None, None, None)


def build_program(stages, n_exp):
    nc = bass.Bass("TRN2", target_bir_lowering=False)

    def din(name, shape):
        return nc.dram_tensor(name, list(shape), F32, kind="ExternalInput").ap()

    def dout(name, shape):
        return nc.dram_tensor(name, list(shape), F32, kind="ExternalOutput").ap()

    d_xT = din("xT", [D, NT])
    d_xh = din("xh", [D, HALO])
    d_hmask = din("hmask", [128, 1])
    d_sconv = din("sconv", [D, NSEQ * HALO])
    d_vecs = din("vecs", [128, NV])
    d_rows = din("rows", [1, NR])
    d_wsp = din("wsp", [128, 8 * 128])
    d_wss = din("wss", [128, 8 * 128])
    d_masks = din("masks", [128, 2 * 128])
    d_ident = din("ident", [128, 128])
    d_router = din("router", [D, NE])
    d_iotas = din("iotas", [128, 512 + 4])
    d_ut = din("ut", [128, 128])
    d_cwin = din("conv_w_in", [D, 2 * D])
    d_cwout = din("conv_w_out", [D, D])
    d_gwin = din("gmlp_w_in", [D, 2 * D])
    d_gwout = din("gmlp_w_out", [D, D])
    d_fwg = din("ffn_wg", [D, FF])
    d_fwu = din("ffn_wu", [D, FF])
    d_fwd = din("ffn_wd", [FF, D])
    d_mwg = din("moe_wg", [NE * D, FF])
    d_mwu = din("moe_wu", [NE * D, FF])
    d_mwd = din("moe_wd", [NE * FF, D])

    o_yT = dout("yT", [D, NT])
    o_ncp = dout("ncp", [D, HALO])
    o_ncs = dout("ncs", [D, NSEQ * HALO])
    o_vout = dout("vout", [128, D])

    sem_names = ["pe", "act", "dve"] + ["R%d" % i for i in range(6)] + ["S%d" % i for i in range(8)]

    import contextlib
    with contextlib.ExitStack() as es:
        arena = es.enter_context(nc.sbuf_tensor("arena", [128, ARENA_BYTES // 4], F32))
        psum = es.enter_context(nc.psum_tensor("psum", [128, 8 * 512], F32))
        sems = {n: es.enter_context(nc.semaphore("sem_" + n)) for n in sem_names}
        P = Prog(nc, sems)

        cur = [0]

        def alloc(shape, dtype, at=None):
            esz = 4 if dtype == F32 else 2
            nb = (_prod(shape) * esz + 3) // 4 * 4
            if at is None:
                off = cur[0]
                cur[0] += nb
            else:
                off = at
            assert off + nb <= ARENA_BYTES, (off, nb)
            return Reg(arena, "sb", off, shape, dtype)

        PS = [Reg(psum, "ps", b * 2048, [512], F32) for b in range(8)]

        XT = alloc([KC, NT], F32)
        ring_off = cur[0]
        cur[0] += 6 * 8192
        VECS = alloc([NV], F32)
        RSTD = alloc([NT], F32)
        SCR = [alloc([416], F32) for _ in range(2)]
        SCR2 = [alloc([416], F32) for _ in range(2)]
        ONES = alloc([128], F32)
        IDENT = alloc([128], F32)
        MISC = alloc([64], F32)
        HALOS = alloc([KC, HALO], F32)
        stage_off = cur[0]
        STAGE_BYTES = ARENA_BYTES - stage_off

        def ring_gu(slot):
            return Reg(arena, "sb", ring_off + slot * 8192, [KC, 256], BF16)

        def ring_dn(slot):
            return Reg(arena, "sb", ring_off + slot * 8192, [2, D], BF16)

        HMASK = MISC[:, 0:1]
        EPSR = MISC[:, 1:2]
        EPSL = MISC[:, 2:3]

        def vcol(base, kc):
            return VECS[:, base + kc:base + kc + 1]

        for q4 in range(4):
            P.dma("sp", XT[:, 4 * q4:4 * q4 + 4, :],
                  d_xT[512 * q4:512 * (q4 + 1), :].rearrange("(k p) t -> p k t", p=128))
        P.dma("sp", VECS.all(), d_vecs)
        P.dma("sp", IDENT.all(), d_ident)
        P.dma("sp", MISC[:, 0:1], d_hmask)
        P.memset(ONES.all(), 1.0)
        P.memset(MISC[:, 1:2], EPS_RMS)
        P.memset(MISC[:, 2:3], EPS_LN)

        wcount = [0]

        def load_gu(w_ap, row0, col0, nslots, slot_base=0):
            slot = slot_base + wcount[0] % nslots
            wcount[0] += 1
            r = ring_gu(slot)
            src = w_ap[row0:row0 + D, col0:col0 + 256].rearrange("(k p) c -> p k c", p=128)
            P.dma("pool", r.all(), src, sem="R%d" % slot)
            return r

        def load_dn(w_ap, row0, nslots):
            slot = wcount[0] % nslots
            wcount[0] += 1
            r = ring_dn(slot)
            src = w_ap[row0:row0 + 256, :].rearrange("(j p) c -> p j c", p=128)
            P.dma("pool", r.all(), src, sem="R%d" % slot)
            return r

        def rms(xv, n, gbase, hv, rstd, out_f32=False):
            ps = PS[6][:, 0:n]
            for kc in range(KC):
                s = SCR[kc % 2][:, 0:n]
                P.act(s, xv(kc), AF.Square)
                P.mm(ps, ONES.all(), s, start=(kc == 0), stop=(kc == KC - 1), inc=True)
            P.act(rstd, ps, AF.Sqrt, bias=EPSR, scale=1.0 / D)
            o, a = rstd.ap, rstd.ap
            P.op("dve", lambda e: e.reciprocal(out=o, in_=a), [rstd], [rstd])
            for kc in range(KC):
                P.stt(hv(kc), xv(kc), vcol(gbase, kc), rstd, ALU.mult, ALU.mult)

        def out_proj(w_ap, sin, n, t0, bbase, nslots):
            unit = None
            for co in range(KC):
                if co % 2 == 0:
                    unit = load_gu(w_ap, 0, (co // 2) * 256, nslots)
                j = co % 2
                po = PS[4 + co % 2][:, 0:n]
                for kc in range(KC):
                    P.mm(po, unit[:, kc, j * 128:(j + 1) * 128], sin(kc), start=(kc == 0),
                         stop=(kc == KC - 1), inc=(kc == KC - 1))
                xs = XT[:, co, t0:t0 + n]
                P.stt(xs, po, vcol(bbase, co), xs, ALU.add, ALU.add)

        def conv_stage():
            cur[0] = stage_off
            conv_pe = os.environ.get("MK_CONVPE", "1") == "1"
            HP = alloc([KC, 416], BF16)
            Y = alloc([KC, BLK], F32)
            ST = alloc([KC, BLK], BF16)
            GLU = [alloc([416], F32) for _ in range(2)]
            SCB = [alloc([NSEQ, HALO + DSEQ], F32) for _ in range(2)]
            XH = alloc([KC, 32], F32)
            MEAN = alloc([BLK], F32)
            RS = alloc([BLK], F32)
            MSQ = alloc([BLK], F32)
            RH = alloc([32], F32)
            P.dma("sp", XH[:, :, 0:HALO], d_xh.rearrange("(k p) t -> p k t", p=128))
            GLUB = [alloc([416], BF16) for _ in range(2)]
            DG = [Reg(arena, "sb", ring_off + (4 + i) * 8192, [NTAP, 128], BF16) for i in range(2)]
            ident_b = V(IDENT.ap[:, None, :].broadcast_to([128, NTAP, 128]), "sb", IDENT.off,
                        IDENT.off + IDENT.nbytes)

            def conv_taps(c, nprompt):
                dg, glub = DG[c % 2], GLUB[c % 2]
                pc = PS[4 + c % 2][:, 0:nprompt]
                for j2 in range(NTAP):
                    P.mm(pc, dg[:, j2, :], glub[:, j2:j2 + nprompt], start=(j2 == 0), stop=(j2 == NTAP - 1),
                         inc=(j2 == NTAP - 1))
                P.ts(Y[:, c, 0:nprompt], pc, vcol(V_CBDW, c), ALU.add)

            for b in range(NB):
                t0 = b * BLK
                c0 = 0 if b == 0 else HALO
                nprompt = BLK if b < 2 else 256
                rms(lambda kc: XT[:, kc, t0:t0 + BLK], BLK, V_NM0,
                    lambda kc: HP[:, kc, HALO:HALO + BLK], RSTD[:, t0:t0 + BLK])
                if b == 0:
                    rms(lambda kc: XH[:, kc, 0:HALO], HALO, V_NM0,
                        lambda kc: HP[:, kc, 0:HALO], RH[:, 0:HALO])
                ua = ug = None
                for c in range(KC):
                    if c % 2 == 0:
                        ua = load_gu(d_cwin, 0, (c // 2) * 256, 4)
                        ug = load_gu(d_cwin, 0, D + (c // 2) * 256, 4)
                    j = c % 2
                    n = HALO + BLK - c0
                    pa = PS[c % 2][:, c0:c0 + n]
                    pg = PS[2 + c % 2][:, c0:c0 + n]
                    for kc in range(KC):
                        P.mm(pa, ua[:, kc, j * 128:(j + 1) * 128], HP[:, kc, c0:c0 + n],
                             start=(kc == 0), stop=(kc == KC - 1), inc=(kc == KC - 1))
                    for kc in range(KC):
                        P.mm(pg, ug[:, kc, j * 128:(j + 1) * 128], HP[:, kc, c0:c0 + n],
                             start=(kc == 0), stop=(kc == KC - 1), inc=(kc == KC - 1))
                    sg = SCR[c % 2][:, c0:c0 + n]
                    P.act(sg, pg, AF.Sigmoid, bias=vcol(V_CBG, c))
                    glu = GLU[c % 2]
                    P.stt(glu[:, c0:c0 + n], pa, vcol(V_CBA, c), sg, ALU.add, ALU.mult)
                    if b == 0:
                        P.ts(glu[:, 0:HALO], glu[:, 0:HALO], HMASK, ALU.mult)
                    else:
                        P.copy(glu[:, 0:HALO], HALOS[:, c, :])
                    if conv_pe:
                        P.copy(GLUB[c % 2][:, 0:nprompt + HALO], glu[:, 0:nprompt + HALO])
                        wt = VECS.ap[:, V_CWDW:V_CWDW + 16 * NTAP].rearrange("p (j c) -> p j c", c=16)[:, :, c]
                        wt_b = V(wt[:, :, None].broadcast_to([128, NTAP, 128]), "sb", VECS.off + V_CWDW * 4,
                                 VECS.off + (V_CWDW + 16 * NTAP) * 4)
                        P.tt(DG[c % 2].all(), ident_b, wt_b, ALU.mult)
                    else:
                        acc = Y[:, c, 0:nprompt]
                        P.ts(acc, glu[:, 0:nprompt], vcol(V_CWDW + 0, c), ALU.mult, vcol(V_CBDW, c), ALU.add)
                        for j2 in range(1, NTAP):
                            P.stt(acc, glu[:, j2:j2 + nprompt], vcol(V_CWDW + 16 * j2, c), acc, ALU.mult, ALU.add)
                    P.copy(HALOS[:, c, :], glu[:, nprompt:nprompt + HALO])
                    if b == 2:
                        scb = SCB[c % 2]
                        P.dma("sp", scb[:, :, 0:HALO],
                              d_sconv[c * 128:(c + 1) * 128, :].rearrange("p (s r) -> p s r", s=NSEQ))
                        gs = V(glu.ap[:, HALO + 256:HALO + 384].rearrange("p (s r) -> p s r", s=NSEQ), "sb",
                               glu.off + (HALO + 256) * 4, glu.off + (HALO + 384) * 4)
                        P.copy(scb[:, :, HALO:HALO + DSEQ], gs)
                        accs = V(Y.ap[:, c, 256:384].rearrange("p (s r) -> p s r", s=NSEQ), "sb",
                                 Y.off + (c * BLK + 256) * 4, Y.off + (c * BLK + 384) * 4)
                        P.ts(accs, scb[:, :, 0:DSEQ], vcol(V_CWDW + 0, c), ALU.mult, vcol(V_CBDW, c), ALU.add)
                        for j2 in range(1, NTAP):
                            P.stt(accs, scb[:, :, j2:j2 + DSEQ], vcol(V_CWDW + 16 * j2, c), accs,
                                  ALU.mult, ALU.add)
                        P.dma("sp", o_ncs[c * 128:(c + 1) * 128, :].rearrange("p (s r) -> p s r", s=NSEQ),
                              scb[:, :, DSEQ:DSEQ + HALO], is_output=True)
                    if conv_pe and c >= 1:
                        conv_taps(c - 1, nprompt)
                if conv_pe:
                    conv_taps(KC - 1, nprompt)
                if b == 2:
                    P.dma("sp", o_ncp.rearrange("(k p) r -> p k r", p=128), HALOS.all(), is_output=True)
                s1 = PS[6][:, 0:BLK]
                s2 = PS[7][:, 0:BLK]
                for c in range(KC):
                    P.mm(s1, ONES.all(), Y[:, c, :], start=(c == 0), stop=(c == KC - 1), inc=True)
                    sq = SCR[c % 2][:, 0:BLK]
                    P.act(sq, Y[:, c, :], AF.Square)
                    P.mm(s2, ONES.all(), sq, start=(c == 0), stop=(c == KC - 1), inc=True)
                P.ts(MEAN.all(), s1, 1.0 / D, ALU.mult)
                P.tt(MSQ.all(), MEAN.all(), MEAN.all(), ALU.mult)
                P.stt(MSQ.all(), s2, 1.0 / D, MSQ.all(), ALU.mult, ALU.subtract)
                P.act(RS.all(), MSQ.all(), AF.Sqrt, bias=EPSL, scale=1.0)
                o, a = RS.all().ap, RS.all().ap
                P.op("dve", lambda e: e.reciprocal(out=o, in_=a), [RS.all()], [RS.all()])
                for c in range(KC):
                    P.tt(Y[:, c, :], Y[:, c, :], MEAN.all(), ALU.subtract)
                    P.tt(Y[:, c, :], Y[:, c, :], RS.all(), ALU.mult)
                    P.act(ST[:, c, :], Y[:, c, :], AF.Silu, bias=vcol(V_CLNB, c), scale=vcol(V_CLNG, c))
                out_proj(d_cwout, lambda kc: ST[:, kc, :], BLK, t0, V_CBOUT, 4)

        def ffn_pass(HT, AT, wg, wu, wd, grow0, drow0, CB=None):
            NG = FC // 2
            units = {}

            def gu_phase(g):
                ug = load_gu(wg, grow0, g * 256, 6)
                uu = load_gu(wu, grow0, g * 256, 6)
                units[g] = load_dn(wd, drow0 + g * 256, 6)
                at = AT[g % 2]
                k = 0
                for j in range(2):
                    for blk in range(NB):
                        pg = PS[k % 2][:, 0:BLK]
                        pu = PS[2 + k % 2][:, 0:BLK]
                        for kc in range(KC):
                            P.mm(pg, ug[:, kc, j * 128:(j + 1) * 128], HT[:, kc, blk * BLK:(blk + 1) * BLK],
                                 start=(kc == 0), stop=(kc == KC - 1), inc=(kc == KC - 1))
                        for kc in range(KC):
                            P.mm(pu, uu[:, kc, j * 128:(j + 1) * 128], HT[:, kc, blk * BLK:(blk + 1) * BLK],
                                 start=(kc == 0), stop=(kc == KC - 1), inc=(kc == KC - 1))
                        sg = SCR[k % 2][:, 0:BLK]
                        P.act(sg, pg, AF.Silu)
                        a_out = at[:, j, blk * BLK:(blk + 1) * BLK]
                        if CB is None:
                            P.tt(a_out, pu, sg, ALU.mult)
                        else:
                            t2 = SCR2[k % 2][:, 0:BLK]
                            P.tt(t2, pu, sg, ALU.mult)
                            P.tt(a_out, t2, CB[:, blk * BLK:(blk + 1) * BLK], ALU.mult)
                        k += 1

            def dn_phase(g):
                ud = units.pop(g)
                at = AT[g % 2]
                k = 0
                for co in range(KC):
                    for blk in range(NB):
                        pd = PS[4 + k % 2][:, 0:BLK]
                        for j in range(2):
                            P.mm(pd, ud[:, j, co * 128:(co + 1) * 128], at[:, j, blk * BLK:(blk + 1) * BLK],
                                 start=(j == 0), stop=(j == 1), inc=(j == 1))
                        xs = XT[:, co, blk * BLK:(blk + 1) * BLK]
                        P.tt(xs, pd, xs, ALU.add)
                        k += 1

            gu_phase(0)
            for g in range(1, NG):
                gu_phase(g)
                dn_phase(g - 1)
            dn_phase(NG - 1)

        def ffn_stage():
            cur[0] = stage_off
            HT = alloc([KC, NT], BF16)
            AT = [alloc([2, NT], BF16) for _ in range(2)]
            for blk in range(NB):
                t0 = blk * BLK
                rms(lambda kc: XT[:, kc, t0:t0 + BLK], BLK, V_NF0,
                    lambda kc: HT[:, kc, t0:t0 + BLK], RSTD[:, t0:t0 + BLK])
            ffn_pass(HT, AT, d_fwg, d_fwu, d_fwd, 0, 0)

        def gmlp_stage():
            cur[0] = stage_off
            HP = alloc([KC, BLK], BF16)
            VBF = [Reg(arena, "sb", HP.off + i * 4096, [D], BF16) for i in range(2)]
            cur[0] = max(cur[0], HP.off + 12288)
            VRAW = alloc([3, D], F32)
            U = alloc([KC, BLK], BF16)
            WSM = alloc([2, 8, 128], BF16)
            BSB = alloc([2, 8, 128], F32)
            BROW = alloc([D], F32)
            STATS = alloc([4, 6], F32)
            MV = alloc([4], F32)
            LNG = Reg(arena, "sb", ring_off + 4 * 8192, [D], F32)
            LNB = Reg(arena, "sb", ring_off + 5 * 8192, [D], F32)
            WST = Reg(arena, "sb", VRAW.off, [2, 8, 128], F32)
            MSK = Reg(arena, "sb", VRAW.off + 8192, [2, 128], F32)
            P.dma("sp", WST[:, 0, :, :], d_wsp.rearrange("p (g t) -> p g t", g=8))
            P.dma("sp", WST[:, 1, :, :], d_wss.rearrange("p (g t) -> p g t", g=8))
            P.dma("sp", MSK.all(), d_masks.rearrange("p (v t) -> p v t", v=2))
            for v in range(2):
                for g in range(8):
                    P.tt(WSM[:, v, g, :], WST[:, v, g, :], MSK[:, v, :], ALU.mult)
            P.dma("sp", BSB[:, 0, :, :], d_rows[0, R_BSP:R_BSP + 1024].partition_broadcast(128)
                  .rearrange("p (g t) -> p g t", g=8))
            P.dma("sp", BSB[:, 1, :, :], d_rows[0, R_BSS:R_BSS + 1024].partition_broadcast(128)
                  .rearrange("p (g t) -> p g t", g=8))
            P.dma("sp", LNG.all(), d_rows[0, R_LNG:R_LNG + D].partition_broadcast(128))
            P.dma("sp", LNB.all(), d_rows[0, R_LNB:R_LNB + D].partition_broadcast(128))
            P.dma("sp", BROW[0:1, :], d_rows[0:1, R_GBV:R_GBV + D])
            for b in range(NB):
                t0 = b * BLK
                rms(lambda kc: XT[:, kc, t0:t0 + BLK], BLK, V_NM1,
                    lambda kc: HP[:, kc, :], RSTD[:, t0:t0 + BLK])
                unit = None
                for c in range(KC):
                    if c % 2 == 0:
                        unit = load_gu(d_gwin, 0, (c // 2) * 256, 4)
                    j = c % 2
                    pu = PS[c % 2][:, 0:BLK]
                    for kc in range(KC):
                        P.mm(pu, unit[:, kc, j * 128:(j + 1) * 128], HP[:, kc, :], start=(kc == 0),
                             stop=(kc == KC - 1), inc=(kc == KC - 1))
                    P.act(U[:, c, :], pu, AF.Gelu, bias=vcol(V_GBU, c))
                k = 0
                for c2 in range(8):
                    unit = load_gu(d_gwin, 0, D + c2 * 256, 4)
                    for t in range(3):
                        pv = PS[2 + k % 2][:, 0:256]
                        for kc in range(KC):
                            P.mm(pv, HP[:, kc, t * 128:(t + 1) * 128], unit[:, kc, :], start=(kc == 0),
                                 stop=False)
                        P.mm(pv, ONES[0:1, :], BROW[0:1, c2 * 256:(c2 + 1) * 256], start=False, stop=True,
                             inc=True)
                        P.act(VRAW[:, t, c2 * 256:(c2 + 1) * 256], pv, AF.Gelu)
                        k += 1
                for t in range(3):
                    vr = VRAW[:, t, :]
                    for q in range(4):
                        so, si = STATS[:, q, :], VRAW[:, t, q * 512:(q + 1) * 512]
                        P.op("dve", lambda e, so=so, si=si: e.bn_stats(out=so.ap, in_=si.ap), [so], [si])
                    mv = MV[:, 0:2]
                    sa = STATS.all()
                    sflat = V(STATS.ap.rearrange("p a b -> p (a b)"), "sb", STATS.off, STATS.off + STATS.nbytes)
                    P.op("dve", lambda e, mv=mv, sflat=sflat: e.bn_aggr(out=mv.ap, in_=sflat.ap), [mv], [sflat])
                    rs = MV[:, 2:3]
                    P.act(rs, MV[:, 1:2], AF.Sqrt, bias=EPSL, scale=1.0)
                    P.op("dve", lambda e, rs=rs: e.reciprocal(out=rs.ap, in_=rs.ap), [rs], [rs])
                    P.ts(vr, vr, MV[:, 0:1], ALU.subtract, rs, ALU.mult)
                    P.tt(vr, vr, LNG.all(), ALU.mult)
                    P.tt(vr, vr, LNB.all(), ALU.add)
                    sample = (b == 2 and t == 2)
                    if sample:
                        P.dma("sp", o_vout, vr, is_output=True)
                    vb = VBF[t % 2]
                    P.copy(vb.all(), vr)
                    var = 1 if sample else 0
                    for c in range(KC):
                        g = c // 2
                        pm = PS[4 + c % 2][:, 0:128]
                        P.mm(pm, vb[:, c * 128:(c + 1) * 128], WSM[:, var, g, :], start=True, stop=True, inc=True)
                        tmp = SCR[c % 2][:, 0:128]
                        P.tt(tmp, pm, BSB[:, var, g, :], ALU.add)
                        us = U[:, c, t * 128:(t + 1) * 128]
                        P.tt(us, tmp, us, ALU.mult)
                out_proj(d_gwout, lambda kc: U[:, kc, :], BLK, t0, V_GBOUT, 4)

        def moe_stage():
            cur[0] = stage_off
            HT = alloc([KC, NT], BF16)
            AT = [alloc([2, NT], BF16) for _ in range(2)]
            CB = alloc([NT], F32)
            GR = alloc([KC, NE], F32)
            COMB = alloc([9, NE], F32)
            LG = alloc([NE], F32)
            L2 = alloc([NE], F32)
            EQ1 = alloc([NE], F32)
            EQ2 = alloc([NE], F32)
            SM = alloc([8], F32)
            for blk in range(NB):
                t0 = blk * BLK
                rms(lambda kc: XT[:, kc, t0:t0 + BLK], BLK, V_NF1,
                    lambda kc: HT[:, kc, t0:t0 + BLK], RSTD[:, t0:t0 + BLK])
            P.dma("sp", GR.all(), d_router.rearrange("(k p) e -> p k e", p=128))
            for kc in range(KC):
                P.ts(GR[:, kc, :], GR[:, kc, :], vcol(V_NF1, kc), ALU.mult)
            for t in range(9):
                pl = PS[7][:, 0:NE]
                for kc in range(KC):
                    P.mm(pl, XT[:, kc, t * 128:(t + 1) * 128], GR[:, kc, :], start=(kc == 0),
                         stop=(kc == KC - 1), inc=(kc == KC - 1))
                pr = PS[6][:, 0:1]
                P.mm(pr, RSTD[0:1, t * 128:(t + 1) * 128], ONES[0:1, 0:1], start=True, stop=True, inc=True)
                rc = SM[:, 0:1]
                P.copy(rc, pr)
                P.ts(LG.all(), pl, rc, ALU.mult)
                m1 = SM[:, 1:2]
                m2 = SM[:, 2:3]
                o1, i1 = m1.ap, LG.all().ap
                P.op("dve", lambda e, o1=o1, i1=i1: e.reduce_max(out=o1, in_=i1, axis=mybir.AxisListType.X),
                     [m1], [LG.all()])
                P.ts(EQ1.all(), LG.all(), m1, ALU.is_equal)
                P.stt(L2.all(), EQ1.all(), -1e30, LG.all(), ALU.mult, ALU.add)
                o2, i2 = m2.ap, L2.all().ap
                P.op("dve", lambda e, o2=o2, i2=i2: e.reduce_max(out=o2, in_=i2, axis=mybir.AxisListType.X),
                     [m2], [L2.all()])
                P.ts(EQ2.all(), L2.all(), m2, ALU.is_equal)
                dl = SM[:, 3:4]
                P.tt(dl, m2, m1, ALU.subtract)
                ex = SM[:, 4:5]
                P.act(ex, dl, AF.Exp)
                g1 = SM[:, 5:6]
                P.ts(g1, ex, 1.0, ALU.add)
                P.op("dve", lambda e, g1=g1: e.reciprocal(out=g1.ap, in_=g1.ap), [g1], [g1])
                g2 = SM[:, 6:7]
                P.tt(g2, ex, g1, ALU.mult)
                P.ts(EQ1.all(), EQ1.all(), g1, ALU.mult)
                P.stt(COMB[:, t, :], EQ2.all(), g2, EQ1.all(), ALU.mult, ALU.add)
            for e_i in range(n_exp):
                for t in range(9):
                    lb = SCR2[t % 2][:, 0:128]
                    P.ts(lb, ONES.all(), COMB[:, t, e_i:e_i + 1], ALU.mult)
                    pc = PS[7][:, (t % 3) * 128:(t % 3 + 1) * 128]
                    P.mm(pc, lb, IDENT.all(), start=True, stop=True, inc=True)
                    if t % 3 == 2:
                        blk = t // 3
                        P.copy(CB[:, blk * BLK:(blk + 1) * BLK], PS[7][:, 0:BLK])
                ffn_pass(HT, AT, d_mwg, d_mwu, d_mwd, e_i * D, e_i * FF, CB=CB)

        def moe_routed_stage(S0):
            NSC = S0 // 128
            NPASS = -(-NT // S0)
            NG = FC // 2
            cur[0] = HALOS.off
            HTOK = alloc([9, D], BF16)
            hg_off = cur[0]
            cur[0] += max(KC * S0 * 2, KC * BLK * 2)
            HG = Reg(arena, "sb", hg_off, [KC, S0], BF16)
            HTB = Reg(arena, "sb", hg_off, [KC, BLK], BF16)
            OBF = Reg(arena, "sb", hg_off, [NSC, D], BF16)
            OACC = alloc([NSC, D], F32)
            assert cur[0] <= ARENA_BYTES, cur[0]
            c5 = [ring_off + 5 * 8192]

            def a5(shape, dtype):
                esz = 2 if dtype == BF16 else 4
                nb = (_prod(shape) * esz + 3) // 4 * 4
                r = Reg(arena, "sb", c5[0], shape, dtype)
                c5[0] += nb
                assert c5[0] <= ring_off + 6 * 8192, c5[0]
                return r
            AT = [a5([2, S0], BF16) for _ in range(2)]
            COMB = a5([9, NE], F32)
            RR = a5([9, NE], F32)
            POSM = a5([9, NE], F32)
            HIF = a5([9, NE], F32)
            HIB = a5([9, NE], BF16)
            GHL = a5([9, NE, 2], BF16)
            CNT = a5([NE], F32)
            FLG = a5([32], F32)
            FLGI = a5([32], I32)
            PM = a5([12], F32)
            GS = a5([4], F32)
            GT = a5([4, 2], F32)
            IOTA_ROW = a5([S0], F32)
            IOTA_COL = a5([4], F32)
            UT = a5([128], F32)
            IDENTB = a5([128], BF16)
            LG = a5([NE], F32)
            L2 = a5([NE], F32)
            EQ1 = a5([NE], F32)
            EQ2 = a5([NE], F32)
            SM = a5([8], F32)
            GR = a5([KC, NE], F32)
            SEL = [Reg(arena, "sb", RSTD.off + i * 2304, [3, S0], BF16) for i in range(2)]
            SELT = [Reg(arena, "sb", RSTD.off + i * 2304, [NSC, BLK], BF16) for i in range(2)]

            P.dma("sp", IOTA_ROW.all(), d_iotas[:, 0:S0])
            P.dma("sp", IOTA_COL.all(), d_iotas[:, 512:516])
            P.dma("sp", UT.all(), d_ut)
            P.dma("sp", GR.all(), d_router.rearrange("(k p) e -> p k e", p=128))
            P.copy(IDENTB.all(), IDENT.all())
            for blk in range(NB):
                t0 = blk * BLK
                rms(lambda kc: XT[:, kc, t0:t0 + BLK], BLK, V_NF1,
                    lambda kc: HTB[:, kc, :], RSTD[:, t0:t0 + BLK])
                k = 0
                for tl in range(3):
                    t = 3 * blk + tl
                    for q4 in range(4):
                        ps = PS[k % 2]
                        for i in range(4):
                            kc = 4 * q4 + i
                            P.mm(ps[:, i * 128:(i + 1) * 128], HTB[:, kc, tl * 128:(tl + 1) * 128], IDENTB.all(),
                                 start=True, stop=True, inc=(i == 3))
                        P.copy(HTOK[:, t, q4 * 512:(q4 + 1) * 512], ps[:, 0:512])
                        k += 1
            for kc in range(KC):
                P.ts(GR[:, kc, :], GR[:, kc, :], vcol(V_NF1, kc), ALU.mult)
            for t in range(9):
                pl = PS[7][:, 0:NE]
                for kc in range(KC):
                    P.mm(pl, XT[:, kc, t * 128:(t + 1) * 128], GR[:, kc, :], start=(kc == 0),
                         stop=(kc == KC - 1), inc=(kc == KC - 1))
                pr = PS[6][:, 0:1]
                P.mm(pr, RSTD[0:1, t * 128:(t + 1) * 128], ONES[0:1, 0:1], start=True, stop=True, inc=True)
                rc = SM[:, 0:1]
                P.copy(rc, pr)
                P.ts(LG.all(), pl, rc, ALU.mult)
                m1 = SM[:, 1:2]
                m2 = SM[:, 2:3]
                o1, i1 = m1.ap, LG.all().ap
                P.op("dve", lambda e, o1=o1, i1=i1: e.reduce_max(out=o1, in_=i1, axis=mybir.AxisListType.X),
                     [m1], [LG.all()])
                P.ts(EQ1.all(), LG.all(), m1, ALU.is_equal)
                P.stt(L2.all(), EQ1.all(), -1e30, LG.all(), ALU.mult, ALU.add)
                o2, i2 = m2.ap, L2.all().ap
                P.op("dve", lambda e, o2=o2, i2=i2: e.reduce_max(out=o2, in_=i2, axis=mybir.AxisListType.X),
                     [m2], [L2.all()])
                P.ts(EQ2.all(), L2.all(), m2, ALU.is_equal)
                dl = SM[:, 3:4]
                P.tt(dl, m2, m1, ALU.subtract)
                ex = SM[:, 4:5]
                P.act(ex, dl, AF.Exp)
                g1 = SM[:, 5:6]
                P.ts(g1, ex, 1.0, ALU.add)
                P.op("dve", lambda e, g1=g1: e.reciprocal(out=g1.ap, in_=g1.ap), [g1], [g1])
                g2 = SM[:, 6:7]
                P.tt(g2, ex, g1, ALU.mult)
                P.ts(EQ1.all(), EQ1.all(), g1, ALU.mult)
                P.stt(COMB[:, t, :], EQ2.all(), g2, EQ1.all(), ALU.mult, ALU.add)
            P.ts(RR.all(), COMB.all(), 0.0, ALU.is_gt)
            for t in range(9):
                ps = PS[7][:, 0:NE]
                for t2 in range(t):
                    P.mm(ps, ONES.all(), RR[:, t2, :], start=(t2 == 0), stop=False)
                P.mm(ps, UT.all(), RR[:, t, :], start=(t == 0), stop=True, inc=True)
                P.stt(POSM[:, t, :], ps, 1.0, RR[:, t, :], ALU.add, ALU.mult)
                P.ts(POSM[:, t, :], POSM[:, t, :], -1.0, ALU.add)
            pc = PS[6][:, 0:NE]
            for t in range(9):
                P.mm(pc, ONES.all(), RR[:, t, :], start=(t == 0), stop=(t == 8), inc=(t == 8))
            P.copy(CNT.all(), pc)
            P.memset(FLG.all(), 0.0)
            for p in range(1, NPASS):
                P.ts(FLG[:, (p - 1) * NE:p * NE], CNT.all(), float(p * S0), ALU.is_gt)
            P.copy(FLGI.all(), FLG.all())
            P.copy(HIB.all(), COMB.all())
            P.copy(HIF.all(), HIB.all())
            P.copy(GHL[:, :, :, 0], HIB.all())
            P.tt(GHL[:, :, :, 1], COMB.all(), HIF.all(), ALU.subtract)

            mcount = [0]

            def expert_pass(e_i, p):
                P.ts(PM[:, 0:9], POSM[:, :, e_i], float(-p * S0), ALU.add)
                pgs = PS[6]
                for tb in range(3):
                    sel = SEL[tb % 2]
                    for tl in range(3):
                        t = 3 * tb + tl
                        P.ts(sel[:, tl, :], IOTA_ROW.all(), PM[:, t:t + 1], ALU.is_equal)
                    for kc in range(KC):
                        ps = PS[4 + kc % 2][:, 0:S0]
                        for tl in range(3):
                            t = 3 * tb + tl
                            P.mm(ps, HTOK[:, t, kc * 128:(kc + 1) * 128], sel[:, tl, :], start=(tl == 0),
                                 stop=(tl == 2), inc=(tl == 2))
                        if tb == 0:
                            P.copy(HG[:, kc, :], ps)
                        else:
                            P.tt(HG[:, kc, :], ps, HG[:, kc, :], ALU.add)
                    for sc in range(NSC):
                        for tl in range(3):
                            t = 3 * tb + tl
                            P.mm(pgs[:, 2 * sc:2 * sc + 2], sel[:, tl, sc * 128:(sc + 1) * 128], GHL[:, t, e_i, :],
                                 start=(tl == 0), stop=(tl == 2), inc=(tl == 2))
                    gt = V(GT.ap.rearrange("p a b -> p (a b)")[:, 0:2 * NSC], "sb", GT.off, GT.off + GT.nbytes)
                    if tb == 0:
                        P.copy(gt, pgs[:, 0:2 * NSC])
                    else:
                        P.tt(gt, pgs[:, 0:2 * NSC], gt, ALU.add)
                P.tt(GS[:, 0:NSC], GT[:, 0:NSC, 0], GT[:, 0:NSC, 1], ALU.add)
                seq = []
                for g in range(NG):
                    seq += [("g", g), ("u", g)]
                    if g >= 1:
                        seq.append(("d", g - 1))
                seq.append(("d", NG - 1))
                where = {it: i for i, it in enumerate(seq)}
                loaded = {}
                nxt = [0]

                def ensure(item):
                    while nxt[0] <= where[item]:
                        kind, g = seq[nxt[0]]
                        slot = mcount[0] % 5
                        mcount[0] += 1
                        if kind == "d":
                            r = ring_dn(slot)
                            src = d_mwd[e_i * FF + g * 256:e_i * FF + (g + 1) * 256, :].rearrange(
                                "(j p) c -> p j c", p=128)
                        else:
                            r = ring_gu(slot)
                            w_ap = d_mwg if kind == "g" else d_mwu
                            src = w_ap[e_i * D:(e_i + 1) * D, g * 256:(g + 1) * 256].rearrange(
                                "(k p) c -> p k c", p=128)
                        P.dma("pool", r.all(), src, sem="R%d" % slot)
                        loaded[(kind, g)] = r
                        nxt[0] += 1
                    return loaded[item]

                def gu_phase(g):
                    ug = ensure(("g", g))
                    uu = ensure(("u", g))
                    at = AT[g % 2]
                    for j in range(2):
                        pg = PS[j % 2][:, 0:S0]
                        pu = PS[2 + j % 2][:, 0:S0]
                        for kc in range(KC):
                            P.mm(pg, ug[:, kc, j * 128:(j + 1) * 128], HG[:, kc, :],
                                 start=(kc == 0), stop=(kc == KC - 1), inc=(kc == KC - 1))
                        for kc in range(KC):
                            P.mm(pu, uu[:, kc, j * 128:(j + 1) * 128], HG[:, kc, :],
                                 start=(kc == 0), stop=(kc == KC - 1), inc=(kc == KC - 1))
                        sg = SCR[j % 2][:, 0:S0]
                        P.act(sg, pg, AF.Silu)
                        P.tt(at[:, j, :], pu, sg, ALU.mult)

                def dn_phase(g):
                    ud = ensure(("d", g))
                    at = AT[g % 2]
                    k = 0
                    for sc in range(NSC):
                        for dq in range(4):
                            pd = PS[4 + k % 2][:, 0:512]
                            for j in range(2):
                                P.mm(pd, at[:, j, sc * 128:(sc + 1) * 128], ud[:, j, dq * 512:(dq + 1) * 512],
                                     start=(j == 0), stop=(j == 1), inc=(j == 1))
                            oa = OACC[:, sc, dq * 512:(dq + 1) * 512]
                            if g == 0:
                                P.copy(oa, pd)
                            else:
                                P.tt(oa, pd, oa, ALU.add)
                            k += 1

                gu_phase(0)
                for g in range(1, NG):
                    gu_phase(g)
                    dn_phase(g - 1)
                dn_phase(NG - 1)
                for sc in range(NSC):
                    P.ts(OBF[:, sc, :], OACC[:, sc, :], GS[:, sc:sc + 1], ALU.mult)
                k = 0
                for blk in range(NB):
                    for tl in range(3):
                        t = 3 * blk + tl
                        lb = SCR2[tl % 2][:, 0:128]
                        P.ts(lb, ONES.all(), PM[:, t:t + 1], ALU.mult)
                        P.mm(PS[7][:, tl * 128:(tl + 1) * 128], lb, IDENT.all(), start=True, stop=True, inc=True)
                    prow = SCR[blk % 2][:, 0:BLK]
                    P.copy(prow, PS[7][:, 0:BLK])
                    selt = SELT[blk % 2]
                    for sc in range(NSC):
                        P.ts(selt[:, sc, :], prow, IOTA_COL[:, sc:sc + 1], ALU.is_equal)
                    for co in range(KC):
                        ps = PS[k % 2][:, 0:BLK]
                        for sc in range(NSC):
                            P.mm(ps, OBF[:, sc, co * 128:(co + 1) * 128], selt[:, sc, :], start=(sc == 0),
                                 stop=(sc == NSC - 1), inc=(sc == NSC - 1))
                        xs = XT[:, co, blk * BLK:(blk + 1) * BLK]
                        P.tt(xs, ps, xs, ALU.add)
                        k += 1

            npass_emit = int(os.environ.get("MK_NPASS", str(NPASS)))
            for e_i in range(n_exp):
                for p in range(min(NPASS, npass_emit)):
                    if p > 0:
                        fi = (p - 1) * NE + e_i
                        P.region_begin(["pe", "act", "dve", "pool"], FLGI[0:1, fi:fi + 1])
                    expert_pass(e_i, p)
                    if p > 0:
                        P.region_end()

        def final_stage():
            cur[0] = stage_off
            YO = [alloc([KC, BLK], F32) for _ in range(2)]
            for blk in range(NB):
                t0 = blk * BLK
                yo = YO[blk % 2]
                rms(lambda kc: XT[:, kc, t0:t0 + BLK], BLK, V_NFIN,
                    lambda kc: yo[:, kc, :], RSTD[:, t0:t0 + BLK])
                for q4 in range(4):
                    P.dma("sp", o_yT[512 * q4:512 * (q4 + 1), t0:t0 + BLK].rearrange("(k p) t -> p k t", p=128),
                          yo[:, 4 * q4:4 * q4 + 4, :], is_output=True)

        if "conv" in stages:
            conv_stage()
        if "ffn" in stages:
            ffn_stage()
        if "gmlp" in stages:
            gmlp_stage()
        if "moe" in stages:
            if os.environ.get("MK_MOE", "routed") == "dense":
                moe_stage()
            else:
                moe_routed_stage(int(os.environ.get("MK_S0", "384")))
        final_stage()
        P.finish()

        with nc.Block() as block:
            @block.tensor
            def _(e):
                replay(e, P.q["pe"])

            @block.scalar
            def _(e):
                replay(e, P.q["act"])

            @block.vector
            def _(e):
                replay(e, P.q["dve"])

            @block.gpsimd
            def _(e):
                replay(e, P.q["pool"])

            @block.sync
            def _(e):
                replay(e, P.q["sp"])
        stats = {k: len(v) for k, v in P.q.items()}
        stats["waits"] = P.n_wait
    return nc, stats


def _pack_cols(vec):
    return np.ascontiguousarray(np.asarray(vec, np.float32).reshape(KC, 128).T)


def kernel(x_prompt, x_sample, state_conv, norm_mix, norm_ffn, norm_final,
           conv_w_in, conv_b_in, conv_w_dw, conv_b_dw, conv_ln_g, conv_ln_b, conv_w_out, conv_b_out,
           gmlp_w_in, gmlp_b_in, gmlp_ln_g, gmlp_ln_b, gmlp_w_s, gmlp_b_s, gmlp_w_out, gmlp_b_out,
           ffn_w_gate, ffn_w_up, ffn_w_down, moe_router, moe_w_gate, moe_w_up, moe_w_down):
    stages = os.environ.get("MK_STAGES", "conv,ffn,gmlp,moe").split(",")
    n_exp = int(os.environ.get("MK_NEXP", str(NE)))
    f = np.float32
    x_prompt = np.asarray(x_prompt, f)
    x_sample = np.asarray(x_sample, f)
    state_conv = np.asarray(state_conv, f)

    cols = [norm_mix[0], norm_mix[1], norm_ffn[0], norm_ffn[1], norm_final,
            conv_b_in[0, :D], conv_b_in[0, D:], conv_b_dw[0], conv_ln_g[0], conv_ln_b[0], conv_b_out[0],
            gmlp_b_in[0, :D], gmlp_b_out[0]]
    cols += [conv_w_dw[0, j] for j in range(NTAP)]
    vecs = np.ascontiguousarray(np.concatenate([_pack_cols(c) for c in cols], axis=1))
    assert vecs.shape == (128, NV)
    rows = np.zeros((1, NR), f)
    rows[0, R_GBV:R_GBV + D] = np.asarray(gmlp_b_in, f)[0, D:]
    rows[0, R_LNG:R_LNG + D] = np.asarray(gmlp_ln_g, f)[0]
    rows[0, R_LNB:R_LNB + D] = np.asarray(gmlp_ln_b, f)[0]
    bs = np.asarray(gmlp_b_s, f)[0]
    rows[0, R_BSP:R_BSP + 1024] = bs.reshape(-1)
    rows[0, R_BSS:R_BSS + 1024] = np.tile(bs[:, :DSEQ], (1, NSEQ)).reshape(-1)
    ws = np.asarray(gmlp_w_s, f)[0]
    wsp = np.ascontiguousarray(ws.transpose(2, 0, 1)).reshape(128, 1024)
    ws8 = ws[:, :DSEQ, :DSEQ].transpose(2, 0, 1)
    wss = np.ascontiguousarray(np.tile(ws8, (NSEQ, 1, NSEQ))).reshape(128, 1024)
    ii = np.arange(128)
    mtril = (ii[:, None] <= ii[None, :]).astype(f)
    mbd = mtril * ((ii[:, None] // DSEQ) == (ii[None, :] // DSEQ)).astype(f)
    masks = np.ascontiguousarray(np.concatenate([mtril, mbd], axis=1))
    ident = np.eye(128, dtype=f)
    iotas = np.zeros((128, 516), f)
    iotas[:, :512] = np.arange(512, dtype=f)[None, :]
    iotas[:, 512:516] = ii[:, None].astype(f) + 128.0 * np.arange(4, dtype=f)[None, :]
    ut = (ii[:, None] < ii[None, :]).astype(f)
    shared = {
        "iotas": iotas, "ut": ut,
        "vecs": vecs, "rows": rows, "wsp": wsp, "wss": wss, "masks": masks, "ident": ident,
        "router": np.ascontiguousarray(np.asarray(moe_router, f)[0]),
        "conv_w_in": np.asarray(conv_w_in, f)[0], "conv_w_out": np.asarray(conv_w_out, f)[0],
        "gmlp_w_in": np.asarray(gmlp_w_in, f)[0], "gmlp_w_out": np.asarray(gmlp_w_out, f)[0],
        "ffn_wg": np.asarray(ffn_w_gate, f)[0], "ffn_wu": np.asarray(ffn_w_up, f)[0],
        "ffn_wd": np.asarray(ffn_w_down, f)[0],
        "moe_wg": np.asarray(moe_w_gate, f).reshape(NE * D, FF),
        "moe_wu": np.asarray(moe_w_up, f).reshape(NE * D, FF),
        "moe_wd": np.asarray(moe_w_down, f).reshape(NE * FF, D),
    }
    in_maps = []
    for c in range(NCORES):
        b, half = c // 2, c % 2
        xp = x_prompt[b, half * 1024:(half + 1) * 1024]
        xs = x_sample[c * NSEQ:(c + 1) * NSEQ].reshape(NSEQ * DSEQ, D)
        xT = np.ascontiguousarray(np.concatenate([xp, xs], axis=0).T)
        if half == 1:
            xh = np.ascontiguousarray(x_prompt[b, 1024 - HALO:1024].T)
        else:
            xh = np.zeros((D, HALO), f)
        hmask = np.full((128, 1), float(half), f)
        sc = state_conv[0, c * NSEQ:(c + 1) * NSEQ]
        sconv = np.ascontiguousarray(sc.transpose(2, 0, 1)).reshape(D, NSEQ * HALO)
        m = {"xT": xT, "xh": xh, "hmask": hmask, "sconv": sconv}
        m.update(shared)
        in_maps.append(m)

    nc, _ = build_program(stages, n_exp)
    res = run_bass_kernel_spmd(nc, in_maps, core_ids=list(range(NCORES)))
    outs = res.results

    y_prompt = np.empty((4, 2048, D), f)
    y_sample = np.empty((128, DSEQ, D), f)
    ncp = np.empty((1, 4, HALO, D), f)
    ncs = np.empty((1, 128, HALO, D), f)
    vout = np.empty((1, 128, DSEQ, D), f)
    for c in range(NCORES):
        b, half = c // 2, c % 2
        yT = np.asarray(outs[c]["yT"])
        y_prompt[b, half * 1024:(half + 1) * 1024] = yT[:, :1024].T
        y_sample[c * NSEQ:(c + 1) * NSEQ] = yT[:, 1024:].T.reshape(NSEQ, DSEQ, D)
        if half == 1:
            ncp[0, b] = np.asarray(outs[c]["ncp"]).T
        ncs[0, c * NSEQ:(c + 1) * NSEQ] = np.asarray(outs[c]["ncs"]).reshape(D, NSEQ, HALO).transpose(1, 2, 0)
        vout[0, c * NSEQ:(c + 1) * NSEQ] = np.asarray(outs[c]["vout"]).reshape(NSEQ, DSEQ, D)
    return (y_prompt, y_sample, ncp, ncs, vout)
```

```python
import os
import numpy as np
import concourse.bass as bass
import concourse.mybir as mybir
from concourse.bass_utils import run_bass_kernel_spmd

F32 = mybir.dt.float32
BF16 = mybir.dt.bfloat16
ALU = mybir.AluOpType
AF = mybir.ActivationFunctionType

NCORES = 8
D = 2048
KC = 16
NT = 1152
BLK = 384
NB = 3
FF = 7168
FC = 56
NE = 8
HALO = 30
NTAP = 31
NSEQ = 16
DSEQ = 8
EPS_RMS = 1e-6
EPS_LN = 1e-5

V_NM0, V_NM1, V_NF0, V_NF1, V_NFIN, V_CBA, V_CBG, V_CBDW, V_CLNG, V_CLNB, V_CBOUT, V_GBU, V_GBOUT = [
    16 * i for i in range(13)]
V_CWDW = 16 * 13
NV = 16 * 13 + NTAP * 16
R_GBV, R_LNG, R_LNB, R_BSP, R_BSS = 0, 2048, 4096, 6144, 7168
NR = 8192

ARENA_BYTES = 212800
I32 = mybir.dt.int32


def _prod(s):
    r = 1
    for x in s:
        r *= x
    return r


class V:
    __slots__ = ("ap", "mem", "lo", "hi")

    def __init__(self, ap, mem, lo, hi):
        self.ap, self.mem, self.lo, self.hi = ap, mem, lo, hi


class Reg:
    def __init__(self, base_ap, mem, off, shape, dtype):
        self.mem = mem
        self.off = off
        self.shape = tuple(shape)
        self.esz = 2 if dtype == BF16 else 4
        n = _prod(shape)
        self.nbytes = n * self.esz
        assert off % 4 == 0 and self.nbytes % 4 == 0
        ap = base_ap[:, off // 4:(off + self.nbytes) // 4]
        if dtype != F32:
            ap = ap.bitcast(dtype)
        if len(shape) == 2:
            ap = ap.rearrange("p (a b) -> p a b", a=shape[0])
        elif len(shape) == 3:
            ap = ap.rearrange("p (a b c) -> p a b c", a=shape[0], b=shape[1])
        self.ap = ap
        self.strides = [_prod(shape[i + 1:]) for i in range(len(shape))]

    def __getitem__(self, idx):
        if not isinstance(idx, tuple):
            idx = (idx,)
        ap = self.ap[idx]
        lo = 0
        hi = 0
        for d, (sz, st) in enumerate(zip(self.shape, self.strides)):
            ix = idx[d + 1] if d + 1 < len(idx) else slice(None)
            if isinstance(ix, int):
                a, b = ix, ix + 1
            else:
                a = ix.start or 0
                b = sz if ix.stop is None else ix.stop
            lo += a * st
            hi += (b - 1) * st
        hi += 1
        return V(ap, self.mem, self.off + lo * self.esz, self.off + hi * self.esz)

    def all(self):
        return self[(slice(None),)]


class Prog:
    ENGS = ("pe", "act", "dve", "pool", "sp")

    def __init__(self, nc, sems):
        self.nc = nc
        self.sems = sems
        self.q = {e: [] for e in self.ENGS}
        self.cnt = {"pe": 0, "act": 0, "dve": 0}
        self.dcnt = {}
        self.known = {e: {} for e in self.ENGS}
        self.pe_idx = 0
        self.pe_miles_idx = []
        self.pe_miles_cnt = []
        self.recs = {"sb": [], "ps": []}
        self.out_events = {}
        self.sp_rr = 0
        self.n_wait = 0

    def _resolve(self, key, val):
        if key == "PE#":
            import bisect
            i = bisect.bisect_left(self.pe_miles_idx, val)
            if i >= len(self.pe_miles_idx):
                raise RuntimeError("PE read/write without a later milestone (idx %d)" % val)
            return "pe", self.pe_miles_cnt[i]
        return key, val

    def wait(self, eng, key, val):
        if key == "PE#" and eng == "pe":
            return
        key, val = self._resolve(key, val)
        if key == "pe" and eng == "pe":
            return
        if self.known[eng].get(key, 0) >= val:
            return
        self.known[eng][key] = val
        h = self.sems[key]
        self.q[eng].append(lambda e, h=h, v=val: e.wait_ge(h, v))
        self.n_wait += 1

    def _deps(self, eng, outs, ins):
        need = {}

        def add(evd):
            for k, v in evd.items():
                if need.get(k, -1) < v:
                    need[k] = v
        for v in ins:
            for r in self.recs[v.mem]:
                if r[0] < v.hi and v.lo < r[1]:
                    add(r[2])
        for v in outs:
            for r in self.recs[v.mem]:
                if r[0] < v.hi and v.lo < r[1]:
                    add(r[2])
                    add(r[3])
        for k, val in need.items():
            self.wait(eng, k, val)

    def _record(self, outs, ins, key, val):
        for v in ins:
            hit = False
            for r in self.recs[v.mem]:
                if r[0] < v.hi and v.lo < r[1]:
                    if r[3].get(key, -1) < val:
                        r[3][key] = val
                    if r[0] <= v.lo and v.hi <= r[1]:
                        hit = True
            if not hit:
                self.recs[v.mem].append([v.lo, v.hi, {}, {key: val}])
        for v in outs:
            lst = self.recs[v.mem]
            lst[:] = [r for r in lst if not (v.lo <= r[0] and r[1] <= v.hi)]
            lst.append([v.lo, v.hi, {key: val}, {}])

    def op(self, eng, fn, outs, ins):
        self._deps(eng, outs, ins)
        self.cnt[eng] += 1
        val = self.cnt[eng]
        h = self.sems[eng]
        self.q[eng].append(lambda e, fn=fn, h=h: fn(e).then_inc(h, 1))
        self.known[eng][eng] = max(self.known[eng].get(eng, 0), 0)
        self._record(outs, ins, eng, val)

    def mm(self, out, lhsT, rhs, start, stop, inc=False):
        self._deps("pe", [out], [lhsT, rhs])
        idx = self.pe_idx
        self.pe_idx += 1
        o, l, r = out.ap, lhsT.ap, rhs.ap
        if inc:
            self.cnt["pe"] += 1
            self.pe_miles_idx.append(idx)
            self.pe_miles_cnt.append(self.cnt["pe"])
            h = self.sems["pe"]
            self.q["pe"].append(lambda e, o=o, l=l, r=r, s=start, t=stop, h=h:
                                e.matmul(o, l, r, start=s, stop=t).then_inc(h, 1))
        else:
            self.q["pe"].append(lambda e, o=o, l=l, r=r, s=start, t=stop:
                                e.matmul(o, l, r, start=s, stop=t))
        self._record([out], [lhsT, rhs], "PE#", idx)

    def dma(self, queue, out, in_, sem=None, is_output=False):
        outs = [out] if isinstance(out, V) else []
        ins = [in_] if isinstance(in_, V) else []
        if sem is None:
            sem = "S%d" % (self.sp_rr % 8)
            self.sp_rr += 1
        prev = self.dcnt.get(sem, 0)
        if prev:
            self.wait(queue, sem, prev)
        self._deps(queue, outs, ins)
        val = prev + 16
        self.dcnt[sem] = val
        h = self.sems[sem]
        oa = out.ap if isinstance(out, V) else out
        ia = in_.ap if isinstance(in_, V) else in_
        self.q[queue].append(lambda e, oa=oa, ia=ia, h=h: e.dma_start(out=oa, in_=ia).then_inc(h, 16))
        self._record(outs, ins, sem, val)
        if is_output:
            self.out_events[sem] = val

    def act(self, out, in_, func, bias=None, scale=None):
        ins = [in_]
        kw = {}
        if bias is not None:
            if isinstance(bias, V):
                ins.append(bias)
                kw["bias"] = bias.ap
            else:
                kw["bias"] = bias
        if scale is not None:
            if isinstance(scale, V):
                ins.append(scale)
                kw["scale"] = scale.ap
            else:
                kw["scale"] = scale
        o, i = out.ap, in_.ap
        self.op("act", lambda e: e.activation(out=o, in_=i, func=func, **kw), [out], ins)

    def tt(self, out, in0, in1, op):
        o, a, b = out.ap, in0.ap, in1.ap
        self.op("dve", lambda e: e.tensor_tensor(out=o, in0=a, in1=b, op=op), [out], [in0, in1])

    def ts(self, out, in0, s1, op0, s2=None, op1=None):
        ins = [in0]
        a1 = s1
        if isinstance(s1, V):
            ins.append(s1)
            a1 = s1.ap
        a2 = s2
        if isinstance(s2, V):
            ins.append(s2)
            a2 = s2.ap
        o, a = out.ap, in0.ap
        if op1 is None:
            self.op("dve", lambda e: e.tensor_single_scalar(out=o, in_=a, scalar=a1, op=op0), [out], ins)
        else:
            self.op("dve", lambda e: e.tensor_scalar(out=o, in0=a, scalar1=a1, scalar2=a2, op0=op0, op1=op1),
                    [out], ins)

    def stt(self, out, in0, scalar, in1, op0, op1):
        ins = [in0, in1]
        sc = scalar
        if isinstance(scalar, V):
            ins.append(scalar)
            sc = scalar.ap
        o, a, b = out.ap, in0.ap, in1.ap
        self.op("dve", lambda e: e.scalar_tensor_tensor(out=o, in0=a, scalar=sc, in1=b, op0=op0, op1=op1),
                [out], ins)

    def copy(self, out, in_):
        o, a = out.ap, in_.ap
        self.op("dve", lambda e: e.tensor_copy(out=o, in_=a), [out], [in_])

    def memset(self, out, val):
        o = out.ap
        self.op("dve", lambda e: e.memset(o, val), [out], [])

    def finish(self):
        for sem, val in self.out_events.items():
            self.wait("sp", sem, val)

    def region_begin(self, engines, flag_view):
        self._region = dict(engines=engines, known={e: dict(self.known[e]) for e in engines},
                            cnt0=dict(self.cnt), dcnt0=dict(self.dcnt))
        for e in engines:
            self._deps(e, [], [flag_view])
            self.q[e].append(("if", flag_view.ap))

    def region_end(self):
        r = self._region
        self._region = None
        for e in r["engines"]:
            comp = []
            if e in self.cnt:
                m0, m1 = r["cnt0"][e], self.cnt[e]
                if m1 > m0:
                    comp.append((self.sems[e], m0, m1 - m0))
            if e == "pool":
                for sname, v1 in self.dcnt.items():
                    v0 = r["dcnt0"].get(sname, 0)
                    if sname.startswith("R") and v1 > v0:
                        comp.append((self.sems[sname], v0, v1 - v0))
            self.q[e].append(("else", comp))
            self.q[e].append(("endif",))
            self.known[e] = r["known"][e]


def replay(e, items):
    stack = []
    rguard = e.register("flag")
    reg = rguard.__enter__()
    for it in items:
        if isinstance(it, tuple):
            if it[0] == "if":
                e.reg_load(reg, it[1])
                g = e.If_ne(reg, 0)
                g.__enter__()
                stack.append(g)
            elif it[0] == "else":
                stack.pop().__exit__(None, None, None)
                g = e.Else()
                g.__enter__()
                stack.append(g)
                for semh, v0, n in it[1]:
                    if v0 > 0:
                        e.wait_ge(semh, v0)
                    e.sem_inc(semh, n)
            else:
                stack.pop().__exit__(None, None, None)
        else:
            it(e)
    rguard.__exit__(None, None, None)


def build_program(stages, n_exp):
    nc = bass.Bass("TRN2", target_bir_lowering=False)

    def din(name, shape):
        return nc.dram_tensor(name, list(shape), F32, kind="ExternalInput").ap()

    def dout(name, shape):
        return nc.dram_tensor(name, list(shape), F32, kind="ExternalOutput").ap()

    d_xT = din("xT", [D, NT])
    d_xh = din("xh", [D, HALO])
    d_hmask = din("hmask", [128, 1])
    d_sconv = din("sconv", [D, NSEQ * HALO])
    d_vecs = din("vecs", [128, NV])
    d_rows = din("rows", [1, NR])
    d_wsp = din("wsp", [128, 8 * 128])
    d_wss = din("wss", [128, 8 * 128])
    d_masks = din("masks", [128, 2 * 128])
    d_ident = din("ident", [128, 128])
    d_router = din("router", [D, NE])
    d_iotas = din("iotas", [128, 512 + 4])
    d_ut = din("ut", [128, 128])
    d_cwin = din("conv_w_in", [D, 2 * D])
    d_cwout = din("conv_w_out", [D, D])
    d_gwin = din("gmlp_w_in", [D, 2 * D])
    d_gwout = din("gmlp_w_out", [D, D])
    d_fwg = din("ffn_wg", [D, FF])
    d_fwu = din("ffn_wu", [D, FF])
    d_fwd = din("ffn_wd", [FF, D])
    d_mwg = din("moe_wg", [NE * D, FF])
    d_mwu = din("moe_wu", [NE * D, FF])
    d_mwd = din("moe_wd", [NE * FF, D])

    o_yT = dout("yT", [D, NT])
    o_ncp = dout("ncp", [D, HALO])
    o_ncs = dout("ncs", [D, NSEQ * HALO])
    o_vout = dout("vout", [128, D])

    sem_names = ["pe", "act", "dve"] + ["R%d" % i for i in range(6)] + ["S%d" % i for i in range(8)]

    import contextlib
    with contextlib.ExitStack() as es:
        arena = es.enter_context(nc.sbuf_tensor("arena", [128, ARENA_BYTES // 4], F32))
        psum = es.enter_context(nc.psum_tensor("psum", [128, 8 * 512], F32))
        sems = {n: es.enter_context(nc.semaphore("sem_" + n)) for n in sem_names}
        P = Prog(nc, sems)

        cur = [0]

        def alloc(shape, dtype, at=None):
            esz = 4 if dtype == F32 else 2
            nb = (_prod(shape) * esz + 3) // 4 * 4
            if at is None:
                off = cur[0]
                cur[0] += nb
            else:
                off = at
            assert off + nb <= ARENA_BYTES, (off, nb)
            return Reg(arena, "sb", off, shape, dtype)

        PS = [Reg(psum, "ps", b * 2048, [512], F32) for b in range(8)]

        XT = alloc([KC, NT], F32)
        ring_off = cur[0]
        cur[0] += 6 * 8192
        VECS = alloc([NV], F32)
        RSTD = alloc([NT], F32)
        SCR = [alloc([416], F32) for _ in range(2)]
        SCR2 = [alloc([416], F32) for _ in range(2)]
        ONES = alloc([128], F32)
        IDENT = alloc([128], F32)
        MISC = alloc([64], F32)
        HALOS = alloc([KC, HALO], F32)
        stage_off = cur[0]
        STAGE_BYTES = ARENA_BYTES - stage_off

        def ring_gu(slot):
            return Reg(arena, "sb", ring_off + slot * 8192, [KC, 256], BF16)

        def ring_dn(slot):
            return Reg(arena, "sb", ring_off + slot * 8192, [2, D], BF16)

        HMASK = MISC[:, 0:1]
        EPSR = MISC[:, 1:2]
        EPSL = MISC[:, 2:3]

        def vcol(base, kc):
            return VECS[:, base + kc:base + kc + 1]

        for q4 in range(4):
            P.dma("sp", XT[:, 4 * q4:4 * q4 + 4, :],
                  d_xT[512 * q4:512 * (q4 + 1), :].rearrange("(k p) t -> p k t", p=128))
        P.dma("sp", VECS.all(), d_vecs)
        P.dma("sp", IDENT.all(), d_ident)
        P.dma("sp", MISC[:, 0:1], d_hmask)
        P.memset(ONES.all(), 1.0)
        P.memset(MISC[:, 1:2], EPS_RMS)
        P.memset(MISC[:, 2:3], EPS_LN)

        wcount = [0]

        def load_gu(w_ap, row0, col0, nslots, slot_base=0):
            slot = slot_base + wcount[0] % nslots
            wcount[0] += 1
            r = ring_gu(slot)
            src = w_ap[row0:row0 + D, col0:col0 + 256].rearrange("(k p) c -> p k c", p=128)
            P.dma("pool", r.all(), src, sem="R%d" % slot)
            return r

        def load_dn(w_ap, row0, nslots):
            slot = wcount[0] % nslots
            wcount[0] += 1
            r = ring_dn(slot)
            src = w_ap[row0:row0 + 256, :].rearrange("(j p) c -> p j c", p=128)
            P.dma("pool", r.all(), src, sem="R%d" % slot)
            return r

        def rms(xv, n, gbase, hv, rstd, out_f32=False):
            ps = PS[6][:, 0:n]
            for kc in range(KC):
                s = SCR[kc % 2][:, 0:n]
                P.act(s, xv(kc), AF.Square)
                P.mm(ps, ONES.all(), s, start=(kc == 0), stop=(kc == KC - 1), inc=True)
            P.act(rstd, ps, AF.Sqrt, bias=EPSR, scale=1.0 / D)
            o, a = rstd.ap, rstd.ap
            P.op("dve", lambda e: e.reciprocal(out=o, in_=a), [rstd], [rstd])
            for kc in range(KC):
                P.stt(hv(kc), xv(kc), vcol(gbase, kc), rstd, ALU.mult, ALU.mult)

        def out_proj(w_ap, sin, n, t0, bbase, nslots):
            unit = None
            for co in range(KC):
                if co % 2 == 0:
                    unit = load_gu(w_ap, 0, (co // 2) * 256, nslots)
                j = co % 2
                po = PS[4 + co % 2][:, 0:n]
                for kc in range(KC):
                    P.mm(po, unit[:, kc, j * 128:(j + 1) * 128], sin(kc), start=(kc == 0),
                         stop=(kc == KC - 1), inc=(kc == KC - 1))
                xs = XT[:, co, t0:t0 + n]
                P.stt(xs, po, vcol(bbase, co), xs, ALU.add, ALU.add)

        def conv_stage():
            cur[0] = stage_off
            conv_pe = os.environ.get("MK_CONVPE", "1") == "1"
            HP = alloc([KC, 416], BF16)
            Y = alloc([KC, BLK], F32)
            ST = alloc([KC, BLK], BF16)
            GLU = [alloc([416], F32) for _ in range(2)]
            SCB = [alloc([NSEQ, HALO + DSEQ], F32) for _ in range(2)]
            XH = alloc([KC, 32], F32)
            MEAN = alloc([BLK], F32)
            RS = alloc([BLK], F32)
            MSQ = alloc([BLK], F32)
            RH = alloc([32], F32)
            P.dma("sp", XH[:, :, 0:HALO], d_xh.rearrange("(k p) t -> p k t", p=128))
            GLUB = [alloc([416], BF16) for _ in range(2)]
            DG = [Reg(arena, "sb", ring_off + (4 + i) * 8192, [NTAP, 128], BF16) for i in range(2)]
            ident_b = V(IDENT.ap[:, None, :].broadcast_to([128, NTAP, 128]), "sb", IDENT.off,
                        IDENT.off + IDENT.nbytes)

            def conv_taps(c, nprompt):
                dg, glub = DG[c % 2], GLUB[c % 2]
                pc = PS[4 + c % 2][:, 0:nprompt]
                for j2 in range(NTAP):
                    P.mm(pc, dg[:, j2, :], glub[:, j2:j2 + nprompt], start=(j2 == 0), stop=(j2 == NTAP - 1),
                         inc=(j2 == NTAP - 1))
                P.ts(Y[:, c, 0:nprompt], pc, vcol(V_CBDW, c), ALU.add)

            for b in range(NB):
                t0 = b * BLK
                c0 = 0 if b == 0 else HALO
                nprompt = BLK if b < 2 else 256
                rms(lambda kc: XT[:, kc, t0:t0 + BLK], BLK, V_NM0,
                    lambda kc: HP[:, kc, HALO:HALO + BLK], RSTD[:, t0:t0 + BLK])
                if b == 0:
                    rms(lambda kc: XH[:, kc, 0:HALO], HALO, V_NM0,
                        lambda kc: HP[:, kc, 0:HALO], RH[:, 0:HALO])
                ua = ug = None
                for c in range(KC):
                    if c % 2 == 0:
                        ua = load_gu(d_cwin, 0, (c // 2) * 256, 4)
                        ug = load_gu(d_cwin, 0, D + (c // 2) * 256, 4)
                    j = c % 2
                    n = HALO + BLK - c0
                    pa = PS[c % 2][:, c0:c0 + n]
                    pg = PS[2 + c % 2][:, c0:c0 + n]
                    for kc in range(KC):
                        P.mm(pa, ua[:, kc, j * 128:(j + 1) * 128], HP[:, kc, c0:c0 + n],
                             start=(kc == 0), stop=(kc == KC - 1), inc=(kc == KC - 1))
                    for kc in range(KC):
                        P.mm(pg, ug[:, kc, j * 128:(j + 1) * 128], HP[:, kc, c0:c0 + n],
                             start=(kc == 0), stop=(kc == KC - 1), inc=(kc == KC - 1))
                    sg = SCR[c % 2][:, c0:c0 + n]
                    P.act(sg, pg, AF.Sigmoid, bias=vcol(V_CBG, c))
                    glu = GLU[c % 2]
                    P.stt(glu[:, c0:c0 + n], pa, vcol(V_CBA, c), sg, ALU.add, ALU.mult)
                    if b == 0:
                        P.ts(glu[:, 0:HALO], glu[:, 0:HALO], HMASK, ALU.mult)
                    else:
                        P.copy(glu[:, 0:HALO], HALOS[:, c, :])
                    if conv_pe:
                        P.copy(GLUB[c % 2][:, 0:nprompt + HALO], glu[:, 0:nprompt + HALO])
                        wt = VECS.ap[:, V_CWDW:V_CWDW + 16 * NTAP].rearrange("p (j c) -> p j c", c=16)[:, :, c]
                        wt_b = V(wt[:, :, None].broadcast_to([128, NTAP, 128]), "sb", VECS.off + V_CWDW * 4,
                                 VECS.off + (V_CWDW + 16 * NTAP) * 4)
                        P.tt(DG[c % 2].all(), ident_b, wt_b, ALU.mult)
                    else:
                        acc = Y[:, c, 0:nprompt]
                        P.ts(acc, glu[:, 0:nprompt], vcol(V_CWDW + 0, c), ALU.mult, vcol(V_CBDW, c), ALU.add)
                        for j2 in range(1, NTAP):
                            P.stt(acc, glu[:, j2:j2 + nprompt], vcol(V_CWDW + 16 * j2, c), acc, ALU.mult, ALU.add)
                    P.copy(HALOS[:, c, :], glu[:, nprompt:nprompt + HALO])
                    if b == 2:
                        scb = SCB[c % 2]
                        P.dma("sp", scb[:, :, 0:HALO],
                              d_sconv[c * 128:(c + 1) * 128, :].rearrange("p (s r) -> p s r", s=NSEQ))
                        gs = V(glu.ap[:, HALO + 256:HALO + 384].rearrange("p (s r) -> p s r", s=NSEQ), "sb",
                               glu.off + (HALO + 256) * 4, glu.off + (HALO + 384) * 4)
                        P.copy(scb[:, :, HALO:HALO + DSEQ], gs)
                        accs = V(Y.ap[:, c, 256:384].rearrange("p (s r) -> p s r", s=NSEQ), "sb",
                                 Y.off + (c * BLK + 256) * 4, Y.off + (c * BLK + 384) * 4)
                        P.ts(accs, scb[:, :, 0:DSEQ], vcol(V_CWDW + 0, c), ALU.mult, vcol(V_CBDW, c), ALU.add)
                        for j2 in range(1, NTAP):
                            P.stt(accs, scb[:, :, j2:j2 + DSEQ], vcol(V_CWDW + 16 * j2, c), accs,
                                  ALU.mult, ALU.add)
                        P.dma("sp", o_ncs[c * 128:(c + 1) * 128, :].rearrange("p (s r) -> p s r", s=NSEQ),
                              scb[:, :, DSEQ:DSEQ + HALO], is_output=True)
                    if conv_pe and c >= 1:
                        conv_taps(c - 1, nprompt)
                if conv_pe:
                    conv_taps(KC - 1, nprompt)
                if b == 2:
                    P.dma("sp", o_ncp.rearrange("(k p) r -> p k r", p=128), HALOS.all(), is_output=True)
                s1 = PS[6][:, 0:BLK]
                s2 = PS[7][:, 0:BLK]
                for c in range(KC):
                    P.mm(s1, ONES.all(), Y[:, c, :], start=(c == 0), stop=(c == KC - 1), inc=True)
                    sq = SCR[c % 2][:, 0:BLK]
                    P.act(sq, Y[:, c, :], AF.Square)
                    P.mm(s2, ONES.all(), sq, start=(c == 0), stop=(c == KC - 1), inc=True)
                P.ts(MEAN.all(), s1, 1.0 / D, ALU.mult)
                P.tt(MSQ.all(), MEAN.all(), MEAN.all(), ALU.mult)
                P.stt(MSQ.all(), s2, 1.0 / D, MSQ.all(), ALU.mult, ALU.subtract)
                P.act(RS.all(), MSQ.all(), AF.Sqrt, bias=EPSL, scale=1.0)
                o, a = RS.all().ap, RS.all().ap
                P.op("dve", lambda e: e.reciprocal(out=o, in_=a), [RS.all()], [RS.all()])
                for c in range(KC):
                    P.tt(Y[:, c, :], Y[:, c, :], MEAN.all(), ALU.subtract)
                    P.tt(Y[:, c, :], Y[:, c, :], RS.all(), ALU.mult)
                    P.act(ST[:, c, :], Y[:, c, :], AF.Silu, bias=vcol(V_CLNB, c), scale=vcol(V_CLNG, c))
                out_proj(d_cwout, lambda kc: ST[:, kc, :], BLK, t0, V_CBOUT, 4)

        def ffn_pass(HT, AT, wg, wu, wd, grow0, drow0, CB=None):
            NG = FC // 2
            units = {}

            def gu_phase(g):
                ug = load_gu(wg, grow0, g * 256, 6)
                uu = load_gu(wu, grow0, g * 256, 6)
                if g % 2 == 1:
                    units[g - 1] = load_dn(wd, drow0 + (g - 1) * 256, 6)
                    units[g] = load_dn(wd, drow0 + g * 256, 6)
                at = AT[g % 2]
                k = 0
                for j in range(2):
                    for blk in range(NB):
                        pg = PS[k % 2][:, 0:BLK]
                        pu = PS[2 + k % 2][:, 0:BLK]
                        for kc in range(KC):
                            P.mm(pg, ug[:, kc, j * 128:(j + 1) * 128], HT[:, kc, blk * BLK:(blk + 1) * BLK],
                                 start=(kc == 0), stop=(kc == KC - 1), inc=(kc == KC - 1))
                        for kc in range(KC):
                            P.mm(pu, uu[:, kc, j * 128:(j + 1) * 128], HT[:, kc, blk * BLK:(blk + 1) * BLK],
                                 start=(kc == 0), stop=(kc == KC - 1), inc=(kc == KC - 1))
                        sg = SCR[k % 2][:, 0:BLK]
                        P.act(sg, pg, AF.Silu)
                        a_out = at[:, j, blk * BLK:(blk + 1) * BLK]
                        if CB is None:
                            P.tt(a_out, pu, sg, ALU.mult)
                        else:
                            t2 = SCR2[k % 2][:, 0:BLK]
                            P.tt(t2, pu, sg, ALU.mult)
                            P.tt(a_out, t2, CB[:, blk * BLK:(blk + 1) * BLK], ALU.mult)
                        k += 1

            def dn_pair(g):
                uds = [units.pop(g), units.pop(g + 1)]
                k = 0
                for co in range(KC):
                    for blk in range(NB):
                        pd = PS[4 + k % 2][:, 0:BLK]
                        for i in range(4):
                            at, ud, j = AT[(g + i // 2) % 2], uds[i // 2], i % 2
                            P.mm(pd, ud[:, j, co * 128:(co + 1) * 128], at[:, j, blk * BLK:(blk + 1) * BLK],
                                 start=(i == 0), stop=(i == 3), inc=(i == 3))
                        xs = XT[:, co, blk * BLK:(blk + 1) * BLK]
                        P.tt(xs, pd, xs, ALU.add)
                        k += 1

            for g in range(0, NG, 2):
                gu_phase(g)
                gu_phase(g + 1)
                dn_pair(g)

        def ffn_stage():
            cur[0] = stage_off
            HT = alloc([KC, NT], BF16)
            AT = [alloc([2, NT], BF16) for _ in range(2)]
            for blk in range(NB):
                t0 = blk * BLK
                rms(lambda kc: XT[:, kc, t0:t0 + BLK], BLK, V_NF0,
                    lambda kc: HT[:, kc, t0:t0 + BLK], RSTD[:, t0:t0 + BLK])
            ffn_pass(HT, AT, d_fwg, d_fwu, d_fwd, 0, 0)

        def gmlp_stage():
            cur[0] = stage_off
            HP = alloc([KC, BLK], BF16)
            VBF = [Reg(arena, "sb", HP.off + i * 4096, [D], BF16) for i in range(2)]
            cur[0] = max(cur[0], HP.off + 12288)
            VRAW = alloc([3, D], F32)
            U = alloc([KC, BLK], BF16)
            WSM = alloc([2, 8, 128], BF16)
            BSB = alloc([2, 8, 128], F32)
            BROW = alloc([D], F32)
            STATS = alloc([4, 6], F32)
            MV = alloc([4], F32)
            LNG = Reg(arena, "sb", ring_off + 4 * 8192, [D], F32)
            LNB = Reg(arena, "sb", ring_off + 5 * 8192, [D], F32)
            WST = Reg(arena, "sb", VRAW.off, [2, 8, 128], F32)
            MSK = Reg(arena, "sb", VRAW.off + 8192, [2, 128], F32)
            P.dma("sp", WST[:, 0, :, :], d_wsp.rearrange("p (g t) -> p g t", g=8))
            P.dma("sp", WST[:, 1, :, :], d_wss.rearrange("p (g t) -> p g t", g=8))
            P.dma("sp", MSK.all(), d_masks.rearrange("p (v t) -> p v t", v=2))
            for v in range(2):
                for g in range(8):
                    P.tt(WSM[:, v, g, :], WST[:, v, g, :], MSK[:, v, :], ALU.mult)
            P.dma("sp", BSB[:, 0, :, :], d_rows[0, R_BSP:R_BSP + 1024].partition_broadcast(128)
                  .rearrange("p (g t) -> p g t", g=8))
            P.dma("sp", BSB[:, 1, :, :], d_rows[0, R_BSS:R_BSS + 1024].partition_broadcast(128)
                  .rearrange("p (g t) -> p g t", g=8))
            P.dma("sp", LNG.all(), d_rows[0, R_LNG:R_LNG + D].partition_broadcast(128))
            P.dma("sp", LNB.all(), d_rows[0, R_LNB:R_LNB + D].partition_broadcast(128))
            P.dma("sp", BROW[0:1, :], d_rows[0:1, R_GBV:R_GBV + D])
            for b in range(NB):
                t0 = b * BLK
                rms(lambda kc: XT[:, kc, t0:t0 + BLK], BLK, V_NM1,
                    lambda kc: HP[:, kc, :], RSTD[:, t0:t0 + BLK])
                unit = None
                for c in range(KC):
                    if c % 2 == 0:
                        unit = load_gu(d_gwin, 0, (c // 2) * 256, 4)
                    j = c % 2
                    pu = PS[c % 2][:, 0:BLK]
                    for kc in range(KC):
                        P.mm(pu, unit[:, kc, j * 128:(j + 1) * 128], HP[:, kc, :], start=(kc == 0),
                             stop=(kc == KC - 1), inc=(kc == KC - 1))
                    P.act(U[:, c, :], pu, AF.Gelu, bias=vcol(V_GBU, c))
                k = 0
                for c2 in range(8):
                    unit = load_gu(d_gwin, 0, D + c2 * 256, 4)
                    for t in range(3):
                        pv = PS[2 + k % 2][:, 0:256]
                        for kc in range(KC):
                            P.mm(pv, HP[:, kc, t * 128:(t + 1) * 128], unit[:, kc, :], start=(kc == 0),
                                 stop=False)
                        P.mm(pv, ONES[0:1, :], BROW[0:1, c2 * 256:(c2 + 1) * 256], start=False, stop=True,
                             inc=True)
                        P.act(VRAW[:, t, c2 * 256:(c2 + 1) * 256], pv, AF.Gelu)
                        k += 1
                for t in range(3):
                    vr = VRAW[:, t, :]
                    for q in range(4):
                        so, si = STATS[:, q, :], VRAW[:, t, q * 512:(q + 1) * 512]
                        P.op("dve", lambda e, so=so, si=si: e.bn_stats(out=so.ap, in_=si.ap), [so], [si])
                    mv = MV[:, 0:2]
                    sa = STATS.all()
                    sflat = V(STATS.ap.rearrange("p a b -> p (a b)"), "sb", STATS.off, STATS.off + STATS.nbytes)
                    P.op("dve", lambda e, mv=mv, sflat=sflat: e.bn_aggr(out=mv.ap, in_=sflat.ap), [mv], [sflat])
                    rs = MV[:, 2:3]
                    P.act(rs, MV[:, 1:2], AF.Sqrt, bias=EPSL, scale=1.0)
                    P.op("dve", lambda e, rs=rs: e.reciprocal(out=rs.ap, in_=rs.ap), [rs], [rs])
                    P.ts(vr, vr, MV[:, 0:1], ALU.subtract, rs, ALU.mult)
                    P.tt(vr, vr, LNG.all(), ALU.mult)
                    P.tt(vr, vr, LNB.all(), ALU.add)
                    sample = (b == 2 and t == 2)
                    if sample:
                        P.dma("sp", o_vout, vr, is_output=True)
                    vb = VBF[t % 2]
                    P.copy(vb.all(), vr)
                    var = 1 if sample else 0
                    for c in range(KC):
                        g = c // 2
                        pm = PS[4 + c % 2][:, 0:128]
                        P.mm(pm, vb[:, c * 128:(c + 1) * 128], WSM[:, var, g, :], start=True, stop=True, inc=True)
                        tmp = SCR[c % 2][:, 0:128]
                        P.tt(tmp, pm, BSB[:, var, g, :], ALU.add)
                        us = U[:, c, t * 128:(t + 1) * 128]
                        P.tt(us, tmp, us, ALU.mult)
                out_proj(d_gwout, lambda kc: U[:, kc, :], BLK, t0, V_GBOUT, 4)

        def moe_stage():
            cur[0] = stage_off
            HT = alloc([KC, NT], BF16)
            AT = [alloc([2, NT], BF16) for _ in range(2)]
            CB = alloc([NT], F32)
            GR = alloc([KC, NE], F32)
            COMB = alloc([9, NE], F32)
            LG = alloc([NE], F32)
            L2 = alloc([NE], F32)
            EQ1 = alloc([NE], F32)
            EQ2 = alloc([NE], F32)
            SM = alloc([8], F32)
            for blk in range(NB):
                t0 = blk * BLK
                rms(lambda kc: XT[:, kc, t0:t0 + BLK], BLK, V_NF1,
                    lambda kc: HT[:, kc, t0:t0 + BLK], RSTD[:, t0:t0 + BLK])
            P.dma("sp", GR.all(), d_router.rearrange("(k p) e -> p k e", p=128))
            for kc in range(KC):
                P.ts(GR[:, kc, :], GR[:, kc, :], vcol(V_NF1, kc), ALU.mult)
            for t in range(9):
                pl = PS[7][:, 0:NE]
                for kc in range(KC):
                    P.mm(pl, XT[:, kc, t * 128:(t + 1) * 128], GR[:, kc, :], start=(kc == 0),
                         stop=(kc == KC - 1), inc=(kc == KC - 1))
                pr = PS[6][:, 0:1]
                P.mm(pr, RSTD[0:1, t * 128:(t + 1) * 128], ONES[0:1, 0:1], start=True, stop=True, inc=True)
                rc = SM[:, 0:1]
                P.copy(rc, pr)
                P.ts(LG.all(), pl, rc, ALU.mult)
                m1 = SM[:, 1:2]
                m2 = SM[:, 2:3]
                o1, i1 = m1.ap, LG.all().ap
                P.op("dve", lambda e, o1=o1, i1=i1: e.reduce_max(out=o1, in_=i1, axis=mybir.AxisListType.X),
                     [m1], [LG.all()])
                P.ts(EQ1.all(), LG.all(), m1, ALU.is_equal)
                P.stt(L2.all(), EQ1.all(), -1e30, LG.all(), ALU.mult, ALU.add)
                o2, i2 = m2.ap, L2.all().ap
                P.op("dve", lambda e, o2=o2, i2=i2: e.reduce_max(out=o2, in_=i2, axis=mybir.AxisListType.X),
                     [m2], [L2.all()])
                P.ts(EQ2.all(), L2.all(), m2, ALU.is_equal)
                dl = SM[:, 3:4]
                P.tt(dl, m2, m1, ALU.subtract)
                ex = SM[:, 4:5]
                P.act(ex, dl, AF.Exp)
                g1 = SM[:, 5:6]
                P.ts(g1, ex, 1.0, ALU.add)
                P.op("dve", lambda e, g1=g1: e.reciprocal(out=g1.ap, in_=g1.ap), [g1], [g1])
                g2 = SM[:, 6:7]
                P.tt(g2, ex, g1, ALU.mult)
                P.ts(EQ1.all(), EQ1.all(), g1, ALU.mult)
                P.stt(COMB[:, t, :], EQ2.all(), g2, EQ1.all(), ALU.mult, ALU.add)
            for e_i in range(n_exp):
                for t in range(9):
                    lb = SCR2[t % 2][:, 0:128]
                    P.ts(lb, ONES.all(), COMB[:, t, e_i:e_i + 1], ALU.mult)
                    pc = PS[7][:, (t % 3) * 128:(t % 3 + 1) * 128]
                    P.mm(pc, lb, IDENT.all(), start=True, stop=True, inc=True)
                    if t % 3 == 2:
                        blk = t // 3
                        P.copy(CB[:, blk * BLK:(blk + 1) * BLK], PS[7][:, 0:BLK])
                ffn_pass(HT, AT, d_mwg, d_mwu, d_mwd, e_i * D, e_i * FF, CB=CB)

        def moe_routed_stage(S0):
            NSC = S0 // 128
            NPASS = -(-NT // S0)
            NG = FC // 2
            cur[0] = HALOS.off
            HTOK = alloc([9, D], BF16)
            hg_off = cur[0]
            cur[0] += max(KC * S0 * 2, KC * BLK * 2)
            HG = Reg(arena, "sb", hg_off, [KC, S0], BF16)
            HTB = Reg(arena, "sb", hg_off, [KC, BLK], BF16)
            OBF = Reg(arena, "sb", hg_off, [NSC, D], BF16)
            OACC = alloc([NSC, D], F32)
            assert cur[0] <= ARENA_BYTES, cur[0]
            c5 = [ring_off + 5 * 8192]

            def a5(shape, dtype):
                esz = 2 if dtype == BF16 else 4
                nb = (_prod(shape) * esz + 3) // 4 * 4
                r = Reg(arena, "sb", c5[0], shape, dtype)
                c5[0] += nb
                assert c5[0] <= ring_off + 6 * 8192, c5[0]
                return r
            AT = [a5([2, S0], BF16) for _ in range(2)]
            COMB = a5([9, NE], F32)
            RR = a5([9, NE], F32)
            POSM = a5([9, NE], F32)
            HIF = a5([9, NE], F32)
            HIB = a5([9, NE], BF16)
            GHL = a5([9, NE, 2], BF16)
            CNT = a5([NE], F32)
            FLG = a5([32], F32)
            FLGI = a5([32], I32)
            PM = a5([12], F32)
            GS = a5([4], F32)
            GT = a5([4, 2], F32)
            IOTA_ROW = a5([S0], F32)
            IOTA_COL = a5([4], F32)
            UT = a5([128], F32)
            IDENTB = a5([128], BF16)
            LG = a5([NE], F32)
            L2 = a5([NE], F32)
            EQ1 = a5([NE], F32)
            EQ2 = a5([NE], F32)
            SM = a5([8], F32)
            GR = a5([KC, NE], F32)
            SEL = [Reg(arena, "sb", RSTD.off + i * 2304, [3, S0], BF16) for i in range(2)]
            SELT = [Reg(arena, "sb", RSTD.off + i * 2304, [NSC, BLK], BF16) for i in range(2)]

            P.dma("sp", IOTA_ROW.all(), d_iotas[:, 0:S0])
            P.dma("sp", IOTA_COL.all(), d_iotas[:, 512:516])
            P.dma("sp", UT.all(), d_ut)
            P.dma("sp", GR.all(), d_router.rearrange("(k p) e -> p k e", p=128))
            P.copy(IDENTB.all(), IDENT.all())
            for blk in range(NB):
                t0 = blk * BLK
                rms(lambda kc: XT[:, kc, t0:t0 + BLK], BLK, V_NF1,
                    lambda kc: HTB[:, kc, :], RSTD[:, t0:t0 + BLK])
                k = 0
                for tl in range(3):
                    t = 3 * blk + tl
                    for q4 in range(4):
                        ps = PS[k % 2]
                        for i in range(4):
                            kc = 4 * q4 + i
                            P.mm(ps[:, i * 128:(i + 1) * 128], HTB[:, kc, tl * 128:(tl + 1) * 128], IDENTB.all(),
                                 start=True, stop=True, inc=(i == 3))
                        P.copy(HTOK[:, t, q4 * 512:(q4 + 1) * 512], ps[:, 0:512])
                        k += 1
            for kc in range(KC):
                P.ts(GR[:, kc, :], GR[:, kc, :], vcol(V_NF1, kc), ALU.mult)
            for t in range(9):
                pl = PS[7][:, 0:NE]
                for kc in range(KC):
                    P.mm(pl, XT[:, kc, t * 128:(t + 1) * 128], GR[:, kc, :], start=(kc == 0),
                         stop=(kc == KC - 1), inc=(kc == KC - 1))
                pr = PS[6][:, 0:1]
                P.mm(pr, RSTD[0:1, t * 128:(t + 1) * 128], ONES[0:1, 0:1], start=True, stop=True, inc=True)
                rc = SM[:, 0:1]
                P.copy(rc, pr)
                P.ts(LG.all(), pl, rc, ALU.mult)
                m1 = SM[:, 1:2]
                m2 = SM[:, 2:3]
                o1, i1 = m1.ap, LG.all().ap
                P.op("dve", lambda e, o1=o1, i1=i1: e.reduce_max(out=o1, in_=i1, axis=mybir.AxisListType.X),
                     [m1], [LG.all()])
                P.ts(EQ1.all(), LG.all(), m1, ALU.is_equal)
                P.stt(L2.all(), EQ1.all(), -1e30, LG.all(), ALU.mult, ALU.add)
                o2, i2 = m2.ap, L2.all().ap
                P.op("dve", lambda e, o2=o2, i2=i2: e.reduce_max(out=o2, in_=i2, axis=mybir.AxisListType.X),
                     [m2], [L2.all()])
                P.ts(EQ2.all(), L2.all(), m2, ALU.is_equal)
                dl = SM[:, 3:4]
                P.tt(dl, m2, m1, ALU.subtract)
                ex = SM[:, 4:5]
                P.act(ex, dl, AF.Exp)
                g1 = SM[:, 5:6]
                P.ts(g1, ex, 1.0, ALU.add)
                P.op("dve", lambda e, g1=g1: e.reciprocal(out=g1.ap, in_=g1.ap), [g1], [g1])
                g2 = SM[:, 6:7]
                P.tt(g2, ex, g1, ALU.mult)
                P.ts(EQ1.all(), EQ1.all(), g1, ALU.mult)
                P.stt(COMB[:, t, :], EQ2.all(), g2, EQ1.all(), ALU.mult, ALU.add)
            P.ts(RR.all(), COMB.all(), 0.0, ALU.is_gt)
            for t in range(9):
                ps = PS[7][:, 0:NE]
                for t2 in range(t):
                    P.mm(ps, ONES.all(), RR[:, t2, :], start=(t2 == 0), stop=False)
                P.mm(ps, UT.all(), RR[:, t, :], start=(t == 0), stop=True, inc=True)
                P.stt(POSM[:, t, :], ps, 1.0, RR[:, t, :], ALU.add, ALU.mult)
                P.ts(POSM[:, t, :], POSM[:, t, :], -1.0, ALU.add)
            pc = PS[6][:, 0:NE]
            for t in range(9):
                P.mm(pc, ONES.all(), RR[:, t, :], start=(t == 0), stop=(t == 8), inc=(t == 8))
            P.copy(CNT.all(), pc)
            P.memset(FLG.all(), 0.0)
            for p in range(1, NPASS):
                P.ts(FLG[:, (p - 1) * NE:p * NE], CNT.all(), float(p * S0), ALU.is_gt)
            P.copy(FLGI.all(), FLG.all())
            P.copy(HIB.all(), COMB.all())
            P.copy(HIF.all(), HIB.all())
            P.copy(GHL[:, :, :, 0], HIB.all())
            P.tt(GHL[:, :, :, 1], COMB.all(), HIF.all(), ALU.subtract)

            mcount = [0]

            def expert_pass(e_i, p):
                P.ts(PM[:, 0:9], POSM[:, :, e_i], float(-p * S0), ALU.add)
                pgs = PS[6]
                for tb in range(3):
                    sel = SEL[tb % 2]
                    for tl in range(3):
                        t = 3 * tb + tl
                        P.ts(sel[:, tl, :], IOTA_ROW.all(), PM[:, t:t + 1], ALU.is_equal)
                    for kc in range(KC):
                        ps = PS[4 + kc % 2][:, 0:S0]
                        for tl in range(3):
                            t = 3 * tb + tl
                            P.mm(ps, HTOK[:, t, kc * 128:(kc + 1) * 128], sel[:, tl, :], start=(tl == 0),
                                 stop=(tl == 2), inc=(tl == 2))
                        if tb == 0:
                            P.copy(HG[:, kc, :], ps)
                        else:
                            P.tt(HG[:, kc, :], ps, HG[:, kc, :], ALU.add)
                    for sc in range(NSC):
                        for tl in range(3):
                            t = 3 * tb + tl
                            P.mm(pgs[:, 2 * sc:2 * sc + 2], sel[:, tl, sc * 128:(sc + 1) * 128], GHL[:, t, e_i, :],
                                 start=(tl == 0), stop=(tl == 2), inc=(tl == 2))
                    gt = V(GT.ap.rearrange("p a b -> p (a b)")[:, 0:2 * NSC], "sb", GT.off, GT.off + GT.nbytes)
                    if tb == 0:
                        P.copy(gt, pgs[:, 0:2 * NSC])
                    else:
                        P.tt(gt, pgs[:, 0:2 * NSC], gt, ALU.add)
                P.tt(GS[:, 0:NSC], GT[:, 0:NSC, 0], GT[:, 0:NSC, 1], ALU.add)
                seq = []
                for g in range(0, NG, 2):
                    seq += [("g", g), ("u", g), ("g", g + 1), ("u", g + 1), ("d", g), ("d", g + 1)]
                where = {it: i for i, it in enumerate(seq)}
                loaded = {}
                nxt = [0]

                def ensure(item):
                    while nxt[0] <= where[item]:
                        kind, g = seq[nxt[0]]
                        slot = mcount[0] % 5
                        mcount[0] += 1
                        if kind == "d":
                            r = ring_dn(slot)
                            src = d_mwd[e_i * FF + g * 256:e_i * FF + (g + 1) * 256, :].rearrange(
                                "(j p) c -> p j c", p=128)
                        else:
                            r = ring_gu(slot)
                            w_ap = d_mwg if kind == "g" else d_mwu
                            src = w_ap[e_i * D:(e_i + 1) * D, g * 256:(g + 1) * 256].rearrange(
                                "(k p) c -> p k c", p=128)
                        P.dma("pool", r.all(), src, sem="R%d" % slot)
                        loaded[(kind, g)] = r
                        nxt[0] += 1
                    return loaded[item]

                def gu_phase(g):
                    ug = ensure(("g", g))
                    uu = ensure(("u", g))
                    at = AT[g % 2]
                    for j in range(2):
                        pg = PS[j % 2][:, 0:S0]
                        pu = PS[2 + j % 2][:, 0:S0]
                        for kc in range(KC):
                            P.mm(pg, ug[:, kc, j * 128:(j + 1) * 128], HG[:, kc, :],
                                 start=(kc == 0), stop=(kc == KC - 1), inc=(kc == KC - 1))
                        for kc in range(KC):
                            P.mm(pu, uu[:, kc, j * 128:(j + 1) * 128], HG[:, kc, :],
                                 start=(kc == 0), stop=(kc == KC - 1), inc=(kc == KC - 1))
                        sg = SCR[j % 2][:, 0:S0]
                        P.act(sg, pg, AF.Silu)
                        P.tt(at[:, j, :], pu, sg, ALU.mult)

                def dn_pair(g):
                    uds = [ensure(("d", g)), ensure(("d", g + 1))]
                    k = 0
                    for sc in range(NSC):
                        for dq in range(4):
                            pd = PS[4 + k % 2][:, 0:512]
                            for i in range(4):
                                at, ud, j = AT[(g + i // 2) % 2], uds[i // 2], i % 2
                                P.mm(pd, at[:, j, sc * 128:(sc + 1) * 128], ud[:, j, dq * 512:(dq + 1) * 512],
                                     start=(i == 0), stop=(i == 3), inc=(i == 3))
                            oa = OACC[:, sc, dq * 512:(dq + 1) * 512]
                            if g == 0:
                                P.copy(oa, pd)
                            else:
                                P.tt(oa, pd, oa, ALU.add)
                            k += 1

                for g in range(0, NG, 2):
                    gu_phase(g)
                    gu_phase(g + 1)
                    dn_pair(g)
                for sc in range(NSC):
                    P.ts(OBF[:, sc, :], OACC[:, sc, :], GS[:, sc:sc + 1], ALU.mult)
                k = 0
                for blk in range(NB):
                    for tl in range(3):
                        t = 3 * blk + tl
                        lb = SCR2[tl % 2][:, 0:128]
                        P.ts(lb, ONES.all(), PM[:, t:t + 1], ALU.mult)
                        P.mm(PS[7][:, tl * 128:(tl + 1) * 128], lb, IDENT.all(), start=True, stop=True, inc=True)
                    prow = SCR[blk % 2][:, 0:BLK]
                    P.copy(prow, PS[7][:, 0:BLK])
                    selt = SELT[blk % 2]
                    for sc in range(NSC):
                        P.ts(selt[:, sc, :], prow, IOTA_COL[:, sc:sc + 1], ALU.is_equal)
                    for co in range(KC):
                        ps = PS[k % 2][:, 0:BLK]
                        for sc in range(NSC):
                            P.mm(ps, OBF[:, sc, co * 128:(co + 1) * 128], selt[:, sc, :], start=(sc == 0),
                                 stop=(sc == NSC - 1), inc=(sc == NSC - 1))
                        xs = XT[:, co, blk * BLK:(blk + 1) * BLK]
                        P.tt(xs, ps, xs, ALU.add)
                        k += 1

            npass_emit = int(os.environ.get("MK_NPASS", str(NPASS)))
            for e_i in range(n_exp):
                for p in range(min(NPASS, npass_emit)):
                    if p > 0:
                        fi = (p - 1) * NE + e_i
                        P.region_begin(["pe", "act", "dve", "pool"], FLGI[0:1, fi:fi + 1])
                    expert_pass(e_i, p)
                    if p > 0:
                        P.region_end()

        def final_stage():
            cur[0] = stage_off
            YO = [alloc([KC, BLK], F32) for _ in range(2)]
            for blk in range(NB):
                t0 = blk * BLK
                yo = YO[blk % 2]
                rms(lambda kc: XT[:, kc, t0:t0 + BLK], BLK, V_NFIN,
                    lambda kc: yo[:, kc, :], RSTD[:, t0:t0 + BLK])
                for q4 in range(4):
                    P.dma("sp", o_yT[512 * q4:512 * (q4 + 1), t0:t0 + BLK].rearrange("(k p) t -> p k t", p=128),
                          yo[:, 4 * q4:4 * q4 + 4, :], is_output=True)

        if "conv" in stages:
            conv_stage()
        if "ffn" in stages:
            ffn_stage()
        if "gmlp" in stages:
            gmlp_stage()
        if "moe" in stages:
            if os.environ.get("MK_MOE", "routed") == "dense":
                moe_stage()
            else:
                moe_routed_stage(int(os.environ.get("MK_S0", "384")))
        final_stage()
        P.finish()

        with nc.Block() as block:
            @block.tensor
            def _(e):
                replay(e, P.q["pe"])

            @block.scalar
            def _(e):
                replay(e, P.q["act"])

            @block.vector
            def _(e):
                replay(e, P.q["dve"])

            @block.gpsimd
            def _(e):
                replay(e, P.q["pool"])

            @block.sync
            def _(e):
                replay(e, P.q["sp"])
        stats = {k: len(v) for k, v in P.q.items()}
        stats["waits"] = P.n_wait
    return nc, stats


def _pack_cols(vec):
    return np.ascontiguousarray(np.asarray(vec, np.float32).reshape(KC, 128).T)


def kernel(x_prompt, x_sample, state_conv, norm_mix, norm_ffn, norm_final,
           conv_w_in, conv_b_in, conv_w_dw, conv_b_dw, conv_ln_g, conv_ln_b, conv_w_out, conv_b_out,
           gmlp_w_in, gmlp_b_in, gmlp_ln_g, gmlp_ln_b, gmlp_w_s, gmlp_b_s, gmlp_w_out, gmlp_b_out,
           ffn_w_gate, ffn_w_up, ffn_w_down, moe_router, moe_w_gate, moe_w_up, moe_w_down):
    stages = os.environ.get("MK_STAGES", "conv,ffn,gmlp,moe").split(",")
    n_exp = int(os.environ.get("MK_NEXP", str(NE)))
    f = np.float32
    x_prompt = np.asarray(x_prompt, f)
    x_sample = np.asarray(x_sample, f)
    state_conv = np.asarray(state_conv, f)

    cols = [norm_mix[0], norm_mix[1], norm_ffn[0], norm_ffn[1], norm_final,
            conv_b_in[0, :D], conv_b_in[0, D:], conv_b_dw[0], conv_ln_g[0], conv_ln_b[0], conv_b_out[0],
            gmlp_b_in[0, :D], gmlp_b_out[0]]
    cols += [conv_w_dw[0, j] for j in range(NTAP)]
    vecs = np.ascontiguousarray(np.concatenate([_pack_cols(c) for c in cols], axis=1))
    assert vecs.shape == (128, NV)
    rows = np.zeros((1, NR), f)
    rows[0, R_GBV:R_GBV + D] = np.asarray(gmlp_b_in, f)[0, D:]
    rows[0, R_LNG:R_LNG + D] = np.asarray(gmlp_ln_g, f)[0]
    rows[0, R_LNB:R_LNB + D] = np.asarray(gmlp_ln_b, f)[0]
    bs = np.asarray(gmlp_b_s, f)[0]
    rows[0, R_BSP:R_BSP + 1024] = bs.reshape(-1)
    rows[0, R_BSS:R_BSS + 1024] = np.tile(bs[:, :DSEQ], (1, NSEQ)).reshape(-1)
    ws = np.asarray(gmlp_w_s, f)[0]
    wsp = np.ascontiguousarray(ws.transpose(2, 0, 1)).reshape(128, 1024)
    ws8 = ws[:, :DSEQ, :DSEQ].transpose(2, 0, 1)
    wss = np.ascontiguousarray(np.tile(ws8, (NSEQ, 1, NSEQ))).reshape(128, 1024)
    ii = np.arange(128)
    mtril = (ii[:, None] <= ii[None, :]).astype(f)
    mbd = mtril * ((ii[:, None] // DSEQ) == (ii[None, :] // DSEQ)).astype(f)
    masks = np.ascontiguousarray(np.concatenate([mtril, mbd], axis=1))
    ident = np.eye(128, dtype=f)
    iotas = np.zeros((128, 516), f)
    iotas[:, :512] = np.arange(512, dtype=f)[None, :]
    iotas[:, 512:516] = ii[:, None].astype(f) + 128.0 * np.arange(4, dtype=f)[None, :]
    ut = (ii[:, None] < ii[None, :]).astype(f)
    shared = {
        "iotas": iotas, "ut": ut,
        "vecs": vecs, "rows": rows, "wsp": wsp, "wss": wss, "masks": masks, "ident": ident,
        "router": np.ascontiguousarray(np.asarray(moe_router, f)[0]),
        "conv_w_in": np.asarray(conv_w_in, f)[0], "conv_w_out": np.asarray(conv_w_out, f)[0],
        "gmlp_w_in": np.asarray(gmlp_w_in, f)[0], "gmlp_w_out": np.asarray(gmlp_w_out, f)[0],
        "ffn_wg": np.asarray(ffn_w_gate, f)[0], "ffn_wu": np.asarray(ffn_w_up, f)[0],
        "ffn_wd": np.asarray(ffn_w_down, f)[0],
        "moe_wg": np.asarray(moe_w_gate, f).reshape(NE * D, FF),
        "moe_wu": np.asarray(moe_w_up, f).reshape(NE * D, FF),
        "moe_wd": np.asarray(moe_w_down, f).reshape(NE * FF, D),
    }
    in_maps = []
    for c in range(NCORES):
        b, half = c // 2, c % 2
        xp = x_prompt[b, half * 1024:(half + 1) * 1024]
        xs = x_sample[c * NSEQ:(c + 1) * NSEQ].reshape(NSEQ * DSEQ, D)
        xT = np.ascontiguousarray(np.concatenate([xp, xs], axis=0).T)
        if half == 1:
            xh = np.ascontiguousarray(x_prompt[b, 1024 - HALO:1024].T)
        else:
            xh = np.zeros((D, HALO), f)
        hmask = np.full((128, 1), float(half), f)
        sc = state_conv[0, c * NSEQ:(c + 1) * NSEQ]
        sconv = np.ascontiguousarray(sc.transpose(2, 0, 1)).reshape(D, NSEQ * HALO)
        m = {"xT": xT, "xh": xh, "hmask": hmask, "sconv": sconv}
        m.update(shared)
        in_maps.append(m)

    nc, _ = build_program(stages, n_exp)
    res = run_bass_kernel_spmd(nc, in_maps, core_ids=list(range(NCORES)))
    outs = res.results

    y_prompt = np.empty((4, 2048, D), f)
    y_sample = np.empty((128, DSEQ, D), f)
    ncp = np.empty((1, 4, HALO, D), f)
    ncs = np.empty((1, 128, HALO, D), f)
    vout = np.empty((1, 128, DSEQ, D), f)
    for c in range(NCORES):
        b, half = c // 2, c % 2
        yT = np.asarray(outs[c]["yT"])
        y_prompt[b, half * 1024:(half + 1) * 1024] = yT[:, :1024].T
        y_sample[c * NSEQ:(c + 1) * NSEQ] = yT[:, 1024:].T.reshape(NSEQ, DSEQ, D)
        if half == 1:
            ncp[0, b] = np.asarray(outs[c]["ncp"]).T
        ncs[0, c * NSEQ:(c + 1) * NSEQ] = np.asarray(outs[c]["ncs"]).reshape(D, NSEQ, HALO).transpose(1, 2, 0)
        vout[0, c * NSEQ:(c + 1) * NSEQ] = np.asarray(outs[c]["vout"]).reshape(NSEQ, DSEQ, D)
    return (y_prompt, y_sample, ncp, ncs, vout)
```

```python
import os
import numpy as np
import concourse.bass as bass
import concourse.mybir as mybir
from concourse.bass_utils import run_bass_kernel_spmd

F32 = mybir.dt.float32
BF16 = mybir.dt.bfloat16
ALU = mybir.AluOpType
AF = mybir.ActivationFunctionType

NCORES = 8
D = 2048
KC = 16
NT = 1152
BLK = 384
NB = 3
FF = 7168
FC = 56
NE = 8
HALO = 30
NTAP = 31
NSEQ = 16
DSEQ = 8
EPS_RMS = 1e-6
EPS_LN = 1e-5

V_NM0, V_NM1, V_NF0, V_NF1, V_NFIN, V_CBA, V_CBG, V_CBDW, V_CLNG, V_CLNB, V_CBOUT, V_GBU, V_GBOUT = [
    16 * i for i in range(13)]
V_CWDW = 16 * 13
NV = 16 * 13 + NTAP * 16
R_GBV, R_LNG, R_LNB, R_BSP, R_BSS = 0, 2048, 4096, 6144, 7168
NR = 8192

ARENA_BYTES = 212800
I32 = mybir.dt.int32


def _prod(s):
    r = 1
    for x in s:
        r *= x
    return r


class V:
    __slots__ = ("ap", "mem", "lo", "hi")

    def __init__(self, ap, mem, lo, hi):
        self.ap, self.mem, self.lo, self.hi = ap, mem, lo, hi


class Reg:
    def __init__(self, base_ap, mem, off, shape, dtype):
        self.mem = mem
        self.off = off
        self.shape = tuple(shape)
        self.esz = 2 if dtype == BF16 else 4
        n = _prod(shape)
        self.nbytes = n * self.esz
        assert off % 4 == 0 and self.nbytes % 4 == 0
        ap = base_ap[:, off // 4:(off + self.nbytes) // 4]
        if dtype != F32:
            ap = ap.bitcast(dtype)
        if len(shape) == 2:
            ap = ap.rearrange("p (a b) -> p a b", a=shape[0])
        elif len(shape) == 3:
            ap = ap.rearrange("p (a b c) -> p a b c", a=shape[0], b=shape[1])
        self.ap = ap
        self.strides = [_prod(shape[i + 1:]) for i in range(len(shape))]

    def __getitem__(self, idx):
        if not isinstance(idx, tuple):
            idx = (idx,)
        ap = self.ap[idx]
        lo = 0
        hi = 0
        for d, (sz, st) in enumerate(zip(self.shape, self.strides)):
            ix = idx[d + 1] if d + 1 < len(idx) else slice(None)
            if isinstance(ix, int):
                a, b = ix, ix + 1
            else:
                a = ix.start or 0
                b = sz if ix.stop is None else ix.stop
            lo += a * st
            hi += (b - 1) * st
        hi += 1
        return V(ap, self.mem, self.off + lo * self.esz, self.off + hi * self.esz)

    def all(self):
        return self[(slice(None),)]


class Prog:
    ENGS = ("pe", "act", "dve", "pool", "sp")

    def __init__(self, nc, sems):
        self.nc = nc
        self.sems = sems
        self.q = {e: [] for e in self.ENGS}
        self.cnt = {"pe": 0, "act": 0, "dve": 0}
        self.dcnt = {}
        self.known = {e: {} for e in self.ENGS}
        self.pe_idx = 0
        self.pe_miles_idx = []
        self.pe_miles_cnt = []
        self.recs = {"sb": [], "ps": []}
        self.out_events = {}
        self.sp_rr = 0
        self.n_wait = 0

    def _resolve(self, key, val):
        if key == "PE#":
            import bisect
            i = bisect.bisect_left(self.pe_miles_idx, val)
            if i >= len(self.pe_miles_idx):
                raise RuntimeError("PE read/write without a later milestone (idx %d)" % val)
            return "pe", self.pe_miles_cnt[i]
        return key, val

    def wait(self, eng, key, val):
        if key == "PE#" and eng == "pe":
            return
        key, val = self._resolve(key, val)
        if key == "pe" and eng == "pe":
            return
        if self.known[eng].get(key, 0) >= val:
            return
        self.known[eng][key] = val
        h = self.sems[key]
        self.q[eng].append(lambda e, h=h, v=val: e.wait_ge(h, v))
        self.n_wait += 1

    def _deps(self, eng, outs, ins):
        need = {}

        def add(evd):
            for k, v in evd.items():
                if need.get(k, -1) < v:
                    need[k] = v
        for v in ins:
            for r in self.recs[v.mem]:
                if r[0] < v.hi and v.lo < r[1]:
                    add(r[2])
        for v in outs:
            for r in self.recs[v.mem]:
                if r[0] < v.hi and v.lo < r[1]:
                    add(r[2])
                    add(r[3])
        for k, val in need.items():
            self.wait(eng, k, val)

    def _record(self, outs, ins, key, val):
        for v in ins:
            hit = False
            for r in self.recs[v.mem]:
                if r[0] < v.hi and v.lo < r[1]:
                    if r[3].get(key, -1) < val:
                        r[3][key] = val
                    if r[0] <= v.lo and v.hi <= r[1]:
                        hit = True
            if not hit:
                self.recs[v.mem].append([v.lo, v.hi, {}, {key: val}])
        for v in outs:
            lst = self.recs[v.mem]
            lst[:] = [r for r in lst if not (v.lo <= r[0] and r[1] <= v.hi)]
            lst.append([v.lo, v.hi, {key: val}, {}])

    def op(self, eng, fn, outs, ins):
        self._deps(eng, outs, ins)
        self.cnt[eng] += 1
        val = self.cnt[eng]
        h = self.sems[eng]
        self.q[eng].append(lambda e, fn=fn, h=h: fn(e).then_inc(h, 1))
        self.known[eng][eng] = max(self.known[eng].get(eng, 0), 0)
        self._record(outs, ins, eng, val)

    def mm(self, out, lhsT, rhs, start, stop, inc=False):
        self._deps("pe", [out], [lhsT, rhs])
        idx = self.pe_idx
        self.pe_idx += 1
        o, l, r = out.ap, lhsT.ap, rhs.ap
        if inc:
            self.cnt["pe"] += 1
            self.pe_miles_idx.append(idx)
            self.pe_miles_cnt.append(self.cnt["pe"])
            h = self.sems["pe"]
            self.q["pe"].append(lambda e, o=o, l=l, r=r, s=start, t=stop, h=h:
                                e.matmul(o, l, r, start=s, stop=t).then_inc(h, 1))
        else:
            self.q["pe"].append(lambda e, o=o, l=l, r=r, s=start, t=stop:
                                e.matmul(o, l, r, start=s, stop=t))
        self._record([out], [lhsT, rhs], "PE#", idx)

    def dma(self, queue, out, in_, sem=None, is_output=False):
        outs = [out] if isinstance(out, V) else []
        ins = [in_] if isinstance(in_, V) else []
        if sem is None:
            sem = "S%d" % (self.sp_rr % 8)
            self.sp_rr += 1
        prev = self.dcnt.get(sem, 0)
        if prev:
            self.wait(queue, sem, prev)
        self._deps(queue, outs, ins)
        val = prev + 16
        self.dcnt[sem] = val
        h = self.sems[sem]
        oa = out.ap if isinstance(out, V) else out
        ia = in_.ap if isinstance(in_, V) else in_
        self.q[queue].append(lambda e, oa=oa, ia=ia, h=h: e.dma_start(out=oa, in_=ia).then_inc(h, 16))
        self._record(outs, ins, sem, val)
        if is_output:
            self.out_events[sem] = val
        return sem, val

    def act(self, out, in_, func, bias=None, scale=None):
        ins = [in_]
        kw = {}
        if bias is not None:
            if isinstance(bias, V):
                ins.append(bias)
                kw["bias"] = bias.ap
            else:
                kw["bias"] = bias
        if scale is not None:
            if isinstance(scale, V):
                ins.append(scale)
                kw["scale"] = scale.ap
            else:
                kw["scale"] = scale
        o, i = out.ap, in_.ap
        self.op("act", lambda e: e.activation(out=o, in_=i, func=func, **kw), [out], ins)

    def tt(self, out, in0, in1, op):
        o, a, b = out.ap, in0.ap, in1.ap
        self.op("dve", lambda e: e.tensor_tensor(out=o, in0=a, in1=b, op=op), [out], [in0, in1])

    def ts(self, out, in0, s1, op0, s2=None, op1=None):
        ins = [in0]
        a1 = s1
        if isinstance(s1, V):
            ins.append(s1)
            a1 = s1.ap
        a2 = s2
        if isinstance(s2, V):
            ins.append(s2)
            a2 = s2.ap
        o, a = out.ap, in0.ap
        if op1 is None:
            self.op("dve", lambda e: e.tensor_single_scalar(out=o, in_=a, scalar=a1, op=op0), [out], ins)
        else:
            self.op("dve", lambda e: e.tensor_scalar(out=o, in0=a, scalar1=a1, scalar2=a2, op0=op0, op1=op1),
                    [out], ins)

    def stt(self, out, in0, scalar, in1, op0, op1):
        ins = [in0, in1]
        sc = scalar
        if isinstance(scalar, V):
            ins.append(scalar)
            sc = scalar.ap
        o, a, b = out.ap, in0.ap, in1.ap
        self.op("dve", lambda e: e.scalar_tensor_tensor(out=o, in0=a, scalar=sc, in1=b, op0=op0, op1=op1),
                [out], ins)

    def copy(self, out, in_):
        o, a = out.ap, in_.ap
        self.op("dve", lambda e: e.tensor_copy(out=o, in_=a), [out], [in_])

    def memset(self, out, val):
        o = out.ap
        self.op("dve", lambda e: e.memset(o, val), [out], [])

    def finish(self):
        for sem, val in self.out_events.items():
            self.wait("sp", sem, val)

    def region_begin(self, engines, flag_view):
        self._region = dict(engines=engines, known={e: dict(self.known[e]) for e in engines},
                            cnt0=dict(self.cnt), dcnt0=dict(self.dcnt))
        for e in engines:
            self._deps(e, [], [flag_view])
            self.q[e].append(("if", flag_view.ap))

    def region_end(self):
        r = self._region
        self._region = None
        for e in r["engines"]:
            comp = []
            if e in self.cnt:
                m0, m1 = r["cnt0"][e], self.cnt[e]
                if m1 > m0:
                    comp.append((self.sems[e], m0, m1 - m0))
            if e == "pool":
                for sname, v1 in self.dcnt.items():
                    v0 = r["dcnt0"].get(sname, 0)
                    if sname.startswith("R") and v1 > v0:
                        comp.append((self.sems[sname], v0, v1 - v0))
            self.q[e].append(("else", comp))
            self.q[e].append(("endif",))
            self.known[e] = r["known"][e]


def replay(e, items):
    stack = []
    rguard = e.register("flag")
    reg = rguard.__enter__()
    for it in items:
        if isinstance(it, tuple):
            if it[0] == "if":
                e.reg_load(reg, it[1])
                g = e.If_ne(reg, 0)
                g.__enter__()
                stack.append(g)
            elif it[0] == "else":
                stack.pop().__exit__(None, None, None)
                g = e.Else()
                g.__enter__()
                stack.append(g)
                for semh, v0, n in it[1]:
                    if v0 > 0:
                        e.wait_ge(semh, v0)
                    e.sem_inc(semh, n)
            else:
                stack.pop().__exit__(None, None, None)
        else:
            it(e)
    rguard.__exit__(None, None, None)


def build_program(stages, n_exp):
    nc = bass.Bass("TRN2", target_bir_lowering=False)

    def din(name, shape):
        return nc.dram_tensor(name, list(shape), F32, kind="ExternalInput").ap()

    def dout(name, shape):
        return nc.dram_tensor(name, list(shape), F32, kind="ExternalOutput").ap()

    d_xT = din("xT", [D, NT])
    d_xh = din("xh", [D, HALO])
    d_hmask = din("hmask", [128, 1])
    d_sconv = din("sconv", [D, NSEQ * HALO])
    d_vecs = din("vecs", [128, NV])
    d_rows = din("rows", [1, NR])
    d_wsp = din("wsp", [128, 8 * 128])
    d_wss = din("wss", [128, 8 * 128])
    d_masks = din("masks", [128, 2 * 128])
    d_ident = din("ident", [128, 128])
    d_router = din("router", [D, NE])
    d_iotas = din("iotas", [128, 512 + 4])
    d_ut = din("ut", [128, 128])
    d_cwin = din("conv_w_in", [D, 2 * D])
    d_cwout = din("conv_w_out", [D, D])
    d_gwin = din("gmlp_w_in", [D, 2 * D])
    d_gwout = din("gmlp_w_out", [D, D])
    d_fwg = din("ffn_wg", [D, FF])
    d_fwu = din("ffn_wu", [D, FF])
    d_fwd = din("ffn_wd", [FF, D])
    d_mwg = din("moe_wg", [NE * D, FF])
    d_mwu = din("moe_wu", [NE * D, FF])
    d_mwd = din("moe_wd", [NE * FF, D])

    d_htok = nc.dram_tensor("htok_scr", [9, 128, D], BF16).ap()
    o_yT = dout("yT", [D, NT])
    o_ncp = dout("ncp", [D, HALO])
    o_ncs = dout("ncs", [D, NSEQ * HALO])
    o_vout = dout("vout", [128, D])

    sem_names = ["pe", "act", "dve"] + ["R%d" % i for i in range(6)] + ["S%d" % i for i in range(8)]

    import contextlib
    with contextlib.ExitStack() as es:
        arena = es.enter_context(nc.sbuf_tensor("arena", [128, ARENA_BYTES // 4], F32))
        psum = es.enter_context(nc.psum_tensor("psum", [128, 8 * 512], F32))
        sems = {n: es.enter_context(nc.semaphore("sem_" + n)) for n in sem_names}
        P = Prog(nc, sems)

        cur = [0]

        def alloc(shape, dtype, at=None):
            esz = 4 if dtype == F32 else 2
            nb = (_prod(shape) * esz + 3) // 4 * 4
            if at is None:
                off = cur[0]
                cur[0] += nb
            else:
                off = at
            assert off + nb <= ARENA_BYTES, (off, nb)
            return Reg(arena, "sb", off, shape, dtype)

        PS = [Reg(psum, "ps", b * 2048, [512], F32) for b in range(8)]

        XT = alloc([KC, NT], F32)
        ring_off = cur[0]
        cur[0] += 6 * 8192
        VECS = alloc([NV], F32)
        RSTD = alloc([NT], F32)
        SCR = [alloc([416], F32) for _ in range(2)]
        SCR2 = [alloc([416], F32) for _ in range(2)]
        ONES = alloc([128], F32)
        IDENT = alloc([128], F32)
        MISC = alloc([64], F32)
        HALOS = alloc([KC, HALO], F32)
        stage_off = cur[0]
        STAGE_BYTES = ARENA_BYTES - stage_off

        def ring_gu(slot):
            return Reg(arena, "sb", ring_off + slot * 8192, [KC, 256], BF16)

        def ring_dn(slot):
            return Reg(arena, "sb", ring_off + slot * 8192, [2, D], BF16)

        HMASK = MISC[:, 0:1]
        EPSR = MISC[:, 1:2]
        EPSL = MISC[:, 2:3]

        def vcol(base, kc):
            return VECS[:, base + kc:base + kc + 1]

        for q4 in range(4):
            P.dma("sp", XT[:, 4 * q4:4 * q4 + 4, :],
                  d_xT[512 * q4:512 * (q4 + 1), :].rearrange("(k p) t -> p k t", p=128))
        P.dma("sp", VECS.all(), d_vecs)
        P.dma("sp", IDENT.all(), d_ident)
        P.dma("sp", MISC[:, 0:1], d_hmask)
        P.memset(ONES.all(), 1.0)
        P.memset(MISC[:, 1:2], EPS_RMS)
        P.memset(MISC[:, 2:3], EPS_LN)

        wcount = [0]

        def load_gu(w_ap, row0, col0, nslots, slot_base=0):
            slot = slot_base + wcount[0] % nslots
            wcount[0] += 1
            r = ring_gu(slot)
            src = w_ap[row0:row0 + D, col0:col0 + 256].rearrange("(k p) c -> p k c", p=128)
            P.dma("pool", r.all(), src, sem="R%d" % slot)
            return r

        def load_dn(w_ap, row0, nslots):
            slot = wcount[0] % nslots
            wcount[0] += 1
            r = ring_dn(slot)
            src = w_ap[row0:row0 + 256, :].rearrange("(j p) c -> p j c", p=128)
            P.dma("pool", r.all(), src, sem="R%d" % slot)
            return r

        def rms(xv, n, gbase, hv, rstd, out_f32=False):
            ps = PS[6][:, 0:n]
            for kc in range(KC):
                s = SCR[kc % 2][:, 0:n]
                P.act(s, xv(kc), AF.Square)
                P.mm(ps, ONES.all(), s, start=(kc == 0), stop=(kc == KC - 1), inc=True)
            P.act(rstd, ps, AF.Sqrt, bias=EPSR, scale=1.0 / D)
            o, a = rstd.ap, rstd.ap
            P.op("dve", lambda e: e.reciprocal(out=o, in_=a), [rstd], [rstd])
            for kc in range(KC):
                P.stt(hv(kc), xv(kc), vcol(gbase, kc), rstd, ALU.mult, ALU.mult)

        def out_proj(w_ap, sin, n, t0, bbase, nslots):
            unit = None
            for co in range(KC):
                if co % 2 == 0:
                    unit = load_gu(w_ap, 0, (co // 2) * 256, nslots)
                j = co % 2
                po = PS[4 + co % 2][:, 0:n]
                for kc in range(KC):
                    P.mm(po, unit[:, kc, j * 128:(j + 1) * 128], sin(kc), start=(kc == 0),
                         stop=(kc == KC - 1), inc=(kc == KC - 1))
                xs = XT[:, co, t0:t0 + n]
                P.stt(xs, po, vcol(bbase, co), xs, ALU.add, ALU.add)

        def conv_stage():
            cur[0] = stage_off
            conv_pe = os.environ.get("MK_CONVPE", "1") == "1"
            HP = alloc([KC, 416], BF16)
            Y = alloc([KC, BLK], F32)
            ST = alloc([KC, BLK], BF16)
            GLU = [alloc([416], F32) for _ in range(2)]
            SCB = [alloc([NSEQ, HALO + DSEQ], F32) for _ in range(2)]
            XH = alloc([KC, 32], F32)
            MEAN = alloc([BLK], F32)
            RS = alloc([BLK], F32)
            MSQ = alloc([BLK], F32)
            RH = alloc([32], F32)
            P.dma("sp", XH[:, :, 0:HALO], d_xh.rearrange("(k p) t -> p k t", p=128))
            GLUB = [alloc([416], BF16) for _ in range(2)]
            DG = [Reg(arena, "sb", ring_off + (4 + i) * 8192, [NTAP, 128], BF16) for i in range(2)]
            ident_b = V(IDENT.ap[:, None, :].broadcast_to([128, NTAP, 128]), "sb", IDENT.off,
                        IDENT.off + IDENT.nbytes)

            def conv_taps(c, nprompt):
                dg, glub = DG[c % 2], GLUB[c % 2]
                pc = PS[4 + c % 2][:, 0:nprompt]
                for j2 in range(NTAP):
                    P.mm(pc, dg[:, j2, :], glub[:, j2:j2 + nprompt], start=(j2 == 0), stop=(j2 == NTAP - 1),
                         inc=(j2 == NTAP - 1))
                P.ts(Y[:, c, 0:nprompt], pc, vcol(V_CBDW, c), ALU.add)

            for b in range(NB):
                t0 = b * BLK
                c0 = 0 if b == 0 else HALO
                nprompt = BLK if b < 2 else 256
                rms(lambda kc: XT[:, kc, t0:t0 + BLK], BLK, V_NM0,
                    lambda kc: HP[:, kc, HALO:HALO + BLK], RSTD[:, t0:t0 + BLK])
                if b == 0:
                    rms(lambda kc: XH[:, kc, 0:HALO], HALO, V_NM0,
                        lambda kc: HP[:, kc, 0:HALO], RH[:, 0:HALO])
                ua = ug = None
                for c in range(KC):
                    if c % 2 == 0:
                        ua = load_gu(d_cwin, 0, (c // 2) * 256, 4)
                        ug = load_gu(d_cwin, 0, D + (c // 2) * 256, 4)
                    j = c % 2
                    n = HALO + BLK - c0
                    pa = PS[c % 2][:, c0:c0 + n]
                    pg = PS[2 + c % 2][:, c0:c0 + n]
                    for kc in range(KC):
                        P.mm(pa, ua[:, kc, j * 128:(j + 1) * 128], HP[:, kc, c0:c0 + n],
                             start=(kc == 0), stop=(kc == KC - 1), inc=(kc == KC - 1))
                    for kc in range(KC):
                        P.mm(pg, ug[:, kc, j * 128:(j + 1) * 128], HP[:, kc, c0:c0 + n],
                             start=(kc == 0), stop=(kc == KC - 1), inc=(kc == KC - 1))
                    sg = SCR[c % 2][:, c0:c0 + n]
                    P.act(sg, pg, AF.Sigmoid, bias=vcol(V_CBG, c))
                    glu = GLU[c % 2]
                    P.stt(glu[:, c0:c0 + n], pa, vcol(V_CBA, c), sg, ALU.add, ALU.mult)
                    if b == 0:
                        P.ts(glu[:, 0:HALO], glu[:, 0:HALO], HMASK, ALU.mult)
                    else:
                        P.copy(glu[:, 0:HALO], HALOS[:, c, :])
                    if conv_pe:
                        P.copy(GLUB[c % 2][:, 0:nprompt + HALO], glu[:, 0:nprompt + HALO])
                        wt = VECS.ap[:, V_CWDW:V_CWDW + 16 * NTAP].rearrange("p (j c) -> p j c", c=16)[:, :, c]
                        wt_b = V(wt[:, :, None].broadcast_to([128, NTAP, 128]), "sb", VECS.off + V_CWDW * 4,
                                 VECS.off + (V_CWDW + 16 * NTAP) * 4)
                        P.tt(DG[c % 2].all(), ident_b, wt_b, ALU.mult)
                    else:
                        acc = Y[:, c, 0:nprompt]
                        P.ts(acc, glu[:, 0:nprompt], vcol(V_CWDW + 0, c), ALU.mult, vcol(V_CBDW, c), ALU.add)
                        for j2 in range(1, NTAP):
                            P.stt(acc, glu[:, j2:j2 + nprompt], vcol(V_CWDW + 16 * j2, c), acc, ALU.mult, ALU.add)
                    P.copy(HALOS[:, c, :], glu[:, nprompt:nprompt + HALO])
                    if b == 2:
                        scb = SCB[c % 2]
                        P.dma("sp", scb[:, :, 0:HALO],
                              d_sconv[c * 128:(c + 1) * 128, :].rearrange("p (s r) -> p s r", s=NSEQ))
                        gs = V(glu.ap[:, HALO + 256:HALO + 384].rearrange("p (s r) -> p s r", s=NSEQ), "sb",
                               glu.off + (HALO + 256) * 4, glu.off + (HALO + 384) * 4)
                        P.copy(scb[:, :, HALO:HALO + DSEQ], gs)
                        accs = V(Y.ap[:, c, 256:384].rearrange("p (s r) -> p s r", s=NSEQ), "sb",
                                 Y.off + (c * BLK + 256) * 4, Y.off + (c * BLK + 384) * 4)
                        P.ts(accs, scb[:, :, 0:DSEQ], vcol(V_CWDW + 0, c), ALU.mult, vcol(V_CBDW, c), ALU.add)
                        for j2 in range(1, NTAP):
                            P.stt(accs, scb[:, :, j2:j2 + DSEQ], vcol(V_CWDW + 16 * j2, c), accs,
                                  ALU.mult, ALU.add)
                        P.dma("sp", o_ncs[c * 128:(c + 1) * 128, :].rearrange("p (s r) -> p s r", s=NSEQ),
                              scb[:, :, DSEQ:DSEQ + HALO], is_output=True)
                    if conv_pe and c >= 1:
                        conv_taps(c - 1, nprompt)
                if conv_pe:
                    conv_taps(KC - 1, nprompt)
                if b == 2:
                    P.dma("sp", o_ncp.rearrange("(k p) r -> p k r", p=128), HALOS.all(), is_output=True)
                s1 = PS[6][:, 0:BLK]
                s2 = PS[7][:, 0:BLK]
                for c in range(KC):
                    P.mm(s1, ONES.all(), Y[:, c, :], start=(c == 0), stop=(c == KC - 1), inc=True)
                    sq = SCR[c % 2][:, 0:BLK]
                    P.act(sq, Y[:, c, :], AF.Square)
                    P.mm(s2, ONES.all(), sq, start=(c == 0), stop=(c == KC - 1), inc=True)
                P.ts(MEAN.all(), s1, 1.0 / D, ALU.mult)
                P.tt(MSQ.all(), MEAN.all(), MEAN.all(), ALU.mult)
                P.stt(MSQ.all(), s2, 1.0 / D, MSQ.all(), ALU.mult, ALU.subtract)
                P.act(RS.all(), MSQ.all(), AF.Sqrt, bias=EPSL, scale=1.0)
                o, a = RS.all().ap, RS.all().ap
                P.op("dve", lambda e: e.reciprocal(out=o, in_=a), [RS.all()], [RS.all()])
                for c in range(KC):
                    P.tt(Y[:, c, :], Y[:, c, :], MEAN.all(), ALU.subtract)
                    P.tt(Y[:, c, :], Y[:, c, :], RS.all(), ALU.mult)
                    P.act(ST[:, c, :], Y[:, c, :], AF.Silu, bias=vcol(V_CLNB, c), scale=vcol(V_CLNG, c))
                out_proj(d_cwout, lambda kc: ST[:, kc, :], BLK, t0, V_CBOUT, 4)

        def ffn_pass(HT, AT, wg, wu, wd, grow0, drow0, CB=None):
            NG = FC // 2
            units = {}

            def gu_phase(g):
                ug = load_gu(wg, grow0, g * 256, 6)
                uu = load_gu(wu, grow0, g * 256, 6)
                if g % 2 == 1:
                    units[g - 1] = load_dn(wd, drow0 + (g - 1) * 256, 6)
                    units[g] = load_dn(wd, drow0 + g * 256, 6)
                at = AT[g % 2]
                k = 0
                for j in range(2):
                    for blk in range(NB):
                        pg = PS[k % 2][:, 0:BLK]
                        pu = PS[2 + k % 2][:, 0:BLK]
                        for kc in range(KC):
                            P.mm(pg, ug[:, kc, j * 128:(j + 1) * 128], HT[:, kc, blk * BLK:(blk + 1) * BLK],
                                 start=(kc == 0), stop=(kc == KC - 1), inc=(kc == KC - 1))
                        for kc in range(KC):
                            P.mm(pu, uu[:, kc, j * 128:(j + 1) * 128], HT[:, kc, blk * BLK:(blk + 1) * BLK],
                                 start=(kc == 0), stop=(kc == KC - 1), inc=(kc == KC - 1))
                        sg = SCR[k % 2][:, 0:BLK]
                        P.act(sg, pg, AF.Silu)
                        a_out = at[:, j, blk * BLK:(blk + 1) * BLK]
                        if CB is None:
                            P.tt(a_out, pu, sg, ALU.mult)
                        else:
                            t2 = SCR2[k % 2][:, 0:BLK]
                            P.tt(t2, pu, sg, ALU.mult)
                            P.tt(a_out, t2, CB[:, blk * BLK:(blk + 1) * BLK], ALU.mult)
                        k += 1

            def dn_pair(g):
                uds = [units.pop(g), units.pop(g + 1)]
                k = 0
                for co in range(KC):
                    for blk in range(NB):
                        pd = PS[4 + k % 2][:, 0:BLK]
                        for i in range(4):
                            at, ud, j = AT[(g + i // 2) % 2], uds[i // 2], i % 2
                            P.mm(pd, ud[:, j, co * 128:(co + 1) * 128], at[:, j, blk * BLK:(blk + 1) * BLK],
                                 start=(i == 0), stop=(i == 3), inc=(i == 3))
                        xs = XT[:, co, blk * BLK:(blk + 1) * BLK]
                        P.tt(xs, pd, xs, ALU.add)
                        k += 1

            for g in range(0, NG, 2):
                gu_phase(g)
                gu_phase(g + 1)
                dn_pair(g)

        def ffn_stage():
            cur[0] = stage_off
            HT = alloc([KC, NT], BF16)
            AT = [alloc([2, NT], BF16) for _ in range(2)]
            for blk in range(NB):
                t0 = blk * BLK
                rms(lambda kc: XT[:, kc, t0:t0 + BLK], BLK, V_NF0,
                    lambda kc: HT[:, kc, t0:t0 + BLK], RSTD[:, t0:t0 + BLK])
            ffn_pass(HT, AT, d_fwg, d_fwu, d_fwd, 0, 0)

        def gmlp_stage():
            cur[0] = stage_off
            HP = alloc([KC, BLK], BF16)
            VBF = [Reg(arena, "sb", HP.off + i * 4096, [D], BF16) for i in range(2)]
            cur[0] = max(cur[0], HP.off + 12288)
            VRAW = alloc([3, D], F32)
            U = alloc([KC, BLK], BF16)
            WSM = alloc([2, 8, 128], BF16)
            BSB = alloc([2, 8, 128], F32)
            BROW = alloc([D], F32)
            STATS = alloc([4, 6], F32)
            MV = alloc([4], F32)
            LNG = Reg(arena, "sb", ring_off + 4 * 8192, [D], F32)
            LNB = Reg(arena, "sb", ring_off + 5 * 8192, [D], F32)
            WST = Reg(arena, "sb", VRAW.off, [2, 8, 128], F32)
            MSK = Reg(arena, "sb", VRAW.off + 8192, [2, 128], F32)
            P.dma("sp", WST[:, 0, :, :], d_wsp.rearrange("p (g t) -> p g t", g=8))
            P.dma("sp", WST[:, 1, :, :], d_wss.rearrange("p (g t) -> p g t", g=8))
            P.dma("sp", MSK.all(), d_masks.rearrange("p (v t) -> p v t", v=2))
            for v in range(2):
                for g in range(8):
                    P.tt(WSM[:, v, g, :], WST[:, v, g, :], MSK[:, v, :], ALU.mult)
            P.dma("sp", BSB[:, 0, :, :], d_rows[0, R_BSP:R_BSP + 1024].partition_broadcast(128)
                  .rearrange("p (g t) -> p g t", g=8))
            P.dma("sp", BSB[:, 1, :, :], d_rows[0, R_BSS:R_BSS + 1024].partition_broadcast(128)
                  .rearrange("p (g t) -> p g t", g=8))
            P.dma("sp", LNG.all(), d_rows[0, R_LNG:R_LNG + D].partition_broadcast(128))
            P.dma("sp", LNB.all(), d_rows[0, R_LNB:R_LNB + D].partition_broadcast(128))
            P.dma("sp", BROW[0:1, :], d_rows[0:1, R_GBV:R_GBV + D])
            for b in range(NB):
                t0 = b * BLK
                rms(lambda kc: XT[:, kc, t0:t0 + BLK], BLK, V_NM1,
                    lambda kc: HP[:, kc, :], RSTD[:, t0:t0 + BLK])
                unit = None
                for c in range(KC):
                    if c % 2 == 0:
                        unit = load_gu(d_gwin, 0, (c // 2) * 256, 4)
                    j = c % 2
                    pu = PS[c % 2][:, 0:BLK]
                    for kc in range(KC):
                        P.mm(pu, unit[:, kc, j * 128:(j + 1) * 128], HP[:, kc, :], start=(kc == 0),
                             stop=(kc == KC - 1), inc=(kc == KC - 1))
                    P.act(U[:, c, :], pu, AF.Gelu, bias=vcol(V_GBU, c))
                k = 0
                for c2 in range(8):
                    unit = load_gu(d_gwin, 0, D + c2 * 256, 4)
                    for t in range(3):
                        pv = PS[2 + k % 2][:, 0:256]
                        for kc in range(KC):
                            P.mm(pv, HP[:, kc, t * 128:(t + 1) * 128], unit[:, kc, :], start=(kc == 0),
                                 stop=False)
                        P.mm(pv, ONES[0:1, :], BROW[0:1, c2 * 256:(c2 + 1) * 256], start=False, stop=True,
                             inc=True)
                        P.act(VRAW[:, t, c2 * 256:(c2 + 1) * 256], pv, AF.Gelu)
                        k += 1
                for t in range(3):
                    vr = VRAW[:, t, :]
                    for q in range(4):
                        so, si = STATS[:, q, :], VRAW[:, t, q * 512:(q + 1) * 512]
                        P.op("dve", lambda e, so=so, si=si: e.bn_stats(out=so.ap, in_=si.ap), [so], [si])
                    mv = MV[:, 0:2]
                    sa = STATS.all()
                    sflat = V(STATS.ap.rearrange("p a b -> p (a b)"), "sb", STATS.off, STATS.off + STATS.nbytes)
                    P.op("dve", lambda e, mv=mv, sflat=sflat: e.bn_aggr(out=mv.ap, in_=sflat.ap), [mv], [sflat])
                    rs = MV[:, 2:3]
                    P.act(rs, MV[:, 1:2], AF.Sqrt, bias=EPSL, scale=1.0)
                    P.op("dve", lambda e, rs=rs: e.reciprocal(out=rs.ap, in_=rs.ap), [rs], [rs])
                    P.ts(vr, vr, MV[:, 0:1], ALU.subtract, rs, ALU.mult)
                    P.tt(vr, vr, LNG.all(), ALU.mult)
                    P.tt(vr, vr, LNB.all(), ALU.add)
                    sample = (b == 2 and t == 2)
                    if sample:
                        P.dma("sp", o_vout, vr, is_output=True)
                    vb = VBF[t % 2]
                    P.copy(vb.all(), vr)
                    var = 1 if sample else 0
                    for c in range(KC):
                        g = c // 2
                        pm = PS[4 + c % 2][:, 0:128]
                        P.mm(pm, vb[:, c * 128:(c + 1) * 128], WSM[:, var, g, :], start=True, stop=True, inc=True)
                        tmp = SCR[c % 2][:, 0:128]
                        P.tt(tmp, pm, BSB[:, var, g, :], ALU.add)
                        us = U[:, c, t * 128:(t + 1) * 128]
                        P.tt(us, tmp, us, ALU.mult)
                out_proj(d_gwout, lambda kc: U[:, kc, :], BLK, t0, V_GBOUT, 4)

        def moe_stage():
            cur[0] = stage_off
            HT = alloc([KC, NT], BF16)
            AT = [alloc([2, NT], BF16) for _ in range(2)]
            CB = alloc([NT], F32)
            GR = alloc([KC, NE], F32)
            COMB = alloc([9, NE], F32)
            LG = alloc([NE], F32)
            L2 = alloc([NE], F32)
            EQ1 = alloc([NE], F32)
            EQ2 = alloc([NE], F32)
            SM = alloc([8], F32)
            for blk in range(NB):
                t0 = blk * BLK
                rms(lambda kc: XT[:, kc, t0:t0 + BLK], BLK, V_NF1,
                    lambda kc: HT[:, kc, t0:t0 + BLK], RSTD[:, t0:t0 + BLK])
            P.dma("sp", GR.all(), d_router.rearrange("(k p) e -> p k e", p=128))
            for kc in range(KC):
                P.ts(GR[:, kc, :], GR[:, kc, :], vcol(V_NF1, kc), ALU.mult)
            for t in range(9):
                pl = PS[7][:, 0:NE]
                for kc in range(KC):
                    P.mm(pl, XT[:, kc, t * 128:(t + 1) * 128], GR[:, kc, :], start=(kc == 0),
                         stop=(kc == KC - 1), inc=(kc == KC - 1))
                pr = PS[6][:, 0:1]
                P.mm(pr, RSTD[0:1, t * 128:(t + 1) * 128], ONES[0:1, 0:1], start=True, stop=True, inc=True)
                rc = SM[:, 0:1]
                P.copy(rc, pr)
                P.ts(LG.all(), pl, rc, ALU.mult)
                m1 = SM[:, 1:2]
                m2 = SM[:, 2:3]
                o1, i1 = m1.ap, LG.all().ap
                P.op("dve", lambda e, o1=o1, i1=i1: e.reduce_max(out=o1, in_=i1, axis=mybir.AxisListType.X),
                     [m1], [LG.all()])
                P.ts(EQ1.all(), LG.all(), m1, ALU.is_equal)
                P.stt(L2.all(), EQ1.all(), -1e30, LG.all(), ALU.mult, ALU.add)
                o2, i2 = m2.ap, L2.all().ap
                P.op("dve", lambda e, o2=o2, i2=i2: e.reduce_max(out=o2, in_=i2, axis=mybir.AxisListType.X),
                     [m2], [L2.all()])
                P.ts(EQ2.all(), L2.all(), m2, ALU.is_equal)
                dl = SM[:, 3:4]
                P.tt(dl, m2, m1, ALU.subtract)
                ex = SM[:, 4:5]
                P.act(ex, dl, AF.Exp)
                g1 = SM[:, 5:6]
                P.ts(g1, ex, 1.0, ALU.add)
                P.op("dve", lambda e, g1=g1: e.reciprocal(out=g1.ap, in_=g1.ap), [g1], [g1])
                g2 = SM[:, 6:7]
                P.tt(g2, ex, g1, ALU.mult)
                P.ts(EQ1.all(), EQ1.all(), g1, ALU.mult)
                P.stt(COMB[:, t, :], EQ2.all(), g2, EQ1.all(), ALU.mult, ALU.add)
            for e_i in range(n_exp):
                for t in range(9):
                    lb = SCR2[t % 2][:, 0:128]
                    P.ts(lb, ONES.all(), COMB[:, t, e_i:e_i + 1], ALU.mult)
                    pc = PS[7][:, (t % 3) * 128:(t % 3 + 1) * 128]
                    P.mm(pc, lb, IDENT.all(), start=True, stop=True, inc=True)
                    if t % 3 == 2:
                        blk = t // 3
                        P.copy(CB[:, blk * BLK:(blk + 1) * BLK], PS[7][:, 0:BLK])
                ffn_pass(HT, AT, d_mwg, d_mwu, d_mwd, e_i * D, e_i * FF, CB=CB)

        def moe_routed_stage(S0):
            NSC = S0 // 128
            NPASS = -(-NT // S0)
            NG = FC // 2
            cur[0] = HALOS.off
            HTK2 = [alloc([3, D], BF16) for _ in range(2)]
            hg_off = cur[0]
            cur[0] += max(KC * S0 * 2, KC * BLK * 2)
            HG = Reg(arena, "sb", hg_off, [KC, S0], BF16)
            HTB = Reg(arena, "sb", hg_off, [KC, BLK], BF16)
            OBF = alloc([NSC, D], BF16)
            OACC = alloc([NSC, D], F32)
            PMS = Reg(arena, "sb", MISC.off + 16 * 4, [12], F32)
            GSB = [Reg(arena, "sb", MISC.off + (32 + 4 * i) * 4, [4], F32) for i in range(2)]
            assert cur[0] <= ARENA_BYTES, cur[0]
            c5 = [ring_off + 5 * 8192]

            def a5(shape, dtype):
                esz = 2 if dtype == BF16 else 4
                nb = (_prod(shape) * esz + 3) // 4 * 4
                r = Reg(arena, "sb", c5[0], shape, dtype)
                c5[0] += nb
                assert c5[0] <= ring_off + 6 * 8192, c5[0]
                return r
            AT = [a5([2, S0], BF16) for _ in range(2)]
            COMB = a5([9, NE], F32)
            RR = a5([9, NE], F32)
            POSM = a5([9, NE], F32)
            HIF = a5([9, NE], F32)
            HIB = a5([9, NE], BF16)
            GHL = a5([9, NE, 2], BF16)
            CNT = a5([NE], F32)
            FLG = a5([32], F32)
            FLGI = a5([32], I32)
            PM = a5([12], F32)
            GS = a5([4], F32)
            GT = a5([4, 2], F32)
            IOTA_ROW = a5([S0], F32)
            IOTA_COL = a5([4], F32)
            UT = a5([128], F32)
            IDENTB = a5([128], BF16)
            LG = a5([NE], F32)
            L2 = a5([NE], F32)
            EQ1 = a5([NE], F32)
            EQ2 = a5([NE], F32)
            SM = a5([8], F32)
            GR = a5([KC, NE], F32)
            SEL = [Reg(arena, "sb", RSTD.off + i * 2304, [3, S0], BF16) for i in range(2)]
            SELT = [Reg(arena, "sb", RSTD.off + i * 2304, [NSC, BLK], BF16) for i in range(2)]

            P.dma("sp", IOTA_ROW.all(), d_iotas[:, 0:S0])
            P.dma("sp", IOTA_COL.all(), d_iotas[:, 512:516])
            P.dma("sp", UT.all(), d_ut)
            P.dma("sp", GR.all(), d_router.rearrange("(k p) e -> p k e", p=128))
            P.copy(IDENTB.all(), IDENT.all())
            htok_writes = []
            for blk in range(NB):
                t0 = blk * BLK
                rms(lambda kc: XT[:, kc, t0:t0 + BLK], BLK, V_NF1,
                    lambda kc: HTB[:, kc, :], RSTD[:, t0:t0 + BLK])
                k = 0
                for tl in range(3):
                    t = 3 * blk + tl
                    for q4 in range(4):
                        ps = PS[k % 2]
                        for i in range(4):
                            kc = 4 * q4 + i
                            P.mm(ps[:, i * 128:(i + 1) * 128], HTB[:, kc, tl * 128:(tl + 1) * 128], IDENTB.all(),
                                 start=True, stop=True, inc=(i == 3))
                        P.copy(HTK2[blk % 2][:, tl, q4 * 512:(q4 + 1) * 512], ps[:, 0:512])
                        k += 1
                htok_writes.append(P.dma("sp", d_htok[3 * blk:3 * blk + 3].rearrange("t p d -> p t d"),
                                         HTK2[blk % 2].all()))
            for kc in range(KC):
                P.ts(GR[:, kc, :], GR[:, kc, :], vcol(V_NF1, kc), ALU.mult)
            for t in range(9):
                pl = PS[7][:, 0:NE]
                for kc in range(KC):
                    P.mm(pl, XT[:, kc, t * 128:(t + 1) * 128], GR[:, kc, :], start=(kc == 0),
                         stop=(kc == KC - 1), inc=(kc == KC - 1))
                pr = PS[6][:, 0:1]
                P.mm(pr, RSTD[0:1, t * 128:(t + 1) * 128], ONES[0:1, 0:1], start=True, stop=True, inc=True)
                rc = SM[:, 0:1]
                P.copy(rc, pr)
                P.ts(LG.all(), pl, rc, ALU.mult)
                m1 = SM[:, 1:2]
                m2 = SM[:, 2:3]
                o1, i1 = m1.ap, LG.all().ap
                P.op("dve", lambda e, o1=o1, i1=i1: e.reduce_max(out=o1, in_=i1, axis=mybir.AxisListType.X),
                     [m1], [LG.all()])
                P.ts(EQ1.all(), LG.all(), m1, ALU.is_equal)
                P.stt(L2.all(), EQ1.all(), -1e30, LG.all(), ALU.mult, ALU.add)
                o2, i2 = m2.ap, L2.all().ap
                P.op("dve", lambda e, o2=o2, i2=i2: e.reduce_max(out=o2, in_=i2, axis=mybir.AxisListType.X),
                     [m2], [L2.all()])
                P.ts(EQ2.all(), L2.all(), m2, ALU.is_equal)
                dl = SM[:, 3:4]
                P.tt(dl, m2, m1, ALU.subtract)
                ex = SM[:, 4:5]
                P.act(ex, dl, AF.Exp)
                g1 = SM[:, 5:6]
                P.ts(g1, ex, 1.0, ALU.add)
                P.op("dve", lambda e, g1=g1: e.reciprocal(out=g1.ap, in_=g1.ap), [g1], [g1])
                g2 = SM[:, 6:7]
                P.tt(g2, ex, g1, ALU.mult)
                P.ts(EQ1.all(), EQ1.all(), g1, ALU.mult)
                P.stt(COMB[:, t, :], EQ2.all(), g2, EQ1.all(), ALU.mult, ALU.add)
            P.ts(RR.all(), COMB.all(), 0.0, ALU.is_gt)
            for t in range(9):
                ps = PS[7][:, 0:NE]
                for t2 in range(t):
                    P.mm(ps, ONES.all(), RR[:, t2, :], start=(t2 == 0), stop=False)
                P.mm(ps, UT.all(), RR[:, t, :], start=(t == 0), stop=True, inc=True)
                P.stt(POSM[:, t, :], ps, 1.0, RR[:, t, :], ALU.add, ALU.mult)
                P.ts(POSM[:, t, :], POSM[:, t, :], -1.0, ALU.add)
            pc = PS[6][:, 0:NE]
            for t in range(9):
                P.mm(pc, ONES.all(), RR[:, t, :], start=(t == 0), stop=(t == 8), inc=(t == 8))
            P.copy(CNT.all(), pc)
            P.memset(FLG.all(), 0.0)
            for p in range(1, NPASS):
                P.ts(FLG[:, (p - 1) * NE:p * NE], CNT.all(), float(p * S0), ALU.is_gt)
            P.copy(FLGI.all(), FLG.all())
            P.copy(HIB.all(), COMB.all())
            P.copy(HIF.all(), HIB.all())
            P.copy(GHL[:, :, :, 0], HIB.all())
            P.tt(GHL[:, :, :, 1], COMB.all(), HIF.all(), ALU.subtract)

            mcount = [0]
            hcount = [0]

            def gather(e_i, p, gs, queue):
                P.ts(PM[:, 0:9], POSM[:, :, e_i], float(-p * S0), ALU.add)
                pgs = PS[6]
                for tb in range(3):
                    hb = HTK2[hcount[0] % 2]
                    hcount[0] += 1
                    for wsem, wval in htok_writes:
                        P.wait(queue, wsem, wval)
                    P.dma(queue, hb.all(), d_htok[3 * tb:3 * tb + 3].rearrange("t p d -> p t d"),
                          sem=("R5" if queue == "pool" else None))
                    sel = SEL[tb % 2]
                    for tl in range(3):
                        t = 3 * tb + tl
                        P.ts(sel[:, tl, :], IOTA_ROW.all(), PM[:, t:t + 1], ALU.is_equal)
                    for kc in range(KC):
                        ps = PS[4 + kc % 2][:, 0:S0]
                        for tl in range(3):
                            P.mm(ps, hb[:, tl, kc * 128:(kc + 1) * 128], sel[:, tl, :], start=(tl == 0),
                                 stop=(tl == 2), inc=(tl == 2))
                        if tb == 0:
                            P.copy(HG[:, kc, :], ps)
                        else:
                            P.tt(HG[:, kc, :], ps, HG[:, kc, :], ALU.add)
                    for sc in range(NSC):
                        for tl in range(3):
                            t = 3 * tb + tl
                            P.mm(pgs[:, 2 * sc:2 * sc + 2], sel[:, tl, sc * 128:(sc + 1) * 128], GHL[:, t, e_i, :],
                                 start=(tl == 0), stop=(tl == 2), inc=(tl == 2))
                    gt = V(GT.ap.rearrange("p a b -> p (a b)")[:, 0:2 * NSC], "sb", GT.off, GT.off + GT.nbytes)
                    if tb == 0:
                        P.copy(gt, pgs[:, 0:2 * NSC])
                    else:
                        P.tt(gt, pgs[:, 0:2 * NSC], gt, ALU.add)
                P.tt(gs[:, 0:NSC], GT[:, 0:NSC, 0], GT[:, 0:NSC, 1], ALU.add)

            def swiglu(e_i, hooks):
                seq = []
                for g in range(0, NG, 2):
                    seq += [("g", g), ("u", g), ("g", g + 1), ("u", g + 1), ("d", g), ("d", g + 1)]
                where = {it: i for i, it in enumerate(seq)}
                loaded = {}
                nxt = [0]

                def ensure(item):
                    while nxt[0] <= where[item]:
                        kind, g = seq[nxt[0]]
                        slot = mcount[0] % 5
                        mcount[0] += 1
                        if kind == "d":
                            r = ring_dn(slot)
                            src = d_mwd[e_i * FF + g * 256:e_i * FF + (g + 1) * 256, :].rearrange(
                                "(j p) c -> p j c", p=128)
                        else:
                            r = ring_gu(slot)
                            w_ap = d_mwg if kind == "g" else d_mwu
                            src = w_ap[e_i * D:(e_i + 1) * D, g * 256:(g + 1) * 256].rearrange(
                                "(k p) c -> p k c", p=128)
                        P.dma("pool", r.all(), src, sem="R%d" % slot)
                        loaded[(kind, g)] = r
                        nxt[0] += 1
                    return loaded[item]

                def gu_phase(g):
                    ug = ensure(("g", g))
                    uu = ensure(("u", g))
                    at = AT[g % 2]
                    for j in range(2):
                        pg = PS[j % 2][:, 0:S0]
                        pu = PS[2 + j % 2][:, 0:S0]
                        for kc in range(KC):
                            P.mm(pg, ug[:, kc, j * 128:(j + 1) * 128], HG[:, kc, :],
                                 start=(kc == 0), stop=(kc == KC - 1), inc=(kc == KC - 1))
                        for kc in range(KC):
                            P.mm(pu, uu[:, kc, j * 128:(j + 1) * 128], HG[:, kc, :],
                                 start=(kc == 0), stop=(kc == KC - 1), inc=(kc == KC - 1))
                        sg = SCR[j % 2][:, 0:S0]
                        P.act(sg, pg, AF.Silu)
                        P.tt(at[:, j, :], pu, sg, ALU.mult)

                def dn_pair(g):
                    uds = [ensure(("d", g)), ensure(("d", g + 1))]
                    k = 0
                    for sc in range(NSC):
                        for dq in range(4):
                            pd = PS[4 + k % 2][:, 0:512]
                            for i in range(4):
                                at, ud, j = AT[(g + i // 2) % 2], uds[i // 2], i % 2
                                P.mm(pd, at[:, j, sc * 128:(sc + 1) * 128], ud[:, j, dq * 512:(dq + 1) * 512],
                                     start=(i == 0), stop=(i == 3), inc=(i == 3))
                            oa = OACC[:, sc, dq * 512:(dq + 1) * 512]
                            if g == 0:
                                P.copy(oa, pd)
                            else:
                                P.tt(oa, pd, oa, ALU.add)
                            k += 1

                for g in range(0, NG, 2):
                    for kind, gg in (("gu", g), ("gu", g + 1), ("dn", g)):
                        if kind == "gu":
                            gu_phase(gg)
                        else:
                            dn_pair(gg)
                        if (kind, gg) in hooks:
                            hooks[(kind, gg)]()

            def gate(gs):
                for sc in range(NSC):
                    P.ts(OBF[:, sc, :], OACC[:, sc, :], gs[:, sc:sc + 1], ALU.mult)

            kscat = [0]

            def scatter_prep(e_i, p):
                P.ts(PMS[:, 0:9], POSM[:, :, e_i], float(-p * S0), ALU.add)

            def scatter_chunk(blk):
                for tl in range(3):
                    t = 3 * blk + tl
                    lb = SCR2[0][:, tl * 128:(tl + 1) * 128]
                    P.ts(lb, ONES.all(), PMS[:, t:t + 1], ALU.mult)
                    P.mm(PS[7][:, tl * 128:(tl + 1) * 128], lb, IDENT.all(), start=True, stop=True, inc=True)
                prow = SCR2[1][:, 0:BLK]
                P.copy(prow, PS[7][:, 0:BLK])
                selt = SELT[blk % 2]
                for sc in range(NSC):
                    P.ts(selt[:, sc, :], prow, IOTA_COL[:, sc:sc + 1], ALU.is_equal)
                for co in range(KC):
                    ps = PS[6 + kscat[0] % 2][:, 0:BLK]
                    kscat[0] += 1
                    for sc in range(NSC):
                        P.mm(ps, OBF[:, sc, co * 128:(co + 1) * 128], selt[:, sc, :], start=(sc == 0),
                             stop=(sc == NSC - 1), inc=(sc == NSC - 1))
                    xs = XT[:, co, blk * BLK:(blk + 1) * BLK]
                    P.tt(xs, ps, xs, ALU.add)

            gather(0, 0, GSB[0], "sp")
            for e_i in range(n_exp):
                hooks = {}
                if e_i > 0:
                    def mk(blk, prev=e_i - 1):
                        def f():
                            if blk == 0:
                                scatter_prep(prev, 0)
                            scatter_chunk(blk)
                        return f
                    hooks[("gu", 0)] = mk(0)
                    hooks[("gu", 1)] = mk(1)
                    hooks[("gu", 2)] = mk(2)
                if e_i + 1 < n_exp:
                    hooks[("gu", NG - 1)] = (lambda nxt_e=e_i + 1: gather(nxt_e, 0, GSB[nxt_e % 2], "sp"))
                swiglu(e_i, hooks)
                gate(GSB[e_i % 2])
            scatter_prep(n_exp - 1, 0)
            for blk in range(NB):
                scatter_chunk(blk)

            npass_emit = int(os.environ.get("MK_NPASS", str(NPASS)))
            for e_i in range(n_exp):
                for p in range(1, min(NPASS, npass_emit)):
                    fi = (p - 1) * NE + e_i
                    P.region_begin(["pe", "act", "dve", "pool"], FLGI[0:1, fi:fi + 1])
                    gather(e_i, p, GSB[0], "pool")
                    swiglu(e_i, {})
                    gate(GSB[0])
                    scatter_prep(e_i, p)
                    for blk in range(NB):
                        scatter_chunk(blk)
                    P.region_end()

        def final_stage():
            cur[0] = stage_off
            YO = [alloc([KC, BLK], F32) for _ in range(2)]
            for blk in range(NB):
                t0 = blk * BLK
                yo = YO[blk % 2]
                rms(lambda kc: XT[:, kc, t0:t0 + BLK], BLK, V_NFIN,
                    lambda kc: yo[:, kc, :], RSTD[:, t0:t0 + BLK])
                for q4 in range(4):
                    P.dma("sp", o_yT[512 * q4:512 * (q4 + 1), t0:t0 + BLK].rearrange("(k p) t -> p k t", p=128),
                          yo[:, 4 * q4:4 * q4 + 4, :], is_output=True)

        if "conv" in stages:
            conv_stage()
        if "ffn" in stages:
            ffn_stage()
        if "gmlp" in stages:
            gmlp_stage()
        if "moe" in stages:
            if os.environ.get("MK_MOE", "routed") == "dense":
                moe_stage()
            else:
                moe_routed_stage(int(os.environ.get("MK_S0", "384")))
        final_stage()
        P.finish()

        with nc.Block() as block:
            @block.tensor
            def _(e):
                replay(e, P.q["pe"])

            @block.scalar
            def _(e):
                replay(e, P.q["act"])

            @block.vector
            def _(e):
                replay(e, P.q["dve"])

            @block.gpsimd
            def _(e):
                replay(e, P.q["pool"])

            @block.sync
            def _(e):
                replay(e, P.q["sp"])
        stats = {k: len(v) for k, v in P.q.items()}
        stats["waits"] = P.n_wait
    return nc, stats


def _pack_cols(vec):
    return np.ascontiguousarray(np.asarray(vec, np.float32).reshape(KC, 128).T)


def kernel(x_prompt, x_sample, state_conv, norm_mix, norm_ffn, norm_final,
           conv_w_in, conv_b_in, conv_w_dw, conv_b_dw, conv_ln_g, conv_ln_b, conv_w_out, conv_b_out,
           gmlp_w_in, gmlp_b_in, gmlp_ln_g, gmlp_ln_b, gmlp_w_s, gmlp_b_s, gmlp_w_out, gmlp_b_out,
           ffn_w_gate, ffn_w_up, ffn_w_down, moe_router, moe_w_gate, moe_w_up, moe_w_down):
    stages = os.environ.get("MK_STAGES", "conv,ffn,gmlp,moe").split(",")
    n_exp = int(os.environ.get("MK_NEXP", str(NE)))
    f = np.float32
    x_prompt = np.asarray(x_prompt, f)
    x_sample = np.asarray(x_sample, f)
    state_conv = np.asarray(state_conv, f)

    cols = [norm_mix[0], norm_mix[1], norm_ffn[0], norm_ffn[1], norm_final,
            conv_b_in[0, :D], conv_b_in[0, D:], conv_b_dw[0], conv_ln_g[0], conv_ln_b[0], conv_b_out[0],
            gmlp_b_in[0, :D], gmlp_b_out[0]]
    cols += [conv_w_dw[0, j] for j in range(NTAP)]
    vecs = np.ascontiguousarray(np.concatenate([_pack_cols(c) for c in cols], axis=1))
    assert vecs.shape == (128, NV)
    rows = np.zeros((1, NR), f)
    rows[0, R_GBV:R_GBV + D] = np.asarray(gmlp_b_in, f)[0, D:]
    rows[0, R_LNG:R_LNG + D] = np.asarray(gmlp_ln_g, f)[0]
    rows[0, R_LNB:R_LNB + D] = np.asarray(gmlp_ln_b, f)[0]
    bs = np.asarray(gmlp_b_s, f)[0]
    rows[0, R_BSP:R_BSP + 1024] = bs.reshape(-1)
    rows[0, R_BSS:R_BSS + 1024] = np.tile(bs[:, :DSEQ], (1, NSEQ)).reshape(-1)
    ws = np.asarray(gmlp_w_s, f)[0]
    wsp = np.ascontiguousarray(ws.transpose(2, 0, 1)).reshape(128, 1024)
    ws8 = ws[:, :DSEQ, :DSEQ].transpose(2, 0, 1)
    wss = np.ascontiguousarray(np.tile(ws8, (NSEQ, 1, NSEQ))).reshape(128, 1024)
    ii = np.arange(128)
    mtril = (ii[:, None] <= ii[None, :]).astype(f)
    mbd = mtril * ((ii[:, None] // DSEQ) == (ii[None, :] // DSEQ)).astype(f)
    masks = np.ascontiguousarray(np.concatenate([mtril, mbd], axis=1))
    ident = np.eye(128, dtype=f)
    iotas = np.zeros((128, 516), f)
    iotas[:, :512] = np.arange(512, dtype=f)[None, :]
    iotas[:, 512:516] = ii[:, None].astype(f) + 128.0 * np.arange(4, dtype=f)[None, :]
    ut = (ii[:, None] < ii[None, :]).astype(f)
    shared = {
        "iotas": iotas, "ut": ut,
        "vecs": vecs, "rows": rows, "wsp": wsp, "wss": wss, "masks": masks, "ident": ident,
        "router": np.ascontiguousarray(np.asarray(moe_router, f)[0]),
        "conv_w_in": np.asarray(conv_w_in, f)[0], "conv_w_out": np.asarray(conv_w_out, f)[0],
        "gmlp_w_in": np.asarray(gmlp_w_in, f)[0], "gmlp_w_out": np.asarray(gmlp_w_out, f)[0],
        "ffn_wg": np.asarray(ffn_w_gate, f)[0], "ffn_wu": np.asarray(ffn_w_up, f)[0],
        "ffn_wd": np.asarray(ffn_w_down, f)[0],
        "moe_wg": np.asarray(moe_w_gate, f).reshape(NE * D, FF),
        "moe_wu": np.asarray(moe_w_up, f).reshape(NE * D, FF),
        "moe_wd": np.asarray(moe_w_down, f).reshape(NE * FF, D),
    }
    in_maps = []
    for c in range(NCORES):
        b, half = c // 2, c % 2
        xp = x_prompt[b, half * 1024:(half + 1) * 1024]
        xs = x_sample[c * NSEQ:(c + 1) * NSEQ].reshape(NSEQ * DSEQ, D)
        xT = np.ascontiguousarray(np.concatenate([xp, xs], axis=0).T)
        if half == 1:
            xh = np.ascontiguousarray(x_prompt[b, 1024 - HALO:1024].T)
        else:
            xh = np.zeros((D, HALO), f)
        hmask = np.full((128, 1), float(half), f)
        sc = state_conv[0, c * NSEQ:(c + 1) * NSEQ]
        sconv = np.ascontiguousarray(sc.transpose(2, 0, 1)).reshape(D, NSEQ * HALO)
        m = {"xT": xT, "xh": xh, "hmask": hmask, "sconv": sconv}
        m.update(shared)
        in_maps.append(m)

    nc, _ = build_program(stages, n_exp)
    res = run_bass_kernel_spmd(nc, in_maps, core_ids=list(range(NCORES)))
    outs = res.results

    y_prompt = np.empty((4, 2048, D), f)
    y_sample = np.empty((128, DSEQ, D), f)
    ncp = np.empty((1, 4, HALO, D), f)
    ncs = np.empty((1, 128, HALO, D), f)
    vout = np.empty((1, 128, DSEQ, D), f)
    for c in range(NCORES):
        b, half = c // 2, c % 2
        yT = np.asarray(outs[c]["yT"])
        y_prompt[b, half * 1024:(half + 1) * 1024] = yT[:, :1024].T
        y_sample[c * NSEQ:(c + 1) * NSEQ] = yT[:, 1024:].T.reshape(NSEQ, DSEQ, D)
        if half == 1:
            ncp[0, b] = np.asarray(outs[c]["ncp"]).T
        ncs[0, c * NSEQ:(c + 1) * NSEQ] = np.asarray(outs[c]["ncs"]).reshape(D, NSEQ, HALO).transpose(1, 2, 0)
        vout[0, c * NSEQ:(c + 1) * NSEQ] = np.asarray(outs[c]["vout"]).reshape(NSEQ, DSEQ, D)
    return (y_prompt, y_sample, ncp, ncs, vout)
```
